# Optimizing a Trainium2 kernel written in Bass

```python
import jax, jax.numpy as jnp
from jax import lax
import numpy as np

D_MODEL = 2048
BATCH = 16
SEQ = 2048
DEPTH = 1

HEAD_DIM = 128
A_HEADS = D_MODEL // (2 * HEAD_DIM)
A_WIDTH = A_HEADS * HEAD_DIM
DILATED_PAIRS = ((128, 1), (512, 4), (2048, 16))
BLK = 128
ROPE_THETA = 500000.0
A_ROT_DIM = HEAD_DIM // 4

B_HEADS = D_MODEL // (2 * HEAD_DIM)
MLA_Q_RANK = D_MODEL // 4
MLA_KV_RANK = D_MODEL // 4
MLA_NOPE = 128
MLA_ROPE = 64
MLA_V = 128
B_WIDTH = B_HEADS * MLA_V

MIX_WIDTH = A_WIDTH + B_WIDTH
IN_SPLITS = [A_WIDTH, 2 * A_WIDTH, 3 * A_WIDTH, 3 * A_WIDTH + MLA_Q_RANK,
             3 * A_WIDTH + MLA_Q_RANK + MLA_KV_RANK]
IN_COLS = 3 * A_WIDTH + MLA_Q_RANK + MLA_KV_RANK + MLA_ROPE

PEER_HEADS = 8
PEER_N_KEYS = 128
PEER_N_EXPERTS = PEER_N_KEYS ** 2
PEER_TOPK = 16
PEER_KEY_DIM = 256
PEER_CHUNK = 128

DN_ALPHA = (2 * DEPTH) ** 0.25
DN_BETA = (8 * DEPTH) ** -0.25

kernel_name = 'hybrid_dilated_mla_peer_block'


def layer_norm(x, g=None, b=None, eps=1e-5):
    xf = x.astype(jnp.float32)
    mu = xf.mean(-1, keepdims=True)
    var = jnp.square(xf - mu).mean(-1, keepdims=True)
    y = (xf - mu) * lax.rsqrt(var + eps)
    if g is not None:
        y = y * g.astype(jnp.float32) + b.astype(jnp.float32)
    return y.astype(x.dtype)


def rms_norm(x, g, eps=1e-6):
    xf = x.astype(jnp.float32)
    y = xf * lax.rsqrt(jnp.mean(xf * xf, -1, keepdims=True) + eps) * g.astype(jnp.float32)
    return y.astype(x.dtype)


def rope(x, pos, rot_dim):
    half = rot_dim // 2
    inv = ROPE_THETA ** (-jnp.arange(half, dtype=jnp.float32) * 2.0 / rot_dim)
    ang = pos[:, None] * inv[None, :]
    cos = jnp.cos(ang)[:, None, :]
    sin = jnp.sin(ang)[:, None, :]
    xf = x.astype(jnp.float32)
    x1 = xf[..., :half]
    x2 = xf[..., half:rot_dim]
    out = jnp.concatenate([x1 * cos - x2 * sin, x2 * cos + x1 * sin, xf[..., rot_dim:]], -1)
    return out.astype(x.dtype)


def dilated_window_attn(q, k, v, n_back, dil):
    B, S, H, E = q.shape
    L = S // dil
    nb = -(-L // BLK)
    Lp = nb * BLK

    def blocks(t):
        t = t.reshape(B, L, dil, H, E)
        t = jnp.pad(t, ((0, 0), (0, Lp - L), (0, 0), (0, 0), (0, 0)))
        return t.reshape(B, nb, BLK, dil, H, E)

    def with_prev(t):
        prev = jnp.pad(t, ((0, 0), (1, 0), (0, 0), (0, 0), (0, 0), (0, 0)))[:, :-1]
        return jnp.concatenate([prev, t], axis=2)

    qb = blocks(q)
    kk = with_prev(blocks(k))
    vv = with_prev(blocks(v))
    s = jnp.einsum('bnqrhe,bnkrhe->bnrhqk', qb, kk).astype(jnp.float32) * (E ** -0.5)
    qpos = jnp.arange(nb)[:, None] * BLK + jnp.arange(BLK)[None, :]
    kpos = jnp.arange(nb)[:, None] * BLK - BLK + jnp.arange(2 * BLK)[None, :]
    dist = qpos[:, :, None] - kpos[:, None, :]
    valid = (dist >= 0) & (dist <= n_back) & (kpos[:, None, :] >= 0)
    s = jnp.where(valid[None, :, None, None], s, -jnp.inf)
    m = s.max(-1, keepdims=True)
    p = jnp.exp(s - m)
    l = p.sum(-1, keepdims=True)
    o = jnp.einsum('bnrhqk,bnkrhe->bnqrhe', p.astype(vv.dtype), vv).astype(jnp.float32)
    l_t = jnp.moveaxis(l[..., 0], -1, 2)
    lse = jnp.moveaxis((m + jnp.log(l))[..., 0], -1, 2)
    o = o / l_t[..., None]
    o = o.reshape(B, Lp, dil, H, E)[:, :L].reshape(B, S, H, E)
    lse = lse.reshape(B, Lp, dil, H)[:, :L].reshape(B, S, H)
    return o, lse


def dilated_mixture(q, k, v):
    outs, lses = [], []
    for window, dil in DILATED_PAIRS:
        o, lse = dilated_window_attn(q, k, v, window // dil, dil)
        outs.append(o)
        lses.append(lse)
    wts = jax.nn.softmax(jnp.stack(lses, 0), axis=0)
    o = jnp.sum(wts[..., None] * jnp.stack(outs, 0), axis=0)
    return o.astype(q.dtype)


def mla_attention(q_nope, q_rope, k_nope, k_rope, v):
    B, S, H, _ = q_nope.shape
    nq = S // BLK
    scale = (MLA_NOPE + MLA_ROPE) ** -0.5
    kpos = jnp.arange(S)

    def to_blocks(t):
        return jnp.moveaxis(t.reshape(B, nq, BLK, *t.shape[2:]), 1, 0)

    def one_block(args):
        qn, qr, start = args
        s = (jnp.einsum('bqhe,bkhe->bhqk', qn, k_nope).astype(jnp.float32)
             + jnp.einsum('bqhr,bkr->bhqk', qr, k_rope).astype(jnp.float32)) * scale
        qpos = start + jnp.arange(BLK)
        s = jnp.where(kpos[None, :] <= qpos[:, None], s, -jnp.inf)
        p = jax.nn.softmax(s, axis=-1).astype(v.dtype)
        return jnp.einsum('bhqk,bkhe->bqhe', p, v)

    out = lax.map(one_block, (to_blocks(q_nope), to_blocks(q_rope), jnp.arange(nq) * BLK))
    return jnp.moveaxis(out, 0, 1).reshape(B, S, H, MLA_V)


def token_mix(h, pos, w_in, g_q_lat, g_kv_lat, w_uq, w_uk, w_uv, g_out_a, g_out_b, w_o):
    B, S, _ = h.shape
    proj = jnp.dot(h, w_in)
    qa, ka, va, cq, ckv, kr = jnp.split(proj, IN_SPLITS, axis=-1)
    qa = rope(qa.reshape(B, S, A_HEADS, HEAD_DIM), pos, A_ROT_DIM)
    ka = rope(ka.reshape(B, S, A_HEADS, HEAD_DIM), pos, A_ROT_DIM)
    va = va.reshape(B, S, A_HEADS, HEAD_DIM)
    o_a = dilated_mixture(qa, ka, va)
    cq = rms_norm(cq, g_q_lat)
    ckv = rms_norm(ckv, g_kv_lat)
    qb = jnp.dot(cq, w_uq).reshape(B, S, B_HEADS, MLA_NOPE + MLA_ROPE)
    q_nope = qb[..., :MLA_NOPE]
    q_rope = rope(qb[..., MLA_NOPE:], pos, MLA_ROPE)
    k_nope = jnp.dot(ckv, w_uk).reshape(B, S, B_HEADS, MLA_NOPE)
    v_b = jnp.dot(ckv, w_uv).reshape(B, S, B_HEADS, MLA_V)
    k_rope = rope(kr[:, :, None, :], pos, MLA_ROPE)[:, :, 0, :]
    o_b = mla_attention(q_nope, q_rope, k_nope, k_rope, v_b)
    o = jnp.concatenate([rms_norm(o_a.reshape(B, S, A_WIDTH), g_out_a),
                         rms_norm(o_b.reshape(B, S, B_WIDTH), g_out_b)], axis=-1)
    return jnp.dot(o, w_o)


def peer(h, w_pq, sub_key_1, sub_key_2, u_table, v_table):
    B, S, D = h.shape
    q = jnp.dot(h, w_pq).reshape(B, S, PEER_HEADS, 2, PEER_KEY_DIM // 2)
    s1 = jnp.einsum('bshe,ke->bshk', q[..., 0, :], sub_key_1).astype(jnp.float32)
    s2 = jnp.einsum('bshe,ke->bshk', q[..., 1, :], sub_key_2).astype(jnp.float32)
    v1, i1 = lax.top_k(s1, PEER_TOPK)
    v2, i2 = lax.top_k(s2, PEER_TOPK)
    cand = (v1[..., :, None] + v2[..., None, :]).reshape(B, S, PEER_HEADS, PEER_TOPK * PEER_TOPK)
    vals, ci = lax.top_k(cand, PEER_TOPK)
    idx = (jnp.take_along_axis(i1, ci // PEER_TOPK, -1) * PEER_N_KEYS
           + jnp.take_along_axis(i2, ci % PEER_TOPK, -1))
    g = jax.nn.softmax(vals, axis=-1)
    nc = (B * S) // PEER_CHUNK
    kk = PEER_HEADS * PEER_TOPK
    hc = h.reshape(nc, PEER_CHUNK, D)
    idc = idx.reshape(nc, PEER_CHUNK, kk)
    gc = g.reshape(nc, PEER_CHUNK, kk).astype(h.dtype)

    def chunk(args):
        ht, it, gt = args
        u = u_table[it]
        a = jnp.einsum('td,tkd->tk', ht, u)
        w = jax.nn.gelu(a.astype(jnp.float32), approximate=False).astype(ht.dtype) * gt
        return jnp.einsum('tk,tkd->td', w, v_table[it])

    return lax.map(chunk, (hc, idc, gc)).reshape(B, S, D)


def setup_inputs(seed: int = 0) -> dict:
    key = jax.random.key(seed)
    ks = jax.random.split(key, 24)

    def nrm(k, shape, s):
        return jax.random.normal(k, shape, jnp.float32) * s

    col_scale = jnp.ones((IN_COLS,), jnp.float32).at[2 * A_WIDTH:3 * A_WIDTH].set(DN_BETA)
    return {
        'x': nrm(ks[0], (BATCH, SEQ, D_MODEL), 1.0),
        'c': nrm(ks[1], (BATCH, D_MODEL), 1.0),
        'w_ada': nrm(ks[2], (DEPTH, D_MODEL, 6 * D_MODEL), 0.5 * D_MODEL ** -0.5),
        'b_ada': nrm(ks[3], (DEPTH, 6 * D_MODEL), 0.02),
        'w_in': nrm(ks[4], (DEPTH, D_MODEL, IN_COLS), D_MODEL ** -0.5) * col_scale,
        'g_q_lat': 1.0 + nrm(ks[5], (DEPTH, MLA_Q_RANK), 0.02),
        'g_kv_lat': 1.0 + nrm(ks[6], (DEPTH, MLA_KV_RANK), 0.02),
        'w_uq': nrm(ks[7], (DEPTH, MLA_Q_RANK, B_HEADS * (MLA_NOPE + MLA_ROPE)), MLA_Q_RANK ** -0.5),
        'w_uk': nrm(ks[8], (DEPTH, MLA_KV_RANK, B_HEADS * MLA_NOPE), MLA_KV_RANK ** -0.5),
        'w_uv': nrm(ks[9], (DEPTH, MLA_KV_RANK, B_HEADS * MLA_V), MLA_KV_RANK ** -0.5 * DN_BETA),
        'g_out_a': 1.0 + nrm(ks[10], (DEPTH, A_WIDTH), 0.02),
        'g_out_b': 1.0 + nrm(ks[11], (DEPTH, B_WIDTH), 0.02),
        'w_o': nrm(ks[12], (DEPTH, MIX_WIDTH, D_MODEL), MIX_WIDTH ** -0.5 * DN_BETA),
        'ln1_g': 1.0 + nrm(ks[13], (DEPTH, D_MODEL), 0.02),
        'ln1_b': nrm(ks[14], (DEPTH, D_MODEL), 0.02),
        'w_pq': nrm(ks[15], (DEPTH, D_MODEL, PEER_HEADS * PEER_KEY_DIM), D_MODEL ** -0.5),
        'sub_key_1': nrm(ks[16], (DEPTH, PEER_N_KEYS, PEER_KEY_DIM // 2), (PEER_KEY_DIM // 2) ** -0.5),
        'sub_key_2': nrm(ks[17], (DEPTH, PEER_N_KEYS, PEER_KEY_DIM // 2), (PEER_KEY_DIM // 2) ** -0.5),
        'u_table': nrm(ks[18], (DEPTH, PEER_N_EXPERTS, D_MODEL), D_MODEL ** -0.5),
        'v_table': nrm(ks[19], (DEPTH, PEER_N_EXPERTS, D_MODEL), DN_BETA),
        'ln2_g': 1.0 + nrm(ks[20], (DEPTH, D_MODEL), 0.02),
        'ln2_b': nrm(ks[21], (DEPTH, D_MODEL), 0.02),
    }


def reference(x, c, w_ada, b_ada, w_in, g_q_lat, g_kv_lat, w_uq, w_uk, w_uv, g_out_a, g_out_b,
              w_o, ln1_g, ln1_b, w_pq, sub_key_1, sub_key_2, u_table, v_table, ln2_g, ln2_b):
    S = x.shape[1]
    pos = jnp.arange(S, dtype=jnp.float32)
    c_act = jax.nn.silu(c)
    for l in range(DEPTH):
        mod = jnp.dot(c_act, w_ada[l]) + b_ada[l]
        sh1, sc1, g1, sh2, sc2, g2 = [m[:, None, :] for m in jnp.split(mod, 6, axis=-1)]
        h = layer_norm(x) * (1.0 + sc1) + sh1
        mix = token_mix(h, pos, w_in[l], g_q_lat[l], g_kv_lat[l], w_uq[l], w_uk[l], w_uv[l],
                        g_out_a[l], g_out_b[l], w_o[l])
        x = layer_norm(DN_ALPHA * x + g1 * mix, ln1_g[l], ln1_b[l])
        h = layer_norm(x) * (1.0 + sc2) + sh2
        ffn = peer(h, w_pq[l], sub_key_1[l], sub_key_2[l], u_table[l], v_table[l])
        x = layer_norm(DN_ALPHA * x + g2 * ffn, ln2_g[l], ln2_b[l])
    return x
```

```python
import numpy as np
from contextlib import ExitStack
import concourse.bass as bass
import concourse.mybir as mybir
from concourse.bass_utils import run_bass_kernel_spmd

F32 = mybir.dt.float32; BF16 = mybir.dt.bfloat16; I32 = mybir.dt.int32; U32 = mybir.dt.uint32
ALU = mybir.AluOpType; AF = mybir.ActivationFunctionType; AX = mybir.AxisListType
SAME_SYNC = True
NCORES = 8
S_ = 2048; D_ = 2048
ALPHA = 2.0 ** 0.25
NEG = -1.0e30


class Buf:
    __slots__ = ('name', 'w', 'r', 'sem', 'semcnt', 'excl')

    def __init__(self, name, excl=False):
        self.name = name; self.w = {}; self.r = {}; self.sem = None; self.semcnt = 0; self.excl = excl


class Sched:
    ENG = ('pe', 'act', 'dve', 'pool', 'sp')

    def __init__(self, nc, ctx):
        self.nc = nc; self.ctx = ctx
        self.ops = {e: [] for e in self.ENG}
        self.esem = {e: ctx.enter_context(nc.semaphore('sem_' + e)) for e in self.ENG}
        self.dbufs = []; self.dset = set()
        self.uid = 0

    def op(self, eng, fn, reads=(), writes=(), dma=None, ndma=1):
        ops = self.ops[eng]
        idx = len(ops)
        deps = []
        for b in reads:
            deps.extend(b.w.values())
            if b.excl:
                deps.extend(v for kk, v in b.r.items() if kk != eng)
        for b in writes:
            deps.extend(b.w.values()); deps.extend(b.r.values())
        waits = set()
        for ev in deps:
            if ev[0] == 'E':
                e2 = ev[1]
                if e2 == eng and dma is None and (eng == 'pe' or not SAME_SYNC):
                    continue
                self.ops[e2][ev[2]]['inc'] = True
            waits.add(ev)
        if dma is not None:
            if dma.sem is None:
                dma.sem = self.ctx.enter_context(self.nc.semaphore('ds%d' % len(self.dbufs)))
            if id(dma) not in self.dset:
                self.dset.add(id(dma)); self.dbufs.append(dma)
            dma.semcnt += 16 * ndma
            ev = ('D', dma, dma.semcnt)
            self.uid += 1
            key = ('dma', self.uid)
        else:
            ev = ('E', eng, idx)
            key = eng
        ops.append(dict(fn=fn, waits=waits, inc=False, dma=dma))
        for b in reads:
            b.r[key] = ev
        for b in writes:
            b.w = {key: ev}; b.r = {}
        return ev

    def barrier(self):
        last = {}
        for e in self.ENG:
            for i in range(len(self.ops[e]) - 1, -1, -1):
                if self.ops[e][i]['fn'] is not None and self.ops[e][i]['dma'] is None:
                    last[e] = ('E', e, i); self.ops[e][i]['inc'] = True
                    break
        dmaev = [('D', b, b.semcnt) for b in self.dbufs if b.semcnt > 0]
        for e in self.ENG:
            waits = set(v for k, v in last.items() if (k != e or e != 'pe')) | set(dmaev)
            self.ops[e].append(dict(fn=None, waits=waits, inc=False, dma=None))

    def emit(self):
        nc = self.nc
        seq = {}
        for e in self.ENG:
            c = 0
            for i, o in enumerate(self.ops[e]):
                if o['inc']:
                    c += 1; seq[(e, i)] = c

        def run(eng, e):
            seen = {}
            for i, o in enumerate(self.ops[e]):
                for ev in o['waits']:
                    if ev[0] == 'E':
                        sem = self.esem[ev[1]]; val = seq[(ev[1], ev[2])]; key = ev[1]
                    else:
                        sem = ev[1].sem; val = ev[2]; key = id(ev[1])
                    if seen.get(key, 0) >= val:
                        continue
                    eng.wait_ge(sem, val); seen[key] = val
                if o['fn'] is None:
                    continue
                r = o['fn'](eng)
                if o['dma'] is not None:
                    for ins in (r if isinstance(r, (list, tuple)) else [r]):
                        ins.then_inc(o['dma'].sem, 16)
                elif o['inc']:
                    r.then_inc(self.esem[e], 1)

        with nc.Block() as block:
            block.sync(lambda eng: run(eng, 'sp'))
            block.scalar(lambda eng: run(eng, 'act'))
            block.vector(lambda eng: run(eng, 'dve'))
            block.gpsimd(lambda eng: run(eng, 'pool'))
            block.tensor(lambda eng: run(eng, 'pe'))


class Arena:
    def __init__(self, nc, ctx, nwords):
        self.t = ctx.enter_context(nc.sbuf_tensor('arena', [128, nwords], F32))
        self.n = nwords; self.top = 0

    def alloc(self, shape, dtype=F32):
        n = int(np.prod(shape))
        per = 2 if dtype == BF16 else 1
        words = (n + per - 1) // per
        words = (words + 1) // 2 * 2
        assert self.top + words <= self.n, ('arena OOM', self.top, words, self.n)
        ap = self.t[:, self.top:self.top + words]
        self.top += words
        if dtype != F32:
            ap = ap.bitcast(dtype)
        if dtype == BF16 and n != words * 2:
            ap = ap[:, 0:n]
        if len(shape) > 1:
            names = ' '.join('d%d' % i for i in range(len(shape)))
            ap = ap.rearrange('p (%s) -> p %s' % (names, names), **{'d%d' % i: s for i, s in enumerate(shape)})
        return ap


class K:
    def __init__(self, S):
        self.S = S

    def mm(self, out, lhsT, rhs, start, stop, R, W):
        self.S.op('pe', lambda e: e.matmul(out, lhsT=lhsT, rhs=rhs, start=start, stop=stop), R, W)

    def tr(self, out, in_, ident, R, W):
        self.S.op('pe', lambda e: e.transpose(out=out, in_=in_, identity=ident), R, W)

    def act(self, out, in_, func, R, W, scale=1.0, bias=0.0, accum_out=None):
        if accum_out is None:
            self.S.op('act', lambda e: e.activation(out=out, in_=in_, func=func, bias=bias, scale=scale), R, W)
        else:
            self.S.op('act', lambda e: e.activation(out=out, in_=in_, func=func, bias=bias, scale=scale,
                                                    accum_out=accum_out), R, W)

    def copy(self, eng, out, in_, R, W):
        if eng == 'act':
            self.S.op('act', lambda e: e.activation(out=out, in_=in_, func=AF.Copy), R, W)
        else:
            self.S.op(eng, lambda e: e.tensor_copy(out=out, in_=in_), R, W)

    def tt(self, eng, out, in0, in1, op, R, W):
        self.S.op(eng, lambda e: e.tensor_tensor(out=out, in0=in0, in1=in1, op=op), R, W)

    def ts(self, eng, out, in0, s1, s2, op0, op1, R, W, accum_out=None):
        if op1 is None:
            self.S.op(eng, lambda e: e.tensor_scalar(out=out, in0=in0, scalar1=s1, scalar2=None, op0=op0), R, W)
        elif accum_out is None:
            self.S.op(eng, lambda e: e.tensor_scalar(out=out, in0=in0, scalar1=s1, scalar2=s2, op0=op0, op1=op1), R, W)
        else:
            self.S.op(eng, lambda e: e.tensor_scalar(out=out, in0=in0, scalar1=s1, scalar2=s2, op0=op0, op1=op1,
                                                     accum_out=accum_out), R, W)

    def stt(self, out, in0, scalar, in1, op0, op1, R, W, accum_out=None):
        if accum_out is None:
            self.S.op('dve', lambda e: e.scalar_tensor_tensor(out=out, in0=in0, scalar=scalar, in1=in1, op0=op0, op1=op1), R, W)
        else:
            self.S.op('dve', lambda e: e.scalar_tensor_tensor(out=out, in0=in0, scalar=scalar, in1=in1, op0=op0, op1=op1,
                                                              accum_out=accum_out), R, W)

    def recip(self, out, in_, R, W):
        self.S.op('dve', lambda e: e.reciprocal(out=out, in_=in_), R, W)

    def memset(self, eng, ap, val, R, W):
        self.S.op(eng, lambda e: e.memset(ap, val), R, W)

    def dma(self, eng, out, in_, R, W, sem, slow=False):
        if slow:
            self.S.op(eng, lambda e: e.dma_start(out=out, in_=in_, allow_slow_non_contiguous=True), R, W, dma=sem)
        else:
            self.S.op(eng, lambda e: e.dma_start(out=out, in_=in_), R, W, dma=sem)

    def gather(self, out, table, idx_ap, R, W, sem):
        self.S.op('pool', lambda e: e.indirect_dma_start(
            out=out, out_offset=None, in_=table,
            in_offset=bass.IndirectOffsetOnAxis(ap=idx_ap, axis=0)), R, W, dma=sem)


class StopBuild(Exception):
    def __init__(self, items):
        self.items = items


def bc(ap, shape, axis):
    return ap.unsqueeze(axis).to_broadcast(shape)


def build(nseq=2, stop=None, peer_tiles=16, ntab=16384):
    nc = bass.Bass("TRN2", target_bir_lowering=False)

    def DI(name, shape, dt=F32):
        return nc.dram_tensor(name, shape, dt, kind="ExternalInput").ap()

    x = DI('x', [nseq, S_, D_])
    cT = DI('cT', [128, 16, 2])
    w_ada = DI('w_ada_r', [24, 128, 16, 512])
    b_adaT = DI('b_adaT', [128, 96])
    b_ada_row = DI('b_ada_row', [1, 12288])
    w_in = DI('w_in_r', [8, 128, 16, 512])
    w_in_kr = DI('w_in_kr', [128, 16, 64])
    g_qT = DI('g_qT', [128, 4]); g_kvT = DI('g_kvT', [128, 4])
    w_uqn = DI('w_uqn', [128, 4, 8, 128]); w_uqr = DI('w_uqr', [128, 4, 8, 64])
    w_uk = DI('w_uk_r', [128, 4, 1024]); w_uv = DI('w_uv_r', [128, 4, 1024])
    g_oaT = DI('g_oaT', [128, 8]); g_obT = DI('g_obT', [128, 8])
    w_o = DI('w_o_r', [128, 16, 2048])
    ln1_g = DI('ln1_g', [1, D_]); ln1_b = DI('ln1_b', [1, D_]); ln2_g = DI('ln2_g', [1, D_]); ln2_b = DI('ln2_b', [1, D_])
    w_pq = DI('w_pq_r', [16, 128, 16, 128])
    skT = DI('skT', [128, 2, 128])
    u_table = DI('u_table', [ntab, D_]); v_table = DI('v_table', [ntab, D_])
    ident_d = DI('ident', [128, 128])
    maskA_d = DI('maskA', [128, 31, 128]); maskC_d = DI('maskC', [128, 128])
    cosA_d = DI('cosA', [128, 16, 16]); sinA_d = DI('sinA', [128, 16, 16])
    cosB_d = DI('cosB', [128, 16, 32]); sinB_d = DI('sinB', [128, 16, 32])
    iota_d = DI('iota', [128, 256])
    out = nc.dram_tensor('out', [nseq, S_, D_], F32, kind="ExternalOutput").ap()
    dbgk = "ExternalOutput" if stop else "Internal"
    modrow = nc.dram_tensor('modrow', [nseq, 4, D_], F32, kind="Internal").ap()
    ssqscr = nc.dram_tensor('ssqscr', [nseq, S_], F32, kind="Internal").ap()
    x1scr = nc.dram_tensor('x1scr', [nseq, S_, D_], F32, kind=dbgk).ap()
    uvb = nc.dram_tensor('uvb', [ntab, 2 * D_], BF16, kind="Internal").ap()
    dbg = nc.dram_tensor('dbg', [128, 16, 2048], F32, kind=dbgk).ap() if stop else None

    ctx = ExitStack()
    with ctx:
        S = Sched(nc, ctx)
        k = K(S)
        A = Arena(nc, ctx, 53000)
        A.n = 53000 if not stop else 53000 - 0
        pf = [ctx.enter_context(nc.psum_tensor('pf%d' % i, [128, 512], F32)) for i in range(6)]
        pbf = [ctx.enter_context(nc.psum_tensor('pb%d' % i, [128, 512], F32)) for i in range(2)]
        pb = [t[:, :].bitcast(BF16) for t in pbf]
        pfB = [Buf('pf%d' % i, True) for i in range(6)]
        pbB = [Buf('pb%d' % i, True) for i in range(2)]
        nb_ = [0]

        bcnt = {}; bprev = {}

        def B(name='b'):
            nb_[0] += 1
            bcnt[name] = bcnt.get(name, 0) + 1
            key = (name, bcnt[name])
            nb = Buf('%s%d' % (name, nb_[0]))
            if key in bprev:
                nb.sem = bprev[key].sem; nb.semcnt = bprev[key].semcnt
            bprev[key] = nb
            return nb

        ident_f = A.alloc([128]); ident_b = A.alloc([128], BF16)
        maskA = A.alloc([31, 128], BF16); maskC = A.alloc([128], BF16)
        cosA = A.alloc([16, 16]); sinA = A.alloc([16, 16]); cosB = A.alloc([16, 32]); sinB = A.alloc([16, 32])
        sh1T = A.alloc([16, 2]); sc1T = A.alloc([16, 2])
        gq = A.alloc([4]); gkv = A.alloc([4]); goa = A.alloc([8]); gob = A.alloc([8])
        skT_s = A.alloc([2, 128])
        iota = A.alloc([256])
        constB = B('const')
        def cdma(eng, dst, src):
            cb = B('c'); k.dma(eng, dst, src, [], [cb], cb)
        cdma('sp', ident_f, ident_d)
        cdma('pool', ident_b, ident_d)
        cdma('pool', maskA, maskA_d)
        cdma('pool', maskC, maskC_d)
        for dst, src in ((cosA, cosA_d), (sinA, sinA_d), (cosB, cosB_d), (sinB, sinB_d), (gq, g_qT), (gkv, g_kvT),
                         (goa, g_oaT), (gob, g_obT), (skT_s, skT), (iota, iota_d)):
            cdma('sp', dst, src)
        P_END = A.top

        cact = A.alloc([16, 2]); badaT = A.alloc([96]); brow = A.alloc([12288]); rowt = [A.alloc([512]) for _ in range(2)]
        wblk0 = [A.alloc([16, 512]) for _ in range(2)]
        s0B = B('s0'); wB0 = [B('wada') for _ in range(2)]; rowB = [B('rowt') for _ in range(2)]; modB = B('modT'); mrB = B('modrow')
        s0b2 = B('s0b'); s0b3 = B('s0c')
        k.dma('sp', cact, cT, [], [s0B], s0B)
        k.dma('sp', badaT, b_adaT, [], [s0b2], s0b2)
        k.dma('sp', brow[0:1, :], b_ada_row, [], [s0b3], s0b3)
        k.act(cact, cact, AF.Silu, [s0B], [s0B])
        CF = [A.alloc([4096]) for _ in range(3)]; CFB = [B('cf') for _ in range(3)]
        CB = [A.alloc([4096], BF16) for _ in range(3)]; CBB = [B('cb') for _ in range(3)]; CSB = [B('cs') for _ in range(3)]
        tabB = B('tab')
        ci_ = 0
        for src_t, off_t in ((u_table, 0), (v_table, D_)):
            for ci in range(ntab // 256):
                cf = CF[ci_ % 3]; cfB = CFB[ci_ % 3]; cb = CB[ci_ % 3]; cbB = CBB[ci_ % 3]; csB = CSB[ci_ % 3]
                k.dma('sp', cf, src_t[ci * 256:(ci + 1) * 256, :].rearrange('(p a) d -> p (a d)', a=2), [], [cfB], cfB)
                k.copy('dve' if ci_ % 2 == 0 else 'pool', cb, cf, [cfB], [cbB])
                k.dma('act', uvb[ci * 256:(ci + 1) * 256, off_t:off_t + D_].rearrange('(p a) d -> p a d', a=2),
                      cb.rearrange('p (a d) -> p a d', a=2), [cbB], [tabB], csB)
                ci_ += 1
        ri = 0
        for blk in range(24):
            wb = wblk0[blk % 2]; wbB = wB0[blk % 2]
            k.dma('sp', wb, w_ada[blk], [], [wbB], wbB)
            if blk < 8:
                for j in range(4):
                    ch = (blk % 4) * 4 + j
                    ps = pf[j % 2][:, 0:2]
                    for kc in range(16):
                        k.mm(ps, wb[:, kc, j * 128:(j + 1) * 128], cact[:, kc, :], kc == 0, kc == 15, [wbB, s0B], [pfB[j % 2]])
                    if blk < 4:
                        k.ts('dve', sh1T[:, ch, :], ps, badaT[:, blk * 4 + j:blk * 4 + j + 1], None, ALU.add, None, [pfB[j % 2], s0b2], [modB])
                    else:
                        k.ts('dve', sc1T[:, ch, :], ps, badaT[:, blk * 4 + j:blk * 4 + j + 1], 1.0, ALU.add, ALU.add, [pfB[j % 2], s0b2], [modB])
            else:
                slot = (blk - 8) // 4; q4 = (blk - 8) % 4
                for b in range(nseq):
                    pi = 2 + (ri % 2); ps = pf[pi][0:1, :]
                    for kc in range(16):
                        k.mm(ps, cact[:, kc, b:b + 1], wb[:, kc, :], kc == 0, kc == 15, [wbB, s0B], [pfB[pi]])
                    rt = rowt[ri % 2]; rB = rowB[ri % 2]
                    k.tt('dve', rt[0:1, :], ps, brow[0:1, blk * 512:(blk + 1) * 512], ALU.add, [pfB[pi], s0b3], [rB])
                    if slot == 2:
                        k.ts('dve', rt[0:1, :], rt[0:1, :], 1.0, None, ALU.add, None, [rB], [rB])
                    k.dma('sp', modrow[b, slot:slot + 1, q4 * 512:(q4 + 1) * 512], rt[0:1, :], [rB], [mrB], rB)
                    ri += 1
        S.barrier()
        A.top = P_END

        oTa = A.alloc([8, 2048], BF16)
        ssqA = A.alloc([16, 8]); ssqAB = B('ssqA')
        ssqB_ = A.alloc([16, 8]); ssqBB = B('ssqB')
        rab = A.alloc([2, 16]); rabB = B('rab')
        R1 = A.top
        hT = A.alloc([16, 2048], BF16)
        Z0 = A.top
        ZEND = A.n
        hTB = B('hT'); oTaB = B('oTa'); oTbB = B('oTb')

        try:
          bsnap = dict(bcnt)
          for b in range(nseq):
              bcnt.clear(); bcnt.update(bsnap)
              A.top = Z0
              XT = [A.alloc([2048]) for _ in range(2)]; XN = [A.alloc([2048], BF16) for _ in range(2)]
              STt = [A.alloc([4, 6]) for _ in range(2)]; MV = [A.alloc([8]) for _ in range(2)]
              XTB = [B('xt') for _ in range(2)]; XNB = [B('xn') for _ in range(2)]; STB = [B('st') for _ in range(2)]
              for tt in range(16):
                  xt = XT[tt % 2]; xn = XN[tt % 2]; st = STt[tt % 2]; mv = MV[tt % 2]
                  xtB = XTB[tt % 2]; xnB = XNB[tt % 2]; stB = STB[tt % 2]
                  k.dma('sp', xt, x[b, tt * 128:(tt + 1) * 128, :], [], [xtB], xtB)
                  for c4 in range(4):
                      S.op('dve', (lambda o, i: (lambda e: e.bn_stats(out=o, in_=i)))(st[:, c4, :], xt[:, c4 * 512:(c4 + 1) * 512]), [xtB], [stB])
                  S.op('dve', (lambda o, i: (lambda e: e.bn_aggr(out=o, in_=i)))(mv[:, 0:2], st.rearrange('p a b -> p (a b)')), [stB], [stB])
                  k.act(mv[:, 2:3], mv[:, 1:2], AF.Sqrt, [stB], [stB], bias=1e-5)
                  k.recip(mv[:, 3:4], mv[:, 2:3], [stB], [stB])
                  k.ts('dve', mv[:, 4:5], mv[:, 0:1], mv[:, 3:4], -1.0, ALU.mult, ALU.mult, [stB], [stB])
                  k.act(xn, xt, AF.Identity, [xtB, stB], [xnB], scale=mv[:, 3:4], bias=mv[:, 4:5])
                  for g4 in range(4):
                      pbi = g4 % 2
                      for j in range(4):
                          kc = g4 * 4 + j
                          k.tr(pb[pbi][:, j * 128:(j + 1) * 128], xn[:, kc * 128:(kc + 1) * 128], ident_b, [xnB, constB], [pbB[pbi]])
                      for j in range(4):
                          kc = g4 * 4 + j
                          o = hT[:, kc, tt * 128:(tt + 1) * 128]; i = pb[pbi][:, j * 128:(j + 1) * 128]
                          if pbi == 0:
                              k.act(o, i, AF.Identity, [pbB[pbi], modB], [hTB], scale=sc1T[:, kc, b:b + 1], bias=sh1T[:, kc, b:b + 1])
                          else:
                              k.ts('dve', o, i, sc1T[:, kc, b:b + 1], sh1T[:, kc, b:b + 1], ALU.mult, ALU.add, [pbB[pbi], modB], [hTB])
              if stop == 'S1':
                  S.barrier()
                  dt_ = A.alloc([2048]); dB = B('dbg')
                  for kc in range(16):
                      k.copy('dve', dt_, hT[:, kc, :], [hTB], [dB])
                      k.dma('sp', dbg[:, kc, :], dt_, [dB], [], dB)
                  break
              S.barrier()

              A.top = Z0
              QTM = [A.alloc([4, 128], BF16) for _ in range(2)]; QTMB = [B('qtm') for _ in range(2)]
              RT = [A.alloc([4, 4, 16]) for _ in range(2)]; RTB = [B('rt') for _ in range(2)]
              PE_ = [A.alloc([512], BF16) for _ in range(3)]; PEB = [B('pexp') for _ in range(3)]
              PM = [A.alloc([512], BF16) for _ in range(3)]; PMB = [B('pm') for _ in range(3)]
              OBF = [A.alloc([128], BF16) for _ in range(2)]; OBFB = [B('obf') for _ in range(2)]
              RL = [A.alloc([2]) for _ in range(2)]; RLB = [B('rl') for _ in range(2)]
              JK = A.alloc([512], BF16); JKB = B('junk')
              SMALL_END = A.top
              WB = [A.alloc([16, 512], BF16) for _ in range(2)]; WBB = [B('wblk') for _ in range(2)]
              QT = A.alloc([4, 2048], BF16); KT = A.alloc([4, 2048], BF16); V = A.alloc([16, 4, 130], BF16)
              QTB = B('QT'); KTB = B('KT'); VB = B('V')
              wi = 0
              it_ = 0
              for g in range(2):
                  k.memset('pool', V[:, :, :, 128:130], 1.0, [], [VB])
                  for typ in range(3):
                      blk = typ * 2 + g
                      wb = WB[wi % 2]; wbB = WBB[wi % 2]; wi += 1
                      k.dma('pool', wb, w_in[blk], [], [wbB], wbB)
                      if stop == 'B1':
                          raise StopBuild([(wb[:, 0, :], 512), (V[:, 0, :, :].rearrange('p a b -> p (a b)'), 520)])
                      for r in range(16):
                          pi = r % 2; ps = pf[pi]
                          for kc in range(16):
                              k.mm(ps[:, :], hT[:, kc, r:2048:16], wb[:, kc, :], kc == 0, kc == 15, [hTB, wbB], [pfB[pi]])
                          ps3 = ps[:, :].rearrange('p (h e) -> p h e', h=4)
                          if typ == 2:
                              k.copy('act' if r % 2 == 0 else 'dve', V[:, r, :, 0:128], ps3, [pfB[pi]], [VB])
                              continue
                          qi = it_ % 2; it_ += 1
                          qtm = QTM[qi]; qB = QTMB[qi]; rt = RT[qi]; rB = RTB[qi]
                          k.copy('act', qtm[:, :, 32:128], ps3[:, :, 32:128], [pfB[pi]], [qB])
                          if stop == 'B2a':
                              raise StopBuild([(qtm[:, 0, :], 128)])
                          cs = bc(cosA[:, r, :], [128, 4, 16], 1); sn = bc(sinA[:, r, :], [128, 4, 16], 1)
                          x1_ = ps3[:, :, 0:16]; x2_ = ps3[:, :, 16:32]
                          k.tt('dve', rt[:, 0], x1_, cs, ALU.mult, [pfB[pi], constB], [rB])
                          k.tt('dve', rt[:, 1], x2_, sn, ALU.mult, [pfB[pi], constB], [rB])
                          k.tt('dve', rt[:, 2], x2_, cs, ALU.mult, [pfB[pi], constB], [rB])
                          k.tt('dve', rt[:, 3], x1_, sn, ALU.mult, [pfB[pi], constB], [rB])
                          k.tt('dve', qtm[:, :, 0:16], rt[:, 0], rt[:, 1], ALU.subtract, [rB], [qB])
                          k.tt('dve', qtm[:, :, 16:32], rt[:, 2], rt[:, 3], ALU.add, [rB], [qB])
                          if stop == 'B2b':
                              raise StopBuild([(qtm[:, 0, :], 128), (rt[:, 0].rearrange('p a b -> p (a b)'), 64)])
                          pbi = qi
                          for hl in range(4):
                              k.tr(pb[pbi][:, hl * 128:(hl + 1) * 128], qtm[:, hl, :], ident_b, [qB, constB], [pbB[pbi]])
                          dst = (QT if typ == 0 else KT)[:, :, r * 128:(r + 1) * 128]
                          k.copy('act' if r % 2 == 1 else 'dve', dst, pb[pbi][:, 0:512].rearrange('p (h e) -> p h e', h=4),
                                 [pbB[pbi]], [QTB if typ == 0 else KTB])
                          if stop == 'B2':
                              raise StopBuild([(QT[:, 0, 0:128], 128), (qtm[:, 0, :], 128), (rt[:, 0].rearrange('p a b -> p (a b)'), 64)])
                  if stop == 'B3':
                      raise StopBuild([(QT[:, 0, :], 2048), (KT[:, 0, :], 2048), (V[:, 0:3, :, :].rearrange('p a b c -> p (a b c)'), 1560)])
                  batches = [(hl, c, rp) for hl in range(4) for c in range(4) for rp in range(16)]
                  LA = 2
                  SB = [(pf[0], pfB[0]), (pf[1], pfB[1]), (pbf[1], pbB[1])]

                  def qkA(n):
                      hl, c, rp = batches[n]
                      sps, spB = SB[n % 3]
                      k.mm(sps[:, :], KT[:, hl, rp * 128:(rp + 1) * 128], QT[:, hl, c * 512:(c + 1) * 512], True, True, [KTB, QTB], [spB])
                      pe_ = PE_[n % 3]; peB = PEB[n % 3]; pm = PM[n % 3]; pmB = PMB[n % 3]
                      k.act(pe_, sps[:, :], AF.Exp, [spB], [peB], scale=128.0 ** -0.5)
                      n0 = 15 - rp + 4 * c
                      k.tt('pool' if n % 2 else 'dve', pm, pe_, maskA[:, n0:n0 + 4, :].rearrange('p a b -> p (a b)'), ALU.mult, [peB, constB], [pmB])

                  def pvA(n):
                      hl, c, rp = batches[n]
                      h = g * 4 + hl
                      pm = PM[n % 3]; pmB = PMB[n % 3]
                      for j in range(4):
                          k.mm(pf[2 + j][:, 0:130], pm[:, j * 128:(j + 1) * 128], V[:, rp, hl, :], rp == 0, rp == 15, [pmB, VB], [pfB[2 + j]])
                      if rp == 15:
                          for j in range(4):
                              r = 4 * c + j; fi = j % 2; ops_ = pf[2 + j]; opB = pfB[2 + j]
                              k.recip(RL[fi][:, 0:1], ops_[:, 128:129], [opB], [RLB[fi]])
                              k.ts('dve', OBF[fi], ops_[:, 0:128], RL[fi][:, 0:1], None, ALU.mult, None, [opB, RLB[fi]], [OBFB[fi]])
                              k.act(JK[:, 0:128], ops_[:, 0:128], AF.Square, [opB, RLB[fi]], [JKB, ssqAB], scale=RL[fi][:, 0:1], accum_out=ssqA[:, r, h:h + 1])
                              k.tr(pb[0][:, fi * 128:(fi + 1) * 128], OBF[fi], ident_b, [OBFB[fi], constB], [pbB[0]])
                              k.act(oTa[:, h, r:2048:16], pb[0][:, fi * 128:(fi + 1) * 128], AF.Identity, [pbB[0], constB], [oTaB], scale=goa[:, h:h + 1])

                  for n in range(len(batches) + LA):
                      if n < len(batches):
                          qkA(n)
                      if n - LA >= 0:
                          pvA(n - LA)
              if stop == 'S3':
                  S.barrier()
                  A.top = SMALL_END
                  dt_ = A.alloc([2048]); dB = B('dbg')
                  for kc in range(8):
                      k.copy('dve', dt_, oTa[:, kc, :], [oTaB], [dB])
                      k.dma('sp', dbg[:, kc, :], dt_, [dB], [], dB)
                  k.copy('dve', dt_[:, 0:128], ssqA.rearrange('p a b -> p (a b)'), [ssqAB], [dB])
                  k.dma('sp', dbg[:, 8, 0:128], dt_[:, 0:128], [dB], [], dB)
                  break
              S.barrier()

              A.top = SMALL_END
              CQ0 = ZEND - 9216
              WB = [A.alloc([16, 512], BF16) for _ in range(2)]; WBB = [B('wblk') for _ in range(2)]
              WKR = A.alloc([16, 64], BF16); WKRB = B('wkr')
              CTM = [A.alloc([512], BF16) for _ in range(2)]; CTMB = [B('ctm') for _ in range(2)]
              SQ = [A.alloc([4]) for _ in range(2)]; SQB = [B('sq') for _ in range(2)]
              KRT = [A.alloc([4, 32]) for _ in range(2)]; KRTB = [B('krt') for _ in range(2)]
              KRM = [A.alloc([64], BF16) for _ in range(2)]; KRMB = [B('krm') for _ in range(2)]
              assert A.top <= CQ0
              save = A.top
              A.top = CQ0
              cqnT = A.alloc([4, 2048], BF16); ckvnT = A.alloc([4, 2048], BF16); krT = A.alloc([2048], BF16)
              cqB = B('cqnT'); ckvB = B('ckvnT'); krB = B('krT')
              A.top = save
              k.memset('pool', krT[64:128, :], 0.0, [], [krB])
              k.dma('pool', WB[0], w_in[6], [], [WBB[0]], WBB[0])
              k.dma('pool', WB[1], w_in[7], [], [WBB[1]], WBB[1])
              k.dma('pool', WKR, w_in_kr, [], [WKRB], WKRB)
              it_ = 0
              for typ in range(2):
                  wb = WB[typ]; wbB = WBB[typ]
                  dstT = cqnT if typ == 0 else ckvnT; dstB = cqB if typ == 0 else ckvB; gsc = gq if typ == 0 else gkv
                  for tt in range(16):
                      pi = tt % 2; ps = pf[pi]
                      for kc in range(16):
                          k.mm(ps[:, :], hT[:, kc, tt * 128:(tt + 1) * 128], wb[:, kc, :], kc == 0, kc == 15, [hTB, wbB], [pfB[pi]])
                      qi = it_ % 2; it_ += 1
                      sq = SQ[qi]; sqB = SQB[qi]; ctm = CTM[qi]; ctB = CTMB[qi]
                      k.act(JK, ps[:, :], AF.Square, [pfB[pi]], [JKB, sqB], accum_out=sq[:, 0:1])
                      k.act(sq[:, 1:2], sq[:, 0:1], AF.Sqrt, [sqB], [sqB], scale=1.0 / 512, bias=1e-6)
                      k.recip(sq[:, 2:3], sq[:, 1:2], [sqB], [sqB])
                      k.ts('dve', ctm, ps[:, :], sq[:, 2:3], None, ALU.mult, None, [pfB[pi], sqB], [ctB])
                      for j in range(4):
                          k.tr(pb[qi][:, j * 128:(j + 1) * 128], ctm[:, j * 128:(j + 1) * 128], ident_b, [ctB, constB], [pbB[qi]])
                      k.tt('dve', dstT[:, :, tt * 128:(tt + 1) * 128], pb[qi][:, 0:512].rearrange('p (a b) -> p a b', a=4),
                           bc(gsc, [128, 4, 128], 2), ALU.mult, [pbB[qi], constB], [dstB])
              for tt in range(16):
                  pi = 2 + tt % 2; ps = pf[pi]
                  for kc in range(16):
                      k.mm(ps[:, 0:64], hT[:, kc, tt * 128:(tt + 1) * 128], WKR[:, kc, :], kc == 0, kc == 15, [hTB, WKRB], [pfB[pi]])
                  qi = tt % 2; rt = KRT[qi]; rB = KRTB[qi]; km = KRM[qi]; kmB = KRMB[qi]
                  cs = cosB[:, tt, :]; sn = sinB[:, tt, :]
                  k.tt('dve', rt[:, 0], ps[:, 0:32], cs, ALU.mult, [pfB[pi], constB], [rB])
                  k.tt('dve', rt[:, 1], ps[:, 32:64], sn, ALU.mult, [pfB[pi], constB], [rB])
                  k.tt('dve', rt[:, 2], ps[:, 32:64], cs, ALU.mult, [pfB[pi], constB], [rB])
                  k.tt('dve', rt[:, 3], ps[:, 0:32], sn, ALU.mult, [pfB[pi], constB], [rB])
                  k.tt('dve', km[:, 0:32], rt[:, 0], rt[:, 1], ALU.subtract, [rB], [kmB])
                  k.tt('dve', km[:, 32:64], rt[:, 2], rt[:, 3], ALU.add, [rB], [kmB])
                  k.tr(pb[qi][0:64, 0:128], km, ident_b, [kmB, constB], [pbB[qi]])
                  k.copy('act', krT[0:64, tt * 128:(tt + 1) * 128], pb[qi][0:64, 0:128], [pbB[qi]], [krB])
              S.barrier()

              A.top = R1
              oTb = A.alloc([8, 2048], BF16)
              wuqn_s = A.alloc([4, 8, 128], BF16); wuqr_s = A.alloc([4, 8, 64], BF16)
              wuk_s = A.alloc([4, 1024], BF16); wuv_s = A.alloc([4, 1024], BF16)
              mwQN = B('mwqn'); mwQR = B('mwqr'); mwK = B('mwk'); mwV = B('mwv')
              assert A.top <= Z0
              k.dma('pool', wuqn_s, w_uqn, [], [mwQN], mwQN)
              k.dma('pool', wuqr_s, w_uqr, [], [mwQR], mwQR)
              k.dma('pool', wuk_s, w_uk, [], [mwK], mwK)
              k.dma('pool', wuv_s, w_uv, [], [mwV], mwV)
              A.top = SMALL_END
              qnT = A.alloc([2, 2048], BF16); knT = A.alloc([2, 2048], BF16); qrT = A.alloc([2, 2048], BF16)
              Vb = A.alloc([16, 2, 130], BF16)
              qnB = B('qnT'); knB = B('knT'); qrB = B('qrT'); VbB = B('Vb')
              QRM = [A.alloc([2, 64], BF16) for _ in range(2)]; QRMB = [B('qrm') for _ in range(2)]
              QRT = [A.alloc([4, 2, 32]) for _ in range(2)]; QRTB = [B('qrt') for _ in range(2)]
              assert A.top <= CQ0
              k.memset('pool', qrT[64:128, :, :], 0.0, [], [qrB])
              for g in range(4):
                  h0 = g * 2
                  k.memset('pool', Vb[:, :, :, 128:130], 1.0, [], [VbB])
                  ei = 0
                  for hl in range(2):
                      h = h0 + hl
                      for typ in range(2):
                          for ch in range(4):
                              pi = ei % 2; ps = pf[pi]
                              for kc in range(4):
                                  if typ == 0:
                                      k.mm(ps[:, :], wuqn_s[:, kc, h, :], cqnT[:, kc, ch * 512:(ch + 1) * 512], kc == 0, kc == 3, [mwQN, cqB], [pfB[pi]])
                                  else:
                                      k.mm(ps[:, :], wuk_s[:, kc, h * 128:(h + 1) * 128], ckvnT[:, kc, ch * 512:(ch + 1) * 512], kc == 0, kc == 3, [mwK, ckvB], [pfB[pi]])
                              dst = (qnT if typ == 0 else knT)[:, hl, ch * 512:(ch + 1) * 512]
                              k.copy('act' if ei % 2 == 0 else 'dve', dst, ps[:, :], [pfB[pi]], [qnB if typ == 0 else knB])
                              ei += 1
                  for tt in range(16):
                      pi = 2 + tt % 2; ps = pf[pi]
                      for kc in range(4):
                          k.mm(ps[:, 0:128], cqnT[:, kc, tt * 128:(tt + 1) * 128], wuqr_s[:, kc, h0:h0 + 2, :].rearrange('p a b -> p (a b)'),
                               kc == 0, kc == 3, [mwQR, cqB], [pfB[pi]])
                      qi = tt % 2; rt = QRT[qi]; rB = QRTB[qi]; qm = QRM[qi]; qmB = QRMB[qi]
                      ps3 = ps[:, 0:128].rearrange('p (h e) -> p h e', h=2)
                      cs = bc(cosB[:, tt, :], [128, 2, 32], 1); sn = bc(sinB[:, tt, :], [128, 2, 32], 1)
                      k.tt('dve', rt[:, 0], ps3[:, :, 0:32], cs, ALU.mult, [pfB[pi], constB], [rB])
                      k.tt('dve', rt[:, 1], ps3[:, :, 32:64], sn, ALU.mult, [pfB[pi], constB], [rB])
                      k.tt('dve', rt[:, 2], ps3[:, :, 32:64], cs, ALU.mult, [pfB[pi], constB], [rB])
                      k.tt('dve', rt[:, 3], ps3[:, :, 0:32], sn, ALU.mult, [pfB[pi], constB], [rB])
                      k.tt('dve', qm[:, :, 0:32], rt[:, 0], rt[:, 1], ALU.subtract, [rB], [qmB])
                      k.tt('dve', qm[:, :, 32:64], rt[:, 2], rt[:, 3], ALU.add, [rB], [qmB])
                      for hl in range(2):
                          k.tr(pb[qi][0:64, hl * 128:(hl + 1) * 128], qm[:, hl, :], ident_b, [qmB, constB], [pbB[qi]])
                      k.copy('act', qrT[0:64, :, tt * 128:(tt + 1) * 128], pb[qi][0:64, 0:256].rearrange('p (h e) -> p h e', h=2), [pbB[qi]], [qrB])
                      pi = 4 + tt % 2; ps = pf[pi]
                      for kc in range(4):
                          k.mm(ps[:, 0:256], ckvnT[:, kc, tt * 128:(tt + 1) * 128], wuv_s[:, kc, h0 * 128:(h0 + 2) * 128], kc == 0, kc == 3, [mwV, ckvB], [pfB[pi]])
                      k.copy('dve', Vb[:, tt, :, 0:128], ps[:, 0:256].rearrange('p (h e) -> p h e', h=2), [pfB[pi]], [VbB])
                  batches = [(hl, c, kt) for hl in range(2) for c in range(4) for kt in range(4 * c + 4)]
                  LA = 2
                  SB = [(pf[0], pfB[0]), (pf[1], pfB[1]), (pbf[1], pbB[1])]

                  def qkB(n):
                      hl, c, kt = batches[n]
                      sps, spB = SB[n % 3]
                      k.mm(sps[:, :], knT[:, hl, kt * 128:(kt + 1) * 128], qnT[:, hl, c * 512:(c + 1) * 512], True, False, [knB, qnB], [spB])
                      k.mm(sps[:, :], krT[:, kt * 128:(kt + 1) * 128], qrT[:, hl, c * 512:(c + 1) * 512], False, True, [krB, qrB], [spB])
                      pe_ = PE_[n % 3]; peB = PEB[n % 3]; pm = PM[n % 3]; pmB = PMB[n % 3]
                      j0_ = max(0, kt - 4 * c)
                      k.act(pe_[:, j0_ * 128:512], sps[:, j0_ * 128:512], AF.Exp, [spB], [peB], scale=192.0 ** -0.5)
                      if kt >= 4 * c:
                          k.tt('pool' if n % 2 else 'dve', pm[:, 0:128], pe_[:, j0_ * 128:(j0_ + 1) * 128], maskC, ALU.mult, [peB, constB], [pmB])

                  def pvB(n):
                      hl, c, kt = batches[n]
                      h = h0 + hl
                      pe_ = PE_[n % 3]; peB = PEB[n % 3]; pm = PM[n % 3]; pmB = PMB[n % 3]
                      for j in range(4):
                          qt = 4 * c + j
                          if kt > qt:
                              continue
                          ops_ = pf[2 + j]; opB = pfB[2 + j]
                          if kt == qt:
                              k.mm(ops_[:, 0:130], pm[:, 0:128], Vb[:, kt, hl, :], kt == 0, True, [pmB, VbB], [opB])
                              fi = j % 2
                              k.recip(RL[fi][:, 0:1], ops_[:, 128:129], [opB], [RLB[fi]])
                              k.ts('dve', OBF[fi], ops_[:, 0:128], RL[fi][:, 0:1], None, ALU.mult, None, [opB, RLB[fi]], [OBFB[fi]])
                              k.act(JK[:, 0:128], ops_[:, 0:128], AF.Square, [opB, RLB[fi]], [JKB, ssqBB], scale=RL[fi][:, 0:1], accum_out=ssqB_[:, qt, h:h + 1])
                              k.tr(pb[0][:, fi * 128:(fi + 1) * 128], OBF[fi], ident_b, [OBFB[fi], constB], [pbB[0]])
                              k.act(oTb[:, h, qt * 128:(qt + 1) * 128], pb[0][:, fi * 128:(fi + 1) * 128], AF.Identity, [pbB[0], constB], [oTbB], scale=gob[:, h:h + 1])
                          else:
                              k.mm(ops_[:, 0:130], pe_[:, j * 128:(j + 1) * 128], Vb[:, kt, hl, :], kt == 0, False, [peB, VbB], [opB])

                  for n in range(len(batches) + LA):
                      if n < len(batches):
                          qkB(n)
                      if n - LA >= 0:
                          pvB(n - LA)
              sA = A.alloc([16]); sAn = A.alloc([16]); sBn = A.alloc([16]); sB_ = B('ssum')
              k.S.op('dve', (lambda o, i: (lambda e: e.tensor_reduce(out=o, in_=i, axis=AX.X, op=ALU.add)))(sA, ssqA), [ssqAB], [sB_])
              k.S.op('dve', (lambda o, i: (lambda e: e.tensor_reduce(out=o, in_=i, axis=AX.X, op=ALU.add)))(sBn, ssqB_), [ssqBB], [sB_])
              ssB = B('ssqscr')
              k.dma('sp', ssqscr[b].rearrange('(i r) -> i r', r=16), sA, [sB_], [ssB], sB_)
              k.dma('sp', sAn, ssqscr[b].rearrange('(t p) -> p t', p=128), [ssB], [sB_], sB_, slow=True)
              k.act(rab[:, 0, :], sAn, AF.Sqrt, [sB_], [rabB], scale=1.0 / 1024, bias=1e-6)
              k.act(rab[:, 1, :], sBn, AF.Sqrt, [sB_], [rabB], scale=1.0 / 1024, bias=1e-6)
              k.recip(rab.rearrange('p a b -> p (a b)'), rab.rearrange('p a b -> p (a b)'), [rabB], [rabB])
              if stop == 'S6':
                  S.barrier()
                  A.top = SMALL_END
                  dt_ = A.alloc([2048]); dB = B('dbg')
                  for kc in range(16):
                      k.copy('dve', dt_, (oTa if kc < 8 else oTb)[:, kc % 8, :], [oTaB, oTbB], [dB])
                      k.dma('sp', dbg[:, kc, :], dt_, [dB], [], dB)
                  break
              S.barrier()

              A.top = R1 + 8192
              wo_s = A.alloc([16, 2048], BF16); woB = [B('wo') for _ in range(4)]
              for q4 in range(4):
                  k.dma('pool', wo_s[:, q4 * 4:(q4 + 1) * 4, :], w_o[:, q4 * 4:(q4 + 1) * 4, :], [], [woB[q4]], woB[q4])
              G1 = A.alloc([2048]); L1G = A.alloc([2048]); L1B = A.alloc([2048]); G1B = B('g1'); L1GB = B('l1g'); L1BB = B('l1b')
              k.dma('sp', G1, modrow[b, 0:1, :].to_broadcast([128, 2048]), [mrB], [G1B], G1B)
              k.dma('sp', L1G, ln1_g.to_broadcast([128, 2048]), [], [L1GB], L1GB)
              k.dma('sp', L1B, ln1_b.to_broadcast([128, 2048]), [], [L1BB], L1BB)
              XT = [A.alloc([2048]) for _ in range(2)]; XTB = [B('xt') for _ in range(2)]
              YT = [A.alloc([2048]) for _ in range(2)]; YTB = [B('yt') for _ in range(2)]
              TM = [A.alloc([512]) for _ in range(2)]; TMB = [B('tm') for _ in range(2)]
              STt = [A.alloc([4, 6]) for _ in range(2)]; MV = [A.alloc([8]) for _ in range(2)]; STB = [B('st') for _ in range(2)]
              x1B = B('x1scr')
              ti = 0
              for tt in range(16):
                  xt = XT[tt % 2]; xtB = XTB[tt % 2]; yt = YT[tt % 2]; ytB = YTB[tt % 2]
                  st = STt[tt % 2]; mv = MV[tt % 2]; stB = STB[tt % 2]
                  k.dma('sp', xt, x[b, tt * 128:(tt + 1) * 128, :], [], [xtB], xtB)
                  for nb in range(4):
                      pa = pf[(nb % 2) * 2]; paB = pfB[(nb % 2) * 2]; pb_ = pf[(nb % 2) * 2 + 1]; pbB_ = pfB[(nb % 2) * 2 + 1]
                      for fc in range(8):
                          k.mm(pa[:, :], oTa[:, fc, tt * 128:(tt + 1) * 128], wo_s[:, fc, nb * 512:(nb + 1) * 512], fc == 0, fc == 7, [oTaB, woB[fc // 4]], [paB])
                      for fc in range(8):
                          k.mm(pb_[:, :], oTb[:, fc, tt * 128:(tt + 1) * 128], wo_s[:, 8 + fc, nb * 512:(nb + 1) * 512], fc == 0, fc == 7, [oTbB, woB[2 + fc // 4]], [pbB_])
                      tm = TM[ti % 2]; tmB = TMB[ti % 2]; ti += 1
                      sl = slice(nb * 512, (nb + 1) * 512)
                      k.act(tm, pa[:, :], AF.Identity, [paB, rabB], [tmB], scale=rab[:, 0, tt:tt + 1])
                      k.stt(tm, pb_[:, :], rab[:, 1, tt:tt + 1], tm, ALU.mult, ALU.add, [pbB_, rabB, tmB], [tmB])
                      k.tt('pool', tm, tm, G1[:, sl], ALU.mult, [tmB, G1B], [tmB])
                      k.stt(yt[:, sl], xt[:, sl], ALPHA, tm, ALU.mult, ALU.add, [xtB, tmB], [ytB])
                      S.op('dve', (lambda o, i: (lambda e: e.bn_stats(out=o, in_=i)))(st[:, nb, :], yt[:, sl]), [ytB], [stB])
                  S.op('dve', (lambda o, i: (lambda e: e.bn_aggr(out=o, in_=i)))(mv[:, 0:2], st.rearrange('p a b -> p (a b)')), [stB], [stB])
                  k.act(mv[:, 2:3], mv[:, 1:2], AF.Sqrt, [stB], [stB], bias=1e-5)
                  k.recip(mv[:, 3:4], mv[:, 2:3], [stB], [stB])
                  k.ts('dve', mv[:, 4:5], mv[:, 0:1], mv[:, 3:4], -1.0, ALU.mult, ALU.mult, [stB], [stB])
                  k.act(yt, yt, AF.Identity, [ytB, stB], [ytB], scale=mv[:, 3:4], bias=mv[:, 4:5])
                  k.tt('pool', yt, yt, L1G, ALU.mult, [ytB, L1GB], [ytB])
                  k.tt('dve', yt, yt, L1B, ALU.add, [ytB, L1BB], [ytB])
                  k.dma('sp', x1scr[b, tt * 128:(tt + 1) * 128, :], yt, [ytB], [x1B], ytB)
              if stop == 'S7':
                  break
              S.barrier()

              A.top = P_END
              BC = [A.alloc([2048]) for _ in range(5)]; BCB = [B('bc2') for _ in range(5)]
              k.dma('sp', BC[0], modrow[b, 2:3, :].to_broadcast([128, 2048]), [mrB], [BCB[0]], BCB[0])
              k.dma('sp', BC[1], modrow[b, 1:2, :].to_broadcast([128, 2048]), [mrB], [BCB[1]], BCB[1])
              k.dma('sp', BC[2], modrow[b, 3:4, :].to_broadcast([128, 2048]), [mrB], [BCB[2]], BCB[2])
              k.dma('sp', BC[3], ln2_g.to_broadcast([128, 2048]), [], [BCB[3]], BCB[3])
              k.dma('sp', BC[4], ln2_b.to_broadcast([128, 2048]), [], [BCB[4]], BCB[4])
              X1 = [A.alloc([2048]) for _ in range(2)]; X1B = [B('x1') for _ in range(2)]
              ACC = A.alloc([2048]); ACCB = B('acc')
              H2 = ACC; H2B = ACCB
              H2b = [A.alloc([2048], BF16) for _ in range(2)]; H2bB = [B('h2b') for _ in range(2)]
              H2T = A.alloc([16, 128]); H2TB = B('h2T')
              WPQ = [A.alloc([16, 128]) for _ in range(2)]; WPQB = [B('wpq') for _ in range(2)]
              QTC = [A.alloc([128]) for _ in range(2)]; QTCB = [B('qtc') for _ in range(2)]
              SC = A.alloc([16, 128]); SCB = B('sc')
              WK = [A.alloc([128]) for _ in range(2)]; WKB = [B('wk') for _ in range(2)]
              TV = A.alloc([16, 16]); TVB = B('tv')
              TI = A.alloc([16, 16], U32); TIF = A.alloc([16, 16]); TIB = B('ti')
              CAND = A.alloc([8, 256]); CANDB = B('cand')
              WK2 = A.alloc([256]); WK2B = B('wk2')
              VALS = A.alloc([8, 16]); VALSB = B('vals')
              POS = A.alloc([8, 16], U32); POSB = B('pos')
              ABU = A.alloc([2, 128], U32); ABF = A.alloc([2, 8, 16]); ABB = B('ab')
              OH = CAND.rearrange('p h (a b) -> p h a b', a=16); OHB = CANDB
              SEL = A.alloc([2, 8, 16]); SELB = B('sel')
              EIDX = A.alloc([128]); EB0 = B('eidxf')
              EIDXU = [A.alloc([128], U32) for _ in range(2)]; EB = [B('eidx') for _ in range(2)]
              GT2 = [A.alloc([128]) for _ in range(2)]; GT2B = [B('gates') for _ in range(2)]
              GS = A.alloc([24]); GSB = B('gs')
              AA = A.alloc([128]); AAB = B('aa')
              WW = A.alloc([128]); WWB = B('ww')
              NGB = 7
              GB_ = [A.alloc([4096], BF16) for _ in range(NGB)]; GBB = [B('gb') for _ in range(NGB)]
              TG = A.alloc([128])
              DG = [A.alloc([128], BF16) for _ in range(4)]; DGB = [B('dg') for _ in range(4)]
              if b == 0:
                  print('S8 arena top', A.top, 'of', A.n)
              STt = [A.alloc([4, 6]) for _ in range(2)]; MV = [A.alloc([8]) for _ in range(2)]; STB = [B('st') for _ in range(2)]
              ST2 = A.alloc([4, 6]); MV2 = A.alloc([8]); ST2B = B('st2')
              outB = B('out')
              cnt = {'wq': 0, 'gi': 0}

              def ln_stats(src, srcB, st, mv, stB):
                  for c4 in range(4):
                      S.op('dve', (lambda o, i: (lambda e: e.bn_stats(out=o, in_=i)))(st[:, c4, :], src[:, c4 * 512:(c4 + 1) * 512]), [srcB], [stB])
                  S.op('dve', (lambda o, i: (lambda e: e.bn_aggr(out=o, in_=i)))(mv[:, 0:2], st.rearrange('p a b -> p (a b)')), [stB], [stB])
                  k.act(mv[:, 2:3], mv[:, 1:2], AF.Sqrt, [stB], [stB], bias=1e-5)
                  k.recip(mv[:, 3:4], mv[:, 2:3], [stB], [stB])
                  k.ts('dve', mv[:, 4:5], mv[:, 0:1], mv[:, 3:4], -1.0, ALU.mult, ALU.mult, [stB], [stB])

              def stageA(tt):
                  x1 = X1[tt % 2]; x1B_ = X1B[tt % 2]; st = STt[tt % 2]; mv = MV[tt % 2]; stB = STB[tt % 2]
                  h2b = H2b[tt % 2]; h2bB = H2bB[tt % 2]
                  k.dma('sp', x1, x1scr[b, tt * 128:(tt + 1) * 128, :], [x1B], [x1B_], x1B_)
                  ln_stats(x1, x1B_, st, mv, stB)
                  k.act(H2, x1, AF.Identity, [x1B_, stB], [H2B], scale=mv[:, 3:4], bias=mv[:, 4:5])
                  k.tt('dve', H2, H2, BC[0], ALU.mult, [H2B, BCB[0]], [H2B])
                  k.tt('pool', H2, H2, BC[1], ALU.add, [H2B, BCB[1]], [H2B])
                  k.copy('act', h2b, H2, [H2B], [h2bB])
                  for g4 in range(4):
                      pi = g4 % 2
                      for j in range(4):
                          kc = g4 * 4 + j
                          k.tr(pf[pi][:, j * 128:(j + 1) * 128], H2[:, kc * 128:(kc + 1) * 128], ident_f, [H2B, constB], [pfB[pi]])
                      k.copy('act', H2T[:, g4 * 4:(g4 + 1) * 4, :], pf[pi][:, :].rearrange('p (a b) -> p a b', a=4), [pfB[pi]], [H2TB])
                  for c in range(16):
                      wp = WPQ[cnt['wq'] % 2]; wpB = WPQB[cnt['wq'] % 2]; cnt['wq'] += 1
                      k.dma('sp', wp, w_pq[c], [], [wpB], wpB)
                      pi = c % 2
                      for kc in range(16):
                          k.mm(pf[pi][:, 0:128], wp[:, kc, :], H2T[:, kc, :], kc == 0, kc == 15, [wpB, H2TB], [pfB[pi]])
                      qc = QTC[c % 2]; qcB = QTCB[c % 2]
                      k.copy('act', qc, pf[pi][:, 0:128], [pfB[pi]], [qcB])
                      si = (c // 4) % 2
                      k.mm(pbf[si][:, (c % 4) * 128:(c % 4 + 1) * 128], qc, skT_s[:, c % 2, :], True, True, [qcB, constB], [pbB[si]])
                      if c % 4 == 3:
                          k.copy('act', SC[:, c - 3:c + 1, :], pbf[si][:, :].rearrange('p (a b) -> p a b', a=4), [pbB[si]], [SCB])

              def stageT(tt):
                  eu = EIDXU[tt % 2]; eB = EB[tt % 2]; GT = GT2[tt % 2]; GTB = GT2B[tt % 2]
                  for c in range(16):
                      wk = WK[c % 2]; wkB = WKB[c % 2]
                      S.op('dve', (lambda o, i: (lambda e: e.max(out=o, in_=i)))(TV[:, c, 0:8], SC[:, c, :]), [SCB], [TVB])
                      yield
                      S.op('dve', (lambda o, m, i: (lambda e: e.max_index(out=o, in_max=m, in_values=i)))(TI[:, c, 0:8], TV[:, c, 0:8], SC[:, c, :]), [SCB, TVB], [TIB])
                      yield
                      S.op('dve', (lambda o, m, i: (lambda e: e.match_replace(out=o, in_to_replace=m, in_values=i, imm_value=NEG)))(wk, TV[:, c, 0:8], SC[:, c, :]), [SCB, TVB], [wkB])
                      yield
                      S.op('dve', (lambda o, i: (lambda e: e.max(out=o, in_=i)))(TV[:, c, 8:16], wk), [wkB], [TVB])
                      yield
                      S.op('dve', (lambda o, m, i: (lambda e: e.max_index(out=o, in_max=m, in_values=i)))(TI[:, c, 8:16], TV[:, c, 8:16], wk), [wkB, TVB], [TIB])
                      yield
                  k.copy('dve', TIF, TI, [TIB], [TIB])
                  yield
                  tv4 = TV.rearrange('p (h t) k -> p h t k', t=2); ti4 = TIF.rearrange('p (h t) k -> p h t k', t=2)
                  k.tt('dve', CAND.rearrange('p h (a b) -> p h a b', a=16), bc(tv4[:, :, 0, :], [128, 8, 16, 16], 3), bc(tv4[:, :, 1, :], [128, 8, 16, 16], 2),
                       ALU.add, [TVB], [CANDB])
                  yield
                  for hh in range(8):
                      S.op('dve', (lambda o, i: (lambda e: e.max(out=o, in_=i)))(VALS[:, hh, 0:8], CAND[:, hh, :]), [CANDB], [VALSB])
                      yield
                      S.op('dve', (lambda o, m, i: (lambda e: e.max_index(out=o, in_max=m, in_values=i)))(POS[:, hh, 0:8], VALS[:, hh, 0:8], CAND[:, hh, :]), [CANDB, VALSB], [POSB])
                      yield
                      S.op('dve', (lambda o, m, i: (lambda e: e.match_replace(out=o, in_to_replace=m, in_values=i, imm_value=NEG)))(WK2, VALS[:, hh, 0:8], CAND[:, hh, :]), [CANDB, VALSB], [WK2B])
                      yield
                      S.op('dve', (lambda o, i: (lambda e: e.max(out=o, in_=i)))(VALS[:, hh, 8:16], WK2), [WK2B], [VALSB])
                      yield
                      S.op('dve', (lambda o, m, i: (lambda e: e.max_index(out=o, in_max=m, in_values=i)))(POS[:, hh, 8:16], VALS[:, hh, 8:16], WK2), [WK2B, VALSB], [POSB])
                      yield
                  posf = POS.rearrange('p h k -> p (h k)')
                  S.op('dve', (lambda o, i: (lambda e: e.tensor_single_scalar(out=o, in_=i, scalar=4, op=ALU.logical_shift_right)))(ABU[:, 0, :], posf), [POSB], [ABB])
                  yield
                  S.op('dve', (lambda o, i: (lambda e: e.tensor_single_scalar(out=o, in_=i, scalar=15, op=ALU.bitwise_and)))(ABU[:, 1, :], posf), [POSB], [ABB])
                  yield
                  k.copy('dve', ABF.rearrange('p t h k -> p (t h k)'), ABU.rearrange('p t n -> p (t n)'), [ABB], [ABB])
                  yield
                  io16 = iota[:, 0:16].unsqueeze(1).unsqueeze(1).to_broadcast([128, 8, 16, 16])
                  for t2 in range(2):
                      k.tt('dve', OH, io16, bc(ABF[:, t2], [128, 8, 16, 16], 3), ALU.is_equal, [ABB, constB], [OHB])
                      yield
                      k.tt('dve', OH, OH, bc(ti4[:, :, t2, :], [128, 8, 16, 16], 2), ALU.mult, [OHB, TIB], [OHB])
                      yield
                      S.op('dve', (lambda o, i: (lambda e: e.tensor_reduce(out=o, in_=i, axis=AX.X, op=ALU.add)))(SEL[:, t2], OH), [OHB], [SELB])
                      yield
                  k.stt(EIDX.rearrange('p (h k) -> p h k', h=8), SEL[:, 0], 128.0, SEL[:, 1], ALU.mult, ALU.add, [SELB], [EB0])
                  yield
                  k.copy('dve', eu, EIDX, [EB0], [eB])
                  yield
                  k.S.op('dve', (lambda o, i: (lambda e: e.tensor_reduce(out=o, in_=i, axis=AX.X, op=ALU.max)))(GS[:, 0:8], VALS), [VALSB], [GSB])
                  yield
                  k.ts('dve', GS[:, 8:16], GS[:, 0:8], -1.0, None, ALU.mult, None, [GSB], [GSB])
                  yield
                  for hh in range(8):
                      k.act(GT[:, hh * 16:(hh + 1) * 16], VALS[:, hh, :], AF.Exp, [VALSB, GSB], [GTB, GSB], bias=GS[:, 8 + hh:9 + hh], accum_out=GS[:, 16 + hh:17 + hh])
                      yield
                  k.recip(GS[:, 0:8], GS[:, 16:24], [GSB], [GSB])
                  yield
                  k.tt('dve', GT.rearrange('p (h k) -> p h k', h=8), GT.rearrange('p (h k) -> p h k', h=8), bc(GS[:, 0:8], [128, 8, 16], 2), ALU.mult, [GTB, GSB], [GTB])
                  yield

              def capture(stage, *args):
                  lst = []
                  real = S.op
                  S.op = lambda eng, fn, reads=(), writes=(), dma=None, ndma=1: lst.append((eng, fn, list(reads), list(writes), dma, ndma))
                  try:
                      r = stage(*args)
                      if r is not None:
                          for _ in r:
                              pass
                  finally:
                      S.op = real
                  return lst

              def replay(lst, n):
                  for _ in range(min(n, len(lst))):
                      eng, fn, R, W, dma, ndma = lst.pop(0)
                      S.op(eng, fn, R, W, dma=dma, ndma=ndma)

              def stageUV(tt, aops, tops):
                  eu = EIDXU[tt % 2]; eB = EB[tt % 2]; h2b = H2b[tt % 2]; h2bB = H2bB[tt % 2]; GT = GT2[tt % 2]; GTB = GT2B[tt % 2]
                  for kk in range(128):
                      gb = GB_[cnt['gi'] % NGB]; gbB = GBB[cnt['gi'] % NGB]; cnt['gi'] += 1
                      aB = Buf('aa'); wB = Buf('ww')
                      k.gather(gb, uvb, eu[:, kk:kk + 1], [eB, tabB], [gbB], gbB)
                      k.stt(gb[:, 0:2048], gb[:, 0:2048], 1.0, h2b, ALU.mult, ALU.mult, [gbB, h2bB], [aB, gbB], accum_out=AA[:, kk:kk + 1])
                      k.act(TG[:, kk:kk + 1], AA[:, kk:kk + 1], AF.Gelu, [aB], [wB])
                      k.act(WW[:, kk:kk + 1], TG[:, kk:kk + 1], AF.Identity, [wB, GTB], [wB], scale=GT[:, kk:kk + 1])
                      dg = DG[kk % 4]; dgB = DGB[kk % 4]
                      k.act(dg, ident_f, AF.Identity, [wB, constB], [dgB], scale=WW[:, kk:kk + 1])
                      for nb in range(4):
                          k.mm(pf[2 + nb][:, :], dg, gb[:, 2048 + nb * 512:2048 + (nb + 1) * 512], kk == 0, kk == 127, [dgB, gbB], [pfB[2 + nb]])
                      if kk < 40:
                          replay(aops, (len(aops) + 39 - kk) // (40 - kk))
                      else:
                          replay(aops, len(aops))
                          replay(tops, (len(tops) + 119 - kk) // max(1, 120 - kk) if kk < 120 else len(tops))
                  replay(aops, len(aops)); replay(tops, len(tops))

              def stageF(tt):
                  x1 = X1[tt % 2]; x1B_ = X1B[tt % 2]
                  for nb in range(4):
                      sl = slice(nb * 512, (nb + 1) * 512)
                      k.tt('dve', ACC[:, sl], pf[2 + nb][:, :], BC[2][:, sl], ALU.mult, [pfB[2 + nb], BCB[2]], [ACCB])
                  k.stt(ACC, x1, ALPHA, ACC, ALU.mult, ALU.add, [x1B_, ACCB], [ACCB])
                  ln_stats(ACC, ACCB, ST2, MV2, ST2B)
                  k.act(ACC, ACC, AF.Identity, [ACCB, ST2B], [ACCB], scale=MV2[:, 3:4], bias=MV2[:, 4:5])
                  k.tt('pool', ACC, ACC, BC[3], ALU.mult, [ACCB, BCB[3]], [ACCB])
                  k.tt('dve', ACC, ACC, BC[4], ALU.add, [ACCB, BCB[4]], [ACCB])
                  k.dma('sp', out[b, tt * 128:(tt + 1) * 128, :], ACC, [ACCB], [outB], ACCB)

              if peer_tiles > 0:
                  stageA(0)
                  for _ in stageT(0):
                      pass
              for tt in range(peer_tiles):
                  aops, tops = [], []
                  if tt + 1 < peer_tiles:
                      aops = capture(stageA, tt + 1)
                      tops = capture(stageT, tt + 1)
                  stageUV(tt, aops, tops)
                  stageF(tt)
              S.barrier()
        except StopBuild as sb:
            S.barrier()
            A.top = A.n - 2048
            dt_ = A.alloc([2048]); dB = Buf('dbgx')
            for i, (ap, n) in enumerate(sb.items):
                k.copy('dve', dt_[:ap.shape[0], 0:n], ap, [], [dB])
                k.dma('sp', dbg[:ap.shape[0], i, 0:n], dt_[:ap.shape[0], 0:n], [dB], [], dB)
        S.barrier()
        S.emit()
    return nc


def _consts():
    c = {}
    c['ident'] = np.eye(128, dtype=np.float32)
    ip = np.arange(128)[:, None]; i = np.arange(128)[None, :]
    mA = np.zeros((128, 31, 128), np.float32)
    for m in range(31):
        d = m - 15
        dt = 16 * (i - ip) + d
        mult = ((dt >= 0) & (dt <= 128)).astype(np.float32)
        if d % 4 == 0:
            mult += ((dt >= 0) & (dt <= 512))
        if d == 0:
            mult += (dt >= 0)
        mA[:, m, :] = mult
    c['maskA'] = mA
    c['maskC'] = (ip <= i).astype(np.float32)
    theta = 500000.0
    invA = theta ** (-np.arange(16, dtype=np.float64) * 2.0 / 32)
    tA = (16 * np.arange(128)[:, None] + np.arange(16)[None, :]).astype(np.float64)
    angA = tA[:, :, None] * invA[None, None, :]
    c['cosA'] = np.cos(angA).astype(np.float32); c['sinA'] = np.sin(angA).astype(np.float32)
    invB = theta ** (-np.arange(32, dtype=np.float64) * 2.0 / 64)
    tB = (128 * np.arange(16)[None, :] + np.arange(128)[:, None]).astype(np.float64)
    angB = tB[:, :, None] * invB[None, None, :]
    c['cosB'] = np.cos(angB).astype(np.float32); c['sinB'] = np.sin(angB).astype(np.float32)
    c['iota'] = np.broadcast_to(np.arange(256, dtype=np.float32), (128, 256)).copy()
    return c


def _prep_shared(inp):
    f = lambda a: np.ascontiguousarray(a, dtype=np.float32)
    sh = dict(_consts())
    w_ada = inp['w_ada'][0]
    sh['w_ada_r'] = f(w_ada.reshape(16, 128, 24, 512).transpose(2, 1, 0, 3))
    sh['b_adaT'] = f(inp['b_ada'][0].reshape(96, 128).T)
    sh['b_ada_row'] = f(inp['b_ada'][0].reshape(1, 12288))
    w_in = inp['w_in'][0]
    sh['w_in_r'] = f(w_in[:, :4096].reshape(16, 128, 8, 512).transpose(2, 1, 0, 3))
    sh['w_in_kr'] = f(w_in[:, 4096:4160].reshape(16, 128, 64).transpose(1, 0, 2))
    sh['g_qT'] = f(inp['g_q_lat'][0].reshape(4, 128).T); sh['g_kvT'] = f(inp['g_kv_lat'][0].reshape(4, 128).T)
    wuq = inp['w_uq'][0].reshape(4, 128, 8, 192).transpose(1, 0, 2, 3)
    sh['w_uqn'] = f(wuq[..., :128]); sh['w_uqr'] = f(wuq[..., 128:])
    sh['w_uk_r'] = f(inp['w_uk'][0].reshape(4, 128, 1024).transpose(1, 0, 2))
    sh['w_uv_r'] = f(inp['w_uv'][0].reshape(4, 128, 1024).transpose(1, 0, 2))
    sh['g_oaT'] = f(inp['g_out_a'][0].reshape(8, 128).T); sh['g_obT'] = f(inp['g_out_b'][0].reshape(8, 128).T)
    sh['w_o_r'] = f(inp['w_o'][0].reshape(16, 128, 2048).transpose(1, 0, 2))
    for n in ('ln1_g', 'ln1_b', 'ln2_g', 'ln2_b'):
        sh[n] = f(inp[n][0].reshape(1, 2048))
    sh['w_pq_r'] = f(inp['w_pq'][0].reshape(16, 128, 16, 128).transpose(2, 1, 0, 3))
    sh['skT'] = f(np.stack([inp['sub_key_1'][0].T, inp['sub_key_2'][0].T], axis=1))
    sh['u_table'] = f(inp['u_table'][0]); sh['v_table'] = f(inp['v_table'][0])
    return sh


def _core_map(sh, inp, b0, nseq):
    m = dict(sh)
    m['x'] = np.ascontiguousarray(inp['x'][b0:b0 + nseq], dtype=np.float32)
    cc = np.asarray(inp['c'][b0:b0 + nseq], dtype=np.float32)
    if nseq == 1:
        cc = np.concatenate([cc, cc], 0)
    m['cT'] = np.ascontiguousarray(cc.reshape(2, 16, 128).transpose(2, 1, 0))
    return m


_NC_CACHE = {}


def kernel(**inputs):
    inp = {k_: np.asarray(v) for k_, v in inputs.items()}
    nseq = 16 // NCORES
    if 'nc' not in _NC_CACHE:
        _NC_CACHE['nc'] = build(nseq=nseq)
    nc = _NC_CACHE['nc']
    sh = _prep_shared(inp)
    maps = [_core_map(sh, inp, c * nseq, nseq) for c in range(NCORES)]
    res = run_bass_kernel_spmd(nc, maps, core_ids=list(range(NCORES)))
    return np.concatenate([r['out'] for r in res.results], axis=0).astype(np.float32)
```

```python
import numpy as np
from contextlib import ExitStack
import concourse.bass as bass
import concourse.mybir as mybir
from concourse.bass_utils import run_bass_kernel_spmd

F32 = mybir.dt.float32; BF16 = mybir.dt.bfloat16; I32 = mybir.dt.int32; U32 = mybir.dt.uint32
ALU = mybir.AluOpType; AF = mybir.ActivationFunctionType; AX = mybir.AxisListType
SAME_SYNC = True
NCORES = 8
S_ = 2048; D_ = 2048
ALPHA = 2.0 ** 0.25
NEG = -1.0e30


class Buf:
    __slots__ = ('name', 'w', 'r', 'sem', 'semcnt', 'excl')

    def __init__(self, name, excl=False):
        self.name = name; self.w = {}; self.r = {}; self.sem = None; self.semcnt = 0; self.excl = excl


class Sched:
    ENG = ('pe', 'act', 'dve', 'pool', 'sp')

    def __init__(self, nc, ctx):
        self.nc = nc; self.ctx = ctx
        self.ops = {e: [] for e in self.ENG}
        self.esem = {e: ctx.enter_context(nc.semaphore('sem_' + e)) for e in self.ENG}
        self.dbufs = []; self.dset = set()
        self.uid = 0

    def op(self, eng, fn, reads=(), writes=(), dma=None, ndma=1):
        ops = self.ops[eng]
        idx = len(ops)
        deps = []
        for b in reads:
            deps.extend(b.w.values())
            if b.excl:
                deps.extend(v for kk, v in b.r.items() if kk != eng)
        for b in writes:
            deps.extend(b.w.values()); deps.extend(b.r.values())
        waits = set()
        for ev in deps:
            if ev[0] == 'E':
                e2 = ev[1]
                if e2 == eng and dma is None and (eng == 'pe' or not SAME_SYNC):
                    continue
                self.ops[e2][ev[2]]['inc'] = True
            waits.add(ev)
        if dma is not None:
            if dma.sem is None:
                dma.sem = self.ctx.enter_context(self.nc.semaphore('ds%d' % len(self.dbufs)))
            if id(dma) not in self.dset:
                self.dset.add(id(dma)); self.dbufs.append(dma)
            dma.semcnt += 16 * ndma
            ev = ('D', dma, dma.semcnt)
            self.uid += 1
            key = ('dma', self.uid)
        else:
            ev = ('E', eng, idx)
            key = eng
        ops.append(dict(fn=fn, waits=waits, inc=False, dma=dma))
        for b in reads:
            b.r[key] = ev
        for b in writes:
            b.w = {key: ev}; b.r = {}
        return ev

    def barrier(self):
        last = {}
        for e in self.ENG:
            for i in range(len(self.ops[e]) - 1, -1, -1):
                if self.ops[e][i]['fn'] is not None and self.ops[e][i]['dma'] is None:
                    last[e] = ('E', e, i); self.ops[e][i]['inc'] = True
                    break
        dmaev = [('D', b, b.semcnt) for b in self.dbufs if b.semcnt > 0]
        for e in self.ENG:
            waits = set(v for k, v in last.items() if (k != e or e != 'pe')) | set(dmaev)
            self.ops[e].append(dict(fn=None, waits=waits, inc=False, dma=None))

    def emit(self):
        nc = self.nc
        seq = {}
        for e in self.ENG:
            c = 0
            for i, o in enumerate(self.ops[e]):
                if o['inc']:
                    c += 1; seq[(e, i)] = c

        def run(eng, e):
            seen = {}
            for i, o in enumerate(self.ops[e]):
                for ev in o['waits']:
                    if ev[0] == 'E':
                        sem = self.esem[ev[1]]; val = seq[(ev[1], ev[2])]; key = ev[1]
                    else:
                        sem = ev[1].sem; val = ev[2]; key = id(ev[1])
                    if seen.get(key, 0) >= val:
                        continue
                    eng.wait_ge(sem, val); seen[key] = val
                if o['fn'] is None:
                    continue
                r = o['fn'](eng)
                if o['dma'] is not None:
                    for ins in (r if isinstance(r, (list, tuple)) else [r]):
                        ins.then_inc(o['dma'].sem, 16)
                elif o['inc']:
                    r.then_inc(self.esem[e], 1)

        with nc.Block() as block:
            block.sync(lambda eng: run(eng, 'sp'))
            block.scalar(lambda eng: run(eng, 'act'))
            block.vector(lambda eng: run(eng, 'dve'))
            block.gpsimd(lambda eng: run(eng, 'pool'))
            block.tensor(lambda eng: run(eng, 'pe'))


class Arena:
    def __init__(self, nc, ctx, nwords):
        self.t = ctx.enter_context(nc.sbuf_tensor('arena', [128, nwords], F32))
        self.n = nwords; self.top = 0

    def alloc(self, shape, dtype=F32):
        n = int(np.prod(shape))
        per = 2 if dtype == BF16 else 1
        words = (n + per - 1) // per
        words = (words + 1) // 2 * 2
        assert self.top + words <= self.n, ('arena OOM', self.top, words, self.n)
        ap = self.t[:, self.top:self.top + words]
        self.top += words
        if dtype != F32:
            ap = ap.bitcast(dtype)
        if dtype == BF16 and n != words * 2:
            ap = ap[:, 0:n]
        if len(shape) > 1:
            names = ' '.join('d%d' % i for i in range(len(shape)))
            ap = ap.rearrange('p (%s) -> p %s' % (names, names), **{'d%d' % i: s for i, s in enumerate(shape)})
        return ap


class K:
    def __init__(self, S):
        self.S = S

    def mm(self, out, lhsT, rhs, start, stop, R, W):
        self.S.op('pe', lambda e: e.matmul(out, lhsT=lhsT, rhs=rhs, start=start, stop=stop), R, W)

    def tr(self, out, in_, ident, R, W):
        self.S.op('pe', lambda e: e.transpose(out=out, in_=in_, identity=ident), R, W)

    def act(self, out, in_, func, R, W, scale=1.0, bias=0.0, accum_out=None):
        if accum_out is None:
            self.S.op('act', lambda e: e.activation(out=out, in_=in_, func=func, bias=bias, scale=scale), R, W)
        else:
            self.S.op('act', lambda e: e.activation(out=out, in_=in_, func=func, bias=bias, scale=scale,
                                                    accum_out=accum_out), R, W)

    def copy(self, eng, out, in_, R, W):
        if eng == 'act':
            self.S.op('act', lambda e: e.activation(out=out, in_=in_, func=AF.Copy), R, W)
        else:
            self.S.op(eng, lambda e: e.tensor_copy(out=out, in_=in_), R, W)

    def tt(self, eng, out, in0, in1, op, R, W):
        self.S.op(eng, lambda e: e.tensor_tensor(out=out, in0=in0, in1=in1, op=op), R, W)

    def ts(self, eng, out, in0, s1, s2, op0, op1, R, W, accum_out=None):
        if op1 is None:
            self.S.op(eng, lambda e: e.tensor_scalar(out=out, in0=in0, scalar1=s1, scalar2=None, op0=op0), R, W)
        elif accum_out is None:
            self.S.op(eng, lambda e: e.tensor_scalar(out=out, in0=in0, scalar1=s1, scalar2=s2, op0=op0, op1=op1), R, W)
        else:
            self.S.op(eng, lambda e: e.tensor_scalar(out=out, in0=in0, scalar1=s1, scalar2=s2, op0=op0, op1=op1,
                                                     accum_out=accum_out), R, W)

    def stt(self, out, in0, scalar, in1, op0, op1, R, W, accum_out=None):
        if accum_out is None:
            self.S.op('dve', lambda e: e.scalar_tensor_tensor(out=out, in0=in0, scalar=scalar, in1=in1, op0=op0, op1=op1), R, W)
        else:
            self.S.op('dve', lambda e: e.scalar_tensor_tensor(out=out, in0=in0, scalar=scalar, in1=in1, op0=op0, op1=op1,
                                                              accum_out=accum_out), R, W)

    def recip(self, out, in_, R, W):
        self.S.op('dve', lambda e: e.reciprocal(out=out, in_=in_), R, W)

    def memset(self, eng, ap, val, R, W):
        self.S.op(eng, lambda e: e.memset(ap, val), R, W)

    def dma(self, eng, out, in_, R, W, sem, slow=False):
        if slow:
            self.S.op(eng, lambda e: e.dma_start(out=out, in_=in_, allow_slow_non_contiguous=True), R, W, dma=sem)
        else:
            self.S.op(eng, lambda e: e.dma_start(out=out, in_=in_), R, W, dma=sem)

    def gather(self, out, table, idx_ap, R, W, sem):
        self.S.op('pool', lambda e: e.indirect_dma_start(
            out=out, out_offset=None, in_=table,
            in_offset=bass.IndirectOffsetOnAxis(ap=idx_ap, axis=0)), R, W, dma=sem)


class StopBuild(Exception):
    def __init__(self, items):
        self.items = items


def bc(ap, shape, axis):
    return ap.unsqueeze(axis).to_broadcast(shape)


def build(nseq=2, stop=None, peer_tiles=16, ntab=16384):
    nc = bass.Bass("TRN2", target_bir_lowering=False)

    def DI(name, shape, dt=F32):
        return nc.dram_tensor(name, shape, dt, kind="ExternalInput").ap()

    x = DI('x', [nseq, S_, D_])
    cT = DI('cT', [128, 16, 2])
    w_ada = DI('w_ada_r', [24, 128, 16, 512])
    b_adaT = DI('b_adaT', [128, 96])
    b_ada_row = DI('b_ada_row', [1, 12288])
    w_in = DI('w_in_r', [8, 128, 16, 512])
    w_in_kr = DI('w_in_kr', [128, 16, 64])
    g_qT = DI('g_qT', [128, 4]); g_kvT = DI('g_kvT', [128, 4])
    w_uqn = DI('w_uqn', [128, 4, 8, 128]); w_uqr = DI('w_uqr', [128, 4, 8, 64])
    w_uk = DI('w_uk_r', [128, 4, 1024]); w_uv = DI('w_uv_r', [128, 4, 1024])
    g_oaT = DI('g_oaT', [128, 8]); g_obT = DI('g_obT', [128, 8])
    w_o = DI('w_o_r', [128, 16, 2048])
    ln1_g = DI('ln1_g', [1, D_]); ln1_b = DI('ln1_b', [1, D_]); ln2_g = DI('ln2_g', [1, D_]); ln2_b = DI('ln2_b', [1, D_])
    w_pq = DI('w_pq_r', [16, 128, 16, 128])
    skT = DI('skT', [128, 2, 128])
    u_table = DI('u_table', [ntab, D_]); v_table = DI('v_table', [ntab, D_])
    ident_d = DI('ident', [128, 128])
    maskA_d = DI('maskA', [128, 31, 128]); maskC_d = DI('maskC', [128, 128])
    cosA_d = DI('cosA', [128, 16, 16]); sinA_d = DI('sinA', [128, 16, 16])
    cosB_d = DI('cosB', [128, 16, 32]); sinB_d = DI('sinB', [128, 16, 32])
    iota_d = DI('iota', [128, 256])
    out = nc.dram_tensor('out', [nseq, S_, D_], F32, kind="ExternalOutput").ap()
    dbgk = "ExternalOutput" if stop else "Internal"
    modrow = nc.dram_tensor('modrow', [nseq, 4, D_], F32, kind="Internal").ap()
    ssqscr = nc.dram_tensor('ssqscr', [nseq, S_], F32, kind="Internal").ap()
    x1scr = nc.dram_tensor('x1scr', [nseq, S_, D_], F32, kind=dbgk).ap()
    uvb = nc.dram_tensor('uvb', [ntab, 2 * D_], BF16, kind="Internal").ap()
    dbg = nc.dram_tensor('dbg', [128, 16, 2048], F32, kind=dbgk).ap() if stop else None

    ctx = ExitStack()
    with ctx:
        S = Sched(nc, ctx)
        k = K(S)
        A = Arena(nc, ctx, 53000)
        A.n = 53000 if not stop else 53000 - 0
        pf = [ctx.enter_context(nc.psum_tensor('pf%d' % i, [128, 512], F32)) for i in range(6)]
        pbf = [ctx.enter_context(nc.psum_tensor('pb%d' % i, [128, 512], F32)) for i in range(2)]
        pb = [t[:, :].bitcast(BF16) for t in pbf]
        pfB = [Buf('pf%d' % i, True) for i in range(6)]
        pbB = [Buf('pb%d' % i, True) for i in range(2)]
        nb_ = [0]

        bcnt = {}; bprev = {}

        def B(name='b'):
            nb_[0] += 1
            bcnt[name] = bcnt.get(name, 0) + 1
            key = (name, bcnt[name])
            nb = Buf('%s%d' % (name, nb_[0]))
            if key in bprev:
                nb.sem = bprev[key].sem; nb.semcnt = bprev[key].semcnt
            bprev[key] = nb
            return nb

        ident_f = A.alloc([128]); ident_b = A.alloc([128], BF16)
        maskA = A.alloc([31, 128], BF16); maskC = A.alloc([128], BF16)
        cosA = A.alloc([16, 16]); sinA = A.alloc([16, 16]); cosB = A.alloc([16, 32]); sinB = A.alloc([16, 32])
        sh1T = A.alloc([16, 2]); sc1T = A.alloc([16, 2])
        gq = A.alloc([4]); gkv = A.alloc([4]); goa = A.alloc([8]); gob = A.alloc([8])
        skT_s = A.alloc([2, 128])
        iota = A.alloc([256])
        constB = B('const')
        def cdma(eng, dst, src):
            cb = B('c'); k.dma(eng, dst, src, [], [cb], cb)
        cdma('sp', ident_f, ident_d)
        cdma('pool', ident_b, ident_d)
        cdma('pool', maskA, maskA_d)
        cdma('pool', maskC, maskC_d)
        for dst, src in ((cosA, cosA_d), (sinA, sinA_d), (cosB, cosB_d), (sinB, sinB_d), (gq, g_qT), (gkv, g_kvT),
                         (goa, g_oaT), (gob, g_obT), (skT_s, skT), (iota, iota_d)):
            cdma('sp', dst, src)
        P_END = A.top

        cact = A.alloc([16, 2]); badaT = A.alloc([96]); brow = A.alloc([12288]); rowt = [A.alloc([512]) for _ in range(2)]
        wblk0 = [A.alloc([16, 512]) for _ in range(2)]
        s0B = B('s0'); wB0 = [B('wada') for _ in range(2)]; rowB = [B('rowt') for _ in range(2)]; modB = B('modT'); mrB = B('modrow')
        s0b2 = B('s0b'); s0b3 = B('s0c')
        k.dma('sp', cact, cT, [], [s0B], s0B)
        k.dma('sp', badaT, b_adaT, [], [s0b2], s0b2)
        k.dma('sp', brow[0:1, :], b_ada_row, [], [s0b3], s0b3)
        k.act(cact, cact, AF.Silu, [s0B], [s0B])
        CB = [A.alloc([4096], BF16) for _ in range(3)]; CBB = [B('cb') for _ in range(3)]; CSB = [B('cs') for _ in range(3)]
        tabB = B('tab')
        ci_ = 0
        for src_t, off_t in ((u_table, 0), (v_table, D_)):
            for ci in range(ntab // 256):
                cb = CB[ci_ % 3]; cbB = CBB[ci_ % 3]; csB = CSB[ci_ % 3]; ci_ += 1
                k.dma('pool', cb, src_t[ci * 256:(ci + 1) * 256, :].rearrange('(p a) d -> p (a d)', a=2), [], [cbB], cbB)
                k.dma('act', uvb[ci * 256:(ci + 1) * 256, off_t:off_t + D_].rearrange('(p a) d -> p a d', a=2),
                      cb.rearrange('p (a d) -> p a d', a=2), [cbB], [tabB], csB)
        ri = 0
        for blk in range(24):
            wb = wblk0[blk % 2]; wbB = wB0[blk % 2]
            k.dma('sp', wb, w_ada[blk], [], [wbB], wbB)
            if blk < 8:
                for j in range(4):
                    ch = (blk % 4) * 4 + j
                    ps = pf[j % 2][:, 0:2]
                    for kc in range(16):
                        k.mm(ps, wb[:, kc, j * 128:(j + 1) * 128], cact[:, kc, :], kc == 0, kc == 15, [wbB, s0B], [pfB[j % 2]])
                    if blk < 4:
                        k.ts('dve', sh1T[:, ch, :], ps, badaT[:, blk * 4 + j:blk * 4 + j + 1], None, ALU.add, None, [pfB[j % 2], s0b2], [modB])
                    else:
                        k.ts('dve', sc1T[:, ch, :], ps, badaT[:, blk * 4 + j:blk * 4 + j + 1], 1.0, ALU.add, ALU.add, [pfB[j % 2], s0b2], [modB])
            else:
                slot = (blk - 8) // 4; q4 = (blk - 8) % 4
                for b in range(nseq):
                    pi = 2 + (ri % 2); ps = pf[pi][0:1, :]
                    for kc in range(16):
                        k.mm(ps, cact[:, kc, b:b + 1], wb[:, kc, :], kc == 0, kc == 15, [wbB, s0B], [pfB[pi]])
                    rt = rowt[ri % 2]; rB = rowB[ri % 2]
                    k.tt('dve', rt[0:1, :], ps, brow[0:1, blk * 512:(blk + 1) * 512], ALU.add, [pfB[pi], s0b3], [rB])
                    if slot == 2:
                        k.ts('dve', rt[0:1, :], rt[0:1, :], 1.0, None, ALU.add, None, [rB], [rB])
                    k.dma('sp', modrow[b, slot:slot + 1, q4 * 512:(q4 + 1) * 512], rt[0:1, :], [rB], [mrB], rB)
                    ri += 1
        S.barrier()
        A.top = P_END

        oTa = A.alloc([8, 2048], BF16)
        ssqA = A.alloc([16, 8]); ssqAB = B('ssqA')
        ssqB_ = A.alloc([16, 8]); ssqBB = B('ssqB')
        rab = A.alloc([2, 16]); rabB = B('rab')
        R1 = A.top
        hT = A.alloc([16, 2048], BF16)
        Z0 = A.top
        ZEND = A.n
        hTB = B('hT'); oTaB = B('oTa'); oTbB = B('oTb')

        try:
          bsnap = dict(bcnt)
          for b in range(nseq):
              bcnt.clear(); bcnt.update(bsnap)
              A.top = Z0
              XT = [A.alloc([2048]) for _ in range(2)]; XN = [A.alloc([2048], BF16) for _ in range(2)]
              STt = [A.alloc([4, 6]) for _ in range(2)]; MV = [A.alloc([8]) for _ in range(2)]
              XTB = [B('xt') for _ in range(2)]; XNB = [B('xn') for _ in range(2)]; STB = [B('st') for _ in range(2)]
              for tt in range(16):
                  xt = XT[tt % 2]; xn = XN[tt % 2]; st = STt[tt % 2]; mv = MV[tt % 2]
                  xtB = XTB[tt % 2]; xnB = XNB[tt % 2]; stB = STB[tt % 2]
                  k.dma('sp', xt, x[b, tt * 128:(tt + 1) * 128, :], [], [xtB], xtB)
                  for c4 in range(4):
                      S.op('dve', (lambda o, i: (lambda e: e.bn_stats(out=o, in_=i)))(st[:, c4, :], xt[:, c4 * 512:(c4 + 1) * 512]), [xtB], [stB])
                  S.op('dve', (lambda o, i: (lambda e: e.bn_aggr(out=o, in_=i)))(mv[:, 0:2], st.rearrange('p a b -> p (a b)')), [stB], [stB])
                  k.act(mv[:, 2:3], mv[:, 1:2], AF.Sqrt, [stB], [stB], bias=1e-5)
                  k.recip(mv[:, 3:4], mv[:, 2:3], [stB], [stB])
                  k.ts('dve', mv[:, 4:5], mv[:, 0:1], mv[:, 3:4], -1.0, ALU.mult, ALU.mult, [stB], [stB])
                  k.act(xn, xt, AF.Identity, [xtB, stB], [xnB], scale=mv[:, 3:4], bias=mv[:, 4:5])
                  for g4 in range(4):
                      pbi = g4 % 2
                      for j in range(4):
                          kc = g4 * 4 + j
                          k.tr(pb[pbi][:, j * 128:(j + 1) * 128], xn[:, kc * 128:(kc + 1) * 128], ident_b, [xnB, constB], [pbB[pbi]])
                      for j in range(4):
                          kc = g4 * 4 + j
                          o = hT[:, kc, tt * 128:(tt + 1) * 128]; i = pb[pbi][:, j * 128:(j + 1) * 128]
                          if pbi == 0:
                              k.act(o, i, AF.Identity, [pbB[pbi], modB], [hTB], scale=sc1T[:, kc, b:b + 1], bias=sh1T[:, kc, b:b + 1])
                          else:
                              k.ts('dve', o, i, sc1T[:, kc, b:b + 1], sh1T[:, kc, b:b + 1], ALU.mult, ALU.add, [pbB[pbi], modB], [hTB])
              if stop == 'S1':
                  S.barrier()
                  dt_ = A.alloc([2048]); dB = B('dbg')
                  for kc in range(16):
                      k.copy('dve', dt_, hT[:, kc, :], [hTB], [dB])
                      k.dma('sp', dbg[:, kc, :], dt_, [dB], [], dB)
                  break
              S.barrier()

              A.top = Z0
              QTM = [A.alloc([4, 128], BF16) for _ in range(2)]; QTMB = [B('qtm') for _ in range(2)]
              RT = [A.alloc([4, 4, 16]) for _ in range(2)]; RTB = [B('rt') for _ in range(2)]
              PE_ = [A.alloc([512], BF16) for _ in range(3)]; PEB = [B('pexp') for _ in range(3)]
              PM = [A.alloc([512], BF16) for _ in range(3)]; PMB = [B('pm') for _ in range(3)]
              OBF = [A.alloc([128], BF16) for _ in range(2)]; OBFB = [B('obf') for _ in range(2)]
              RL = [A.alloc([2]) for _ in range(2)]; RLB = [B('rl') for _ in range(2)]
              JK = A.alloc([512], BF16); JKB = B('junk')
              SMALL_END = A.top
              WB = [A.alloc([16, 512], BF16) for _ in range(2)]; WBB = [B('wblk') for _ in range(2)]
              QT = A.alloc([4, 2048], BF16); KT = A.alloc([4, 2048], BF16); V = A.alloc([16, 4, 130], BF16)
              QTB = B('QT'); KTB = B('KT'); VB = B('V')
              wi = 0
              it_ = 0
              for g in range(2):
                  k.memset('pool', V[:, :, :, 128:130], 1.0, [], [VB])
                  for typ in range(3):
                      blk = typ * 2 + g
                      wb = WB[wi % 2]; wbB = WBB[wi % 2]; wi += 1
                      k.dma('pool', wb, w_in[blk], [], [wbB], wbB)
                      if stop == 'B1':
                          raise StopBuild([(wb[:, 0, :], 512), (V[:, 0, :, :].rearrange('p a b -> p (a b)'), 520)])
                      for r in range(16):
                          pi = r % 2; ps = pf[pi]
                          for kc in range(16):
                              k.mm(ps[:, :], hT[:, kc, r:2048:16], wb[:, kc, :], kc == 0, kc == 15, [hTB, wbB], [pfB[pi]])
                          ps3 = ps[:, :].rearrange('p (h e) -> p h e', h=4)
                          if typ == 2:
                              k.copy('act' if r % 2 == 0 else 'dve', V[:, r, :, 0:128], ps3, [pfB[pi]], [VB])
                              continue
                          qi = it_ % 2; it_ += 1
                          qtm = QTM[qi]; qB = QTMB[qi]; rt = RT[qi]; rB = RTB[qi]
                          k.copy('act', qtm[:, :, 32:128], ps3[:, :, 32:128], [pfB[pi]], [qB])
                          if stop == 'B2a':
                              raise StopBuild([(qtm[:, 0, :], 128)])
                          cs = bc(cosA[:, r, :], [128, 4, 16], 1); sn = bc(sinA[:, r, :], [128, 4, 16], 1)
                          x1_ = ps3[:, :, 0:16]; x2_ = ps3[:, :, 16:32]
                          k.tt('dve', rt[:, 0], x1_, cs, ALU.mult, [pfB[pi], constB], [rB])
                          k.tt('dve', rt[:, 1], x2_, sn, ALU.mult, [pfB[pi], constB], [rB])
                          k.tt('dve', rt[:, 2], x2_, cs, ALU.mult, [pfB[pi], constB], [rB])
                          k.tt('dve', rt[:, 3], x1_, sn, ALU.mult, [pfB[pi], constB], [rB])
                          k.tt('dve', qtm[:, :, 0:16], rt[:, 0], rt[:, 1], ALU.subtract, [rB], [qB])
                          k.tt('dve', qtm[:, :, 16:32], rt[:, 2], rt[:, 3], ALU.add, [rB], [qB])
                          if stop == 'B2b':
                              raise StopBuild([(qtm[:, 0, :], 128), (rt[:, 0].rearrange('p a b -> p (a b)'), 64)])
                          pbi = qi
                          for hl in range(4):
                              k.tr(pb[pbi][:, hl * 128:(hl + 1) * 128], qtm[:, hl, :], ident_b, [qB, constB], [pbB[pbi]])
                          dst = (QT if typ == 0 else KT)[:, :, r * 128:(r + 1) * 128]
                          k.copy('act' if r % 2 == 1 else 'dve', dst, pb[pbi][:, 0:512].rearrange('p (h e) -> p h e', h=4),
                                 [pbB[pbi]], [QTB if typ == 0 else KTB])
                          if stop == 'B2':
                              raise StopBuild([(QT[:, 0, 0:128], 128), (qtm[:, 0, :], 128), (rt[:, 0].rearrange('p a b -> p (a b)'), 64)])
                  if stop == 'B3':
                      raise StopBuild([(QT[:, 0, :], 2048), (KT[:, 0, :], 2048), (V[:, 0:3, :, :].rearrange('p a b c -> p (a b c)'), 1560)])
                  batches = [(hl, c, rp) for hl in range(4) for c in range(4) for rp in range(16)]
                  LA = 2
                  SB = [(pf[0], pfB[0]), (pf[1], pfB[1]), (pbf[1], pbB[1])]

                  def qkA(n):
                      hl, c, rp = batches[n]
                      sps, spB = SB[n % 3]
                      k.mm(sps[:, :], KT[:, hl, rp * 128:(rp + 1) * 128], QT[:, hl, c * 512:(c + 1) * 512], True, True, [KTB, QTB], [spB])
                      pe_ = PE_[n % 3]; peB = PEB[n % 3]; pm = PM[n % 3]; pmB = PMB[n % 3]
                      k.act(pe_, sps[:, :], AF.Exp, [spB], [peB], scale=128.0 ** -0.5)
                      n0 = 15 - rp + 4 * c
                      k.tt('pool' if n % 2 else 'dve', pm, pe_, maskA[:, n0:n0 + 4, :].rearrange('p a b -> p (a b)'), ALU.mult, [peB, constB], [pmB])

                  def pvA(n):
                      hl, c, rp = batches[n]
                      h = g * 4 + hl
                      pm = PM[n % 3]; pmB = PMB[n % 3]
                      for j in range(4):
                          k.mm(pf[2 + j][:, 0:130], pm[:, j * 128:(j + 1) * 128], V[:, rp, hl, :], rp == 0, rp == 15, [pmB, VB], [pfB[2 + j]])
                      if rp == 15:
                          for j in range(4):
                              r = 4 * c + j; fi = j % 2; ops_ = pf[2 + j]; opB = pfB[2 + j]
                              k.recip(RL[fi][:, 0:1], ops_[:, 128:129], [opB], [RLB[fi]])
                              k.ts('dve', OBF[fi], ops_[:, 0:128], RL[fi][:, 0:1], None, ALU.mult, None, [opB, RLB[fi]], [OBFB[fi]])
                              k.act(JK[:, 0:128], ops_[:, 0:128], AF.Square, [opB, RLB[fi]], [JKB, ssqAB], scale=RL[fi][:, 0:1], accum_out=ssqA[:, r, h:h + 1])
                              k.tr(pb[0][:, fi * 128:(fi + 1) * 128], OBF[fi], ident_b, [OBFB[fi], constB], [pbB[0]])
                              k.act(oTa[:, h, r:2048:16], pb[0][:, fi * 128:(fi + 1) * 128], AF.Identity, [pbB[0], constB], [oTaB], scale=goa[:, h:h + 1])

                  for n in range(len(batches) + LA):
                      if n < len(batches):
                          qkA(n)
                      if n - LA >= 0:
                          pvA(n - LA)
              if stop == 'S3':
                  S.barrier()
                  A.top = SMALL_END
                  dt_ = A.alloc([2048]); dB = B('dbg')
                  for kc in range(8):
                      k.copy('dve', dt_, oTa[:, kc, :], [oTaB], [dB])
                      k.dma('sp', dbg[:, kc, :], dt_, [dB], [], dB)
                  k.copy('dve', dt_[:, 0:128], ssqA.rearrange('p a b -> p (a b)'), [ssqAB], [dB])
                  k.dma('sp', dbg[:, 8, 0:128], dt_[:, 0:128], [dB], [], dB)
                  break
              S.barrier()

              A.top = SMALL_END
              CQ0 = ZEND - 9216
              WB = [A.alloc([16, 512], BF16) for _ in range(2)]; WBB = [B('wblk') for _ in range(2)]
              WKR = A.alloc([16, 64], BF16); WKRB = B('wkr')
              CTM = [A.alloc([512], BF16) for _ in range(2)]; CTMB = [B('ctm') for _ in range(2)]
              SQ = [A.alloc([4]) for _ in range(2)]; SQB = [B('sq') for _ in range(2)]
              KRT = [A.alloc([4, 32]) for _ in range(2)]; KRTB = [B('krt') for _ in range(2)]
              KRM = [A.alloc([64], BF16) for _ in range(2)]; KRMB = [B('krm') for _ in range(2)]
              assert A.top <= CQ0
              save = A.top
              A.top = CQ0
              cqnT = A.alloc([4, 2048], BF16); ckvnT = A.alloc([4, 2048], BF16); krT = A.alloc([2048], BF16)
              cqB = B('cqnT'); ckvB = B('ckvnT'); krB = B('krT')
              A.top = save
              k.memset('pool', krT[64:128, :], 0.0, [], [krB])
              k.dma('pool', WB[0], w_in[6], [], [WBB[0]], WBB[0])
              k.dma('pool', WB[1], w_in[7], [], [WBB[1]], WBB[1])
              k.dma('pool', WKR, w_in_kr, [], [WKRB], WKRB)
              it_ = 0
              for typ in range(2):
                  wb = WB[typ]; wbB = WBB[typ]
                  dstT = cqnT if typ == 0 else ckvnT; dstB = cqB if typ == 0 else ckvB; gsc = gq if typ == 0 else gkv
                  for tt in range(16):
                      pi = tt % 2; ps = pf[pi]
                      for kc in range(16):
                          k.mm(ps[:, :], hT[:, kc, tt * 128:(tt + 1) * 128], wb[:, kc, :], kc == 0, kc == 15, [hTB, wbB], [pfB[pi]])
                      qi = it_ % 2; it_ += 1
                      sq = SQ[qi]; sqB = SQB[qi]; ctm = CTM[qi]; ctB = CTMB[qi]
                      k.act(JK, ps[:, :], AF.Square, [pfB[pi]], [JKB, sqB], accum_out=sq[:, 0:1])
                      k.act(sq[:, 1:2], sq[:, 0:1], AF.Sqrt, [sqB], [sqB], scale=1.0 / 512, bias=1e-6)
                      k.recip(sq[:, 2:3], sq[:, 1:2], [sqB], [sqB])
                      k.ts('dve', ctm, ps[:, :], sq[:, 2:3], None, ALU.mult, None, [pfB[pi], sqB], [ctB])
                      for j in range(4):
                          k.tr(pb[qi][:, j * 128:(j + 1) * 128], ctm[:, j * 128:(j + 1) * 128], ident_b, [ctB, constB], [pbB[qi]])
                      k.tt('dve', dstT[:, :, tt * 128:(tt + 1) * 128], pb[qi][:, 0:512].rearrange('p (a b) -> p a b', a=4),
                           bc(gsc, [128, 4, 128], 2), ALU.mult, [pbB[qi], constB], [dstB])
              for tt in range(16):
                  pi = 2 + tt % 2; ps = pf[pi]
                  for kc in range(16):
                      k.mm(ps[:, 0:64], hT[:, kc, tt * 128:(tt + 1) * 128], WKR[:, kc, :], kc == 0, kc == 15, [hTB, WKRB], [pfB[pi]])
                  qi = tt % 2; rt = KRT[qi]; rB = KRTB[qi]; km = KRM[qi]; kmB = KRMB[qi]
                  cs = cosB[:, tt, :]; sn = sinB[:, tt, :]
                  k.tt('dve', rt[:, 0], ps[:, 0:32], cs, ALU.mult, [pfB[pi], constB], [rB])
                  k.tt('dve', rt[:, 1], ps[:, 32:64], sn, ALU.mult, [pfB[pi], constB], [rB])
                  k.tt('dve', rt[:, 2], ps[:, 32:64], cs, ALU.mult, [pfB[pi], constB], [rB])
                  k.tt('dve', rt[:, 3], ps[:, 0:32], sn, ALU.mult, [pfB[pi], constB], [rB])
                  k.tt('dve', km[:, 0:32], rt[:, 0], rt[:, 1], ALU.subtract, [rB], [kmB])
                  k.tt('dve', km[:, 32:64], rt[:, 2], rt[:, 3], ALU.add, [rB], [kmB])
                  k.tr(pb[qi][0:64, 0:128], km, ident_b, [kmB, constB], [pbB[qi]])
                  k.copy('act', krT[0:64, tt * 128:(tt + 1) * 128], pb[qi][0:64, 0:128], [pbB[qi]], [krB])
              S.barrier()

              A.top = R1
              oTb = A.alloc([8, 2048], BF16)
              wuqn_s = A.alloc([4, 8, 128], BF16); wuqr_s = A.alloc([4, 8, 64], BF16)
              wuk_s = A.alloc([4, 1024], BF16); wuv_s = A.alloc([4, 1024], BF16)
              mwQN = B('mwqn'); mwQR = B('mwqr'); mwK = B('mwk'); mwV = B('mwv')
              assert A.top <= Z0
              k.dma('pool', wuqn_s, w_uqn, [], [mwQN], mwQN)
              k.dma('pool', wuqr_s, w_uqr, [], [mwQR], mwQR)
              k.dma('pool', wuk_s, w_uk, [], [mwK], mwK)
              k.dma('pool', wuv_s, w_uv, [], [mwV], mwV)
              A.top = SMALL_END
              qnT = A.alloc([2, 2048], BF16); knT = A.alloc([2, 2048], BF16); qrT = A.alloc([2, 2048], BF16)
              Vb = A.alloc([16, 2, 130], BF16)
              qnB = B('qnT'); knB = B('knT'); qrB = B('qrT'); VbB = B('Vb')
              QRM = [A.alloc([2, 64], BF16) for _ in range(2)]; QRMB = [B('qrm') for _ in range(2)]
              QRT = [A.alloc([4, 2, 32]) for _ in range(2)]; QRTB = [B('qrt') for _ in range(2)]
              assert A.top <= CQ0
              k.memset('pool', qrT[64:128, :, :], 0.0, [], [qrB])
              for g in range(4):
                  h0 = g * 2
                  k.memset('pool', Vb[:, :, :, 128:130], 1.0, [], [VbB])
                  ei = 0
                  for hl in range(2):
                      h = h0 + hl
                      for typ in range(2):
                          for ch in range(4):
                              pi = ei % 2; ps = pf[pi]
                              for kc in range(4):
                                  if typ == 0:
                                      k.mm(ps[:, :], wuqn_s[:, kc, h, :], cqnT[:, kc, ch * 512:(ch + 1) * 512], kc == 0, kc == 3, [mwQN, cqB], [pfB[pi]])
                                  else:
                                      k.mm(ps[:, :], wuk_s[:, kc, h * 128:(h + 1) * 128], ckvnT[:, kc, ch * 512:(ch + 1) * 512], kc == 0, kc == 3, [mwK, ckvB], [pfB[pi]])
                              dst = (qnT if typ == 0 else knT)[:, hl, ch * 512:(ch + 1) * 512]
                              k.copy('act' if ei % 2 == 0 else 'dve', dst, ps[:, :], [pfB[pi]], [qnB if typ == 0 else knB])
                              ei += 1
                  for tt in range(16):
                      pi = 2 + tt % 2; ps = pf[pi]
                      for kc in range(4):
                          k.mm(ps[:, 0:128], cqnT[:, kc, tt * 128:(tt + 1) * 128], wuqr_s[:, kc, h0:h0 + 2, :].rearrange('p a b -> p (a b)'),
                               kc == 0, kc == 3, [mwQR, cqB], [pfB[pi]])
                      qi = tt % 2; rt = QRT[qi]; rB = QRTB[qi]; qm = QRM[qi]; qmB = QRMB[qi]
                      ps3 = ps[:, 0:128].rearrange('p (h e) -> p h e', h=2)
                      cs = bc(cosB[:, tt, :], [128, 2, 32], 1); sn = bc(sinB[:, tt, :], [128, 2, 32], 1)
                      k.tt('dve', rt[:, 0], ps3[:, :, 0:32], cs, ALU.mult, [pfB[pi], constB], [rB])
                      k.tt('dve', rt[:, 1], ps3[:, :, 32:64], sn, ALU.mult, [pfB[pi], constB], [rB])
                      k.tt('dve', rt[:, 2], ps3[:, :, 32:64], cs, ALU.mult, [pfB[pi], constB], [rB])
                      k.tt('dve', rt[:, 3], ps3[:, :, 0:32], sn, ALU.mult, [pfB[pi], constB], [rB])
                      k.tt('dve', qm[:, :, 0:32], rt[:, 0], rt[:, 1], ALU.subtract, [rB], [qmB])
                      k.tt('dve', qm[:, :, 32:64], rt[:, 2], rt[:, 3], ALU.add, [rB], [qmB])
                      for hl in range(2):
                          k.tr(pb[qi][0:64, hl * 128:(hl + 1) * 128], qm[:, hl, :], ident_b, [qmB, constB], [pbB[qi]])
                      k.copy('act', qrT[0:64, :, tt * 128:(tt + 1) * 128], pb[qi][0:64, 0:256].rearrange('p (h e) -> p h e', h=2), [pbB[qi]], [qrB])
                      pi = 4 + tt % 2; ps = pf[pi]
                      for kc in range(4):
                          k.mm(ps[:, 0:256], ckvnT[:, kc, tt * 128:(tt + 1) * 128], wuv_s[:, kc, h0 * 128:(h0 + 2) * 128], kc == 0, kc == 3, [mwV, ckvB], [pfB[pi]])
                      k.copy('dve', Vb[:, tt, :, 0:128], ps[:, 0:256].rearrange('p (h e) -> p h e', h=2), [pfB[pi]], [VbB])
                  batches = [(hl, c, kt) for hl in range(2) for c in range(4) for kt in range(4 * c + 4)]
                  LA = 2
                  SB = [(pf[0], pfB[0]), (pf[1], pfB[1]), (pbf[1], pbB[1])]

                  def qkB(n):
                      hl, c, kt = batches[n]
                      sps, spB = SB[n % 3]
                      k.mm(sps[:, :], knT[:, hl, kt * 128:(kt + 1) * 128], qnT[:, hl, c * 512:(c + 1) * 512], True, False, [knB, qnB], [spB])
                      k.mm(sps[:, :], krT[:, kt * 128:(kt + 1) * 128], qrT[:, hl, c * 512:(c + 1) * 512], False, True, [krB, qrB], [spB])
                      pe_ = PE_[n % 3]; peB = PEB[n % 3]; pm = PM[n % 3]; pmB = PMB[n % 3]
                      j0_ = max(0, kt - 4 * c)
                      k.act(pe_[:, j0_ * 128:512], sps[:, j0_ * 128:512], AF.Exp, [spB], [peB], scale=192.0 ** -0.5)
                      if kt >= 4 * c:
                          k.tt('pool' if n % 2 else 'dve', pm[:, 0:128], pe_[:, j0_ * 128:(j0_ + 1) * 128], maskC, ALU.mult, [peB, constB], [pmB])

                  def pvB(n):
                      hl, c, kt = batches[n]
                      h = h0 + hl
                      pe_ = PE_[n % 3]; peB = PEB[n % 3]; pm = PM[n % 3]; pmB = PMB[n % 3]
                      for j in range(4):
                          qt = 4 * c + j
                          if kt > qt:
                              continue
                          ops_ = pf[2 + j]; opB = pfB[2 + j]
                          if kt == qt:
                              k.mm(ops_[:, 0:130], pm[:, 0:128], Vb[:, kt, hl, :], kt == 0, True, [pmB, VbB], [opB])
                              fi = j % 2
                              k.recip(RL[fi][:, 0:1], ops_[:, 128:129], [opB], [RLB[fi]])
                              k.ts('dve', OBF[fi], ops_[:, 0:128], RL[fi][:, 0:1], None, ALU.mult, None, [opB, RLB[fi]], [OBFB[fi]])
                              k.act(JK[:, 0:128], ops_[:, 0:128], AF.Square, [opB, RLB[fi]], [JKB, ssqBB], scale=RL[fi][:, 0:1], accum_out=ssqB_[:, qt, h:h + 1])
                              k.tr(pb[0][:, fi * 128:(fi + 1) * 128], OBF[fi], ident_b, [OBFB[fi], constB], [pbB[0]])
                              k.act(oTb[:, h, qt * 128:(qt + 1) * 128], pb[0][:, fi * 128:(fi + 1) * 128], AF.Identity, [pbB[0], constB], [oTbB], scale=gob[:, h:h + 1])
                          else:
                              k.mm(ops_[:, 0:130], pe_[:, j * 128:(j + 1) * 128], Vb[:, kt, hl, :], kt == 0, False, [peB, VbB], [opB])

                  for n in range(len(batches) + LA):
                      if n < len(batches):
                          qkB(n)
                      if n - LA >= 0:
                          pvB(n - LA)
              sA = A.alloc([16]); sAn = A.alloc([16]); sBn = A.alloc([16]); sB_ = B('ssum')
              k.S.op('dve', (lambda o, i: (lambda e: e.tensor_reduce(out=o, in_=i, axis=AX.X, op=ALU.add)))(sA, ssqA), [ssqAB], [sB_])
              k.S.op('dve', (lambda o, i: (lambda e: e.tensor_reduce(out=o, in_=i, axis=AX.X, op=ALU.add)))(sBn, ssqB_), [ssqBB], [sB_])
              ssB = B('ssqscr')
              k.dma('sp', ssqscr[b].rearrange('(i r) -> i r', r=16), sA, [sB_], [ssB], sB_)
              k.dma('sp', sAn, ssqscr[b].rearrange('(t p) -> p t', p=128), [ssB], [sB_], sB_, slow=True)
              k.act(rab[:, 0, :], sAn, AF.Sqrt, [sB_], [rabB], scale=1.0 / 1024, bias=1e-6)
              k.act(rab[:, 1, :], sBn, AF.Sqrt, [sB_], [rabB], scale=1.0 / 1024, bias=1e-6)
              k.recip(rab.rearrange('p a b -> p (a b)'), rab.rearrange('p a b -> p (a b)'), [rabB], [rabB])
              if stop == 'S6':
                  S.barrier()
                  A.top = SMALL_END
                  dt_ = A.alloc([2048]); dB = B('dbg')
                  for kc in range(16):
                      k.copy('dve', dt_, (oTa if kc < 8 else oTb)[:, kc % 8, :], [oTaB, oTbB], [dB])
                      k.dma('sp', dbg[:, kc, :], dt_, [dB], [], dB)
                  break
              S.barrier()

              A.top = R1 + 8192
              wo_s = A.alloc([16, 2048], BF16); woB = [B('wo') for _ in range(4)]
              for q4 in range(4):
                  k.dma('pool', wo_s[:, q4 * 4:(q4 + 1) * 4, :], w_o[:, q4 * 4:(q4 + 1) * 4, :], [], [woB[q4]], woB[q4])
              G1 = A.alloc([2048]); L1G = A.alloc([2048]); L1B = A.alloc([2048]); G1B = B('g1'); L1GB = B('l1g'); L1BB = B('l1b')
              k.dma('sp', G1, modrow[b, 0:1, :].to_broadcast([128, 2048]), [mrB], [G1B], G1B)
              k.dma('sp', L1G, ln1_g.to_broadcast([128, 2048]), [], [L1GB], L1GB)
              k.dma('sp', L1B, ln1_b.to_broadcast([128, 2048]), [], [L1BB], L1BB)
              XT = [A.alloc([2048]) for _ in range(2)]; XTB = [B('xt') for _ in range(2)]
              YT = [A.alloc([2048]) for _ in range(2)]; YTB = [B('yt') for _ in range(2)]
              TM = [A.alloc([512]) for _ in range(2)]; TMB = [B('tm') for _ in range(2)]
              STt = [A.alloc([4, 6]) for _ in range(2)]; MV = [A.alloc([8]) for _ in range(2)]; STB = [B('st') for _ in range(2)]
              x1B = B('x1scr')
              ti = 0
              for tt in range(16):
                  xt = XT[tt % 2]; xtB = XTB[tt % 2]; yt = YT[tt % 2]; ytB = YTB[tt % 2]
                  st = STt[tt % 2]; mv = MV[tt % 2]; stB = STB[tt % 2]
                  k.dma('sp', xt, x[b, tt * 128:(tt + 1) * 128, :], [], [xtB], xtB)
                  for nb in range(4):
                      pa = pf[(nb % 2) * 2]; paB = pfB[(nb % 2) * 2]; pb_ = pf[(nb % 2) * 2 + 1]; pbB_ = pfB[(nb % 2) * 2 + 1]
                      for fc in range(8):
                          k.mm(pa[:, :], oTa[:, fc, tt * 128:(tt + 1) * 128], wo_s[:, fc, nb * 512:(nb + 1) * 512], fc == 0, fc == 7, [oTaB, woB[fc // 4]], [paB])
                      for fc in range(8):
                          k.mm(pb_[:, :], oTb[:, fc, tt * 128:(tt + 1) * 128], wo_s[:, 8 + fc, nb * 512:(nb + 1) * 512], fc == 0, fc == 7, [oTbB, woB[2 + fc // 4]], [pbB_])
                      tm = TM[ti % 2]; tmB = TMB[ti % 2]; ti += 1
                      sl = slice(nb * 512, (nb + 1) * 512)
                      k.act(tm, pa[:, :], AF.Identity, [paB, rabB], [tmB], scale=rab[:, 0, tt:tt + 1])
                      k.stt(tm, pb_[:, :], rab[:, 1, tt:tt + 1], tm, ALU.mult, ALU.add, [pbB_, rabB, tmB], [tmB])
                      k.tt('dve', tm, tm, G1[:, sl], ALU.mult, [tmB, G1B], [tmB])
                      k.stt(yt[:, sl], xt[:, sl], ALPHA, tm, ALU.mult, ALU.add, [xtB, tmB], [ytB])
                      S.op('dve', (lambda o, i: (lambda e: e.bn_stats(out=o, in_=i)))(st[:, nb, :], yt[:, sl]), [ytB], [stB])
                  S.op('dve', (lambda o, i: (lambda e: e.bn_aggr(out=o, in_=i)))(mv[:, 0:2], st.rearrange('p a b -> p (a b)')), [stB], [stB])
                  k.act(mv[:, 2:3], mv[:, 1:2], AF.Sqrt, [stB], [stB], bias=1e-5)
                  k.recip(mv[:, 3:4], mv[:, 2:3], [stB], [stB])
                  k.ts('dve', mv[:, 4:5], mv[:, 0:1], mv[:, 3:4], -1.0, ALU.mult, ALU.mult, [stB], [stB])
                  k.act(yt, yt, AF.Identity, [ytB, stB], [ytB], scale=mv[:, 3:4], bias=mv[:, 4:5])
                  k.tt('dve', yt, yt, L1G, ALU.mult, [ytB, L1GB], [ytB])
                  k.tt('dve', yt, yt, L1B, ALU.add, [ytB, L1BB], [ytB])
                  k.dma('sp', x1scr[b, tt * 128:(tt + 1) * 128, :], yt, [ytB], [x1B], ytB)
              if stop == 'S7':
                  break
              S.barrier()

              A.top = P_END
              BC = [A.alloc([2048]) for _ in range(5)]; BCB = [B('bc2') for _ in range(5)]
              k.dma('sp', BC[0], modrow[b, 2:3, :].to_broadcast([128, 2048]), [mrB], [BCB[0]], BCB[0])
              k.dma('sp', BC[1], modrow[b, 1:2, :].to_broadcast([128, 2048]), [mrB], [BCB[1]], BCB[1])
              k.dma('sp', BC[2], modrow[b, 3:4, :].to_broadcast([128, 2048]), [mrB], [BCB[2]], BCB[2])
              k.dma('sp', BC[3], ln2_g.to_broadcast([128, 2048]), [], [BCB[3]], BCB[3])
              k.dma('sp', BC[4], ln2_b.to_broadcast([128, 2048]), [], [BCB[4]], BCB[4])
              X1 = [A.alloc([2048]) for _ in range(2)]; X1B = [B('x1') for _ in range(2)]
              ACC = A.alloc([2048]); ACCB = B('acc')
              H2 = ACC; H2B = ACCB
              H2b = [A.alloc([2048], BF16) for _ in range(2)]; H2bB = [B('h2b') for _ in range(2)]
              H2T = A.alloc([16, 128]); H2TB = B('h2T')
              WPQ = [A.alloc([16, 128]) for _ in range(2)]; WPQB = [B('wpq') for _ in range(2)]
              QTC = [A.alloc([128]) for _ in range(2)]; QTCB = [B('qtc') for _ in range(2)]
              SC = A.alloc([16, 128]); SCB = B('sc')
              WK = [A.alloc([128]) for _ in range(2)]; WKB = [B('wk') for _ in range(2)]
              TV = A.alloc([16, 16]); TVB = B('tv')
              TI = A.alloc([16, 16], U32); TIF = A.alloc([16, 16]); TIB = B('ti')
              CAND = A.alloc([8, 256]); CANDB = B('cand')
              WK2 = A.alloc([256]); WK2B = B('wk2')
              VALS = A.alloc([8, 16]); VALSB = B('vals')
              POS = A.alloc([8, 16], U32); POSB = B('pos')
              ABU = A.alloc([2, 128], U32); ABF = A.alloc([2, 8, 16]); ABB = B('ab')
              OH = CAND.rearrange('p h (a b) -> p h a b', a=16); OHB = CANDB
              SEL = A.alloc([2, 8, 16]); SELB = B('sel')
              EIDX = A.alloc([128]); EB0 = B('eidxf')
              EIDXU = [A.alloc([128], U32) for _ in range(2)]; EB = [B('eidx') for _ in range(2)]
              GT2 = [A.alloc([128]) for _ in range(2)]; GT2B = [B('gates') for _ in range(2)]
              GS = A.alloc([24]); GSB = B('gs')
              AA = A.alloc([128]); AAB = B('aa')
              WW = A.alloc([128]); WWB = B('ww')
              NGB = 7
              GB_ = [A.alloc([4096], BF16) for _ in range(NGB)]; GBB = [B('gb') for _ in range(NGB)]
              TG = A.alloc([128])
              DG = [A.alloc([128], BF16) for _ in range(4)]; DGB = [B('dg') for _ in range(4)]
              if b == 0:
                  print('S8 arena top', A.top, 'of', A.n)
              STt = [A.alloc([4, 6]) for _ in range(2)]; MV = [A.alloc([8]) for _ in range(2)]; STB = [B('st') for _ in range(2)]
              ST2 = A.alloc([4, 6]); MV2 = A.alloc([8]); ST2B = B('st2')
              outB = B('out')
              cnt = {'wq': 0, 'gi': 0}

              def ln_stats(src, srcB, st, mv, stB):
                  for c4 in range(4):
                      S.op('dve', (lambda o, i: (lambda e: e.bn_stats(out=o, in_=i)))(st[:, c4, :], src[:, c4 * 512:(c4 + 1) * 512]), [srcB], [stB])
                  S.op('dve', (lambda o, i: (lambda e: e.bn_aggr(out=o, in_=i)))(mv[:, 0:2], st.rearrange('p a b -> p (a b)')), [stB], [stB])
                  k.act(mv[:, 2:3], mv[:, 1:2], AF.Sqrt, [stB], [stB], bias=1e-5)
                  k.recip(mv[:, 3:4], mv[:, 2:3], [stB], [stB])
                  k.ts('dve', mv[:, 4:5], mv[:, 0:1], mv[:, 3:4], -1.0, ALU.mult, ALU.mult, [stB], [stB])

              def stageA(tt):
                  x1 = X1[tt % 2]; x1B_ = X1B[tt % 2]; st = STt[tt % 2]; mv = MV[tt % 2]; stB = STB[tt % 2]
                  h2b = H2b[tt % 2]; h2bB = H2bB[tt % 2]
                  k.dma('sp', x1, x1scr[b, tt * 128:(tt + 1) * 128, :], [x1B], [x1B_], x1B_)
                  ln_stats(x1, x1B_, st, mv, stB)
                  k.act(H2, x1, AF.Identity, [x1B_, stB], [H2B], scale=mv[:, 3:4], bias=mv[:, 4:5])
                  k.tt('dve', H2, H2, BC[0], ALU.mult, [H2B, BCB[0]], [H2B])
                  k.tt('pool', H2, H2, BC[1], ALU.add, [H2B, BCB[1]], [H2B])
                  k.copy('act', h2b, H2, [H2B], [h2bB])
                  for g4 in range(4):
                      pi = g4 % 2
                      for j in range(4):
                          kc = g4 * 4 + j
                          k.tr(pf[pi][:, j * 128:(j + 1) * 128], H2[:, kc * 128:(kc + 1) * 128], ident_f, [H2B, constB], [pfB[pi]])
                      k.copy('act', H2T[:, g4 * 4:(g4 + 1) * 4, :], pf[pi][:, :].rearrange('p (a b) -> p a b', a=4), [pfB[pi]], [H2TB])
                  for c in range(16):
                      wp = WPQ[cnt['wq'] % 2]; wpB = WPQB[cnt['wq'] % 2]; cnt['wq'] += 1
                      k.dma('sp', wp, w_pq[c], [], [wpB], wpB)
                      pi = c % 2
                      for kc in range(16):
                          k.mm(pf[pi][:, 0:128], wp[:, kc, :], H2T[:, kc, :], kc == 0, kc == 15, [wpB, H2TB], [pfB[pi]])
                      qc = QTC[c % 2]; qcB = QTCB[c % 2]
                      k.copy('act', qc, pf[pi][:, 0:128], [pfB[pi]], [qcB])
                      si = (c // 4) % 2
                      k.mm(pbf[si][:, (c % 4) * 128:(c % 4 + 1) * 128], qc, skT_s[:, c % 2, :], True, True, [qcB, constB], [pbB[si]])
                      if c % 4 == 3:
                          k.copy('act', SC[:, c - 3:c + 1, :], pbf[si][:, :].rearrange('p (a b) -> p a b', a=4), [pbB[si]], [SCB])

              def stageT(tt):
                  eu = EIDXU[tt % 2]; eB = EB[tt % 2]; GT = GT2[tt % 2]; GTB = GT2B[tt % 2]
                  for c in range(16):
                      wk = WK[c % 2]; wkB = WKB[c % 2]
                      S.op('dve', (lambda o, i: (lambda e: e.max(out=o, in_=i)))(TV[:, c, 0:8], SC[:, c, :]), [SCB], [TVB])
                      yield
                      S.op('dve', (lambda o, m, i: (lambda e: e.max_index(out=o, in_max=m, in_values=i)))(TI[:, c, 0:8], TV[:, c, 0:8], SC[:, c, :]), [SCB, TVB], [TIB])
                      yield
                      S.op('dve', (lambda o, m, i: (lambda e: e.match_replace(out=o, in_to_replace=m, in_values=i, imm_value=NEG)))(wk, TV[:, c, 0:8], SC[:, c, :]), [SCB, TVB], [wkB])
                      yield
                      S.op('dve', (lambda o, i: (lambda e: e.max(out=o, in_=i)))(TV[:, c, 8:16], wk), [wkB], [TVB])
                      yield
                      S.op('dve', (lambda o, m, i: (lambda e: e.max_index(out=o, in_max=m, in_values=i)))(TI[:, c, 8:16], TV[:, c, 8:16], wk), [wkB, TVB], [TIB])
                      yield
                  k.copy('dve', TIF, TI, [TIB], [TIB])
                  yield
                  tv4 = TV.rearrange('p (h t) k -> p h t k', t=2); ti4 = TIF.rearrange('p (h t) k -> p h t k', t=2)
                  k.tt('dve', CAND.rearrange('p h (a b) -> p h a b', a=16), bc(tv4[:, :, 0, :], [128, 8, 16, 16], 3), bc(tv4[:, :, 1, :], [128, 8, 16, 16], 2),
                       ALU.add, [TVB], [CANDB])
                  yield
                  for hh in range(8):
                      S.op('dve', (lambda o, i: (lambda e: e.max(out=o, in_=i)))(VALS[:, hh, 0:8], CAND[:, hh, :]), [CANDB], [VALSB])
                      yield
                      S.op('dve', (lambda o, m, i: (lambda e: e.max_index(out=o, in_max=m, in_values=i)))(POS[:, hh, 0:8], VALS[:, hh, 0:8], CAND[:, hh, :]), [CANDB, VALSB], [POSB])
                      yield
                      S.op('dve', (lambda o, m, i: (lambda e: e.match_replace(out=o, in_to_replace=m, in_values=i, imm_value=NEG)))(WK2, VALS[:, hh, 0:8], CAND[:, hh, :]), [CANDB, VALSB], [WK2B])
                      yield
                      S.op('dve', (lambda o, i: (lambda e: e.max(out=o, in_=i)))(VALS[:, hh, 8:16], WK2), [WK2B], [VALSB])
                      yield
                      S.op('dve', (lambda o, m, i: (lambda e: e.max_index(out=o, in_max=m, in_values=i)))(POS[:, hh, 8:16], VALS[:, hh, 8:16], WK2), [WK2B, VALSB], [POSB])
                      yield
                  posf = POS.rearrange('p h k -> p (h k)')
                  S.op('dve', (lambda o, i: (lambda e: e.tensor_single_scalar(out=o, in_=i, scalar=4, op=ALU.logical_shift_right)))(ABU[:, 0, :], posf), [POSB], [ABB])
                  yield
                  S.op('dve', (lambda o, i: (lambda e: e.tensor_single_scalar(out=o, in_=i, scalar=15, op=ALU.bitwise_and)))(ABU[:, 1, :], posf), [POSB], [ABB])
                  yield
                  k.copy('dve', ABF.rearrange('p t h k -> p (t h k)'), ABU.rearrange('p t n -> p (t n)'), [ABB], [ABB])
                  yield
                  io16 = iota[:, 0:16].unsqueeze(1).unsqueeze(1).to_broadcast([128, 8, 16, 16])
                  for t2 in range(2):
                      k.tt('dve', OH, io16, bc(ABF[:, t2], [128, 8, 16, 16], 3), ALU.is_equal, [ABB, constB], [OHB])
                      yield
                      k.tt('dve', OH, OH, bc(ti4[:, :, t2, :], [128, 8, 16, 16], 2), ALU.mult, [OHB, TIB], [OHB])
                      yield
                      S.op('dve', (lambda o, i: (lambda e: e.tensor_reduce(out=o, in_=i, axis=AX.X, op=ALU.add)))(SEL[:, t2], OH), [OHB], [SELB])
                      yield
                  k.stt(EIDX.rearrange('p (h k) -> p h k', h=8), SEL[:, 0], 128.0, SEL[:, 1], ALU.mult, ALU.add, [SELB], [EB0])
                  yield
                  k.copy('dve', eu, EIDX, [EB0], [eB])
                  yield
                  k.S.op('dve', (lambda o, i: (lambda e: e.tensor_reduce(out=o, in_=i, axis=AX.X, op=ALU.max)))(GS[:, 0:8], VALS), [VALSB], [GSB])
                  yield
                  k.ts('dve', GS[:, 8:16], GS[:, 0:8], -1.0, None, ALU.mult, None, [GSB], [GSB])
                  yield
                  for hh in range(8):
                      k.act(GT[:, hh * 16:(hh + 1) * 16], VALS[:, hh, :], AF.Exp, [VALSB, GSB], [GTB, GSB], bias=GS[:, 8 + hh:9 + hh], accum_out=GS[:, 16 + hh:17 + hh])
                      yield
                  k.recip(GS[:, 0:8], GS[:, 16:24], [GSB], [GSB])
                  yield
                  k.tt('dve', GT.rearrange('p (h k) -> p h k', h=8), GT.rearrange('p (h k) -> p h k', h=8), bc(GS[:, 0:8], [128, 8, 16], 2), ALU.mult, [GTB, GSB], [GTB])
                  yield

              def capture(stage, *args):
                  lst = []
                  real = S.op
                  S.op = lambda eng, fn, reads=(), writes=(), dma=None, ndma=1: lst.append((eng, fn, list(reads), list(writes), dma, ndma))
                  try:
                      r = stage(*args)
                      if r is not None:
                          for _ in r:
                              pass
                  finally:
                      S.op = real
                  return lst

              def replay(lst, n):
                  for _ in range(min(n, len(lst))):
                      eng, fn, R, W, dma, ndma = lst.pop(0)
                      S.op(eng, fn, R, W, dma=dma, ndma=ndma)

              def stageUV(tt, aops, tops):
                  eu = EIDXU[tt % 2]; eB = EB[tt % 2]; h2b = H2b[tt % 2]; h2bB = H2bB[tt % 2]; GT = GT2[tt % 2]; GTB = GT2B[tt % 2]
                  for kk in range(128):
                      gb = GB_[cnt['gi'] % NGB]; gbB = GBB[cnt['gi'] % NGB]; cnt['gi'] += 1
                      aB = Buf('aa'); wB = Buf('ww')
                      k.gather(gb, uvb, eu[:, kk:kk + 1], [eB, tabB], [gbB], gbB)
                      k.stt(gb[:, 0:2048], gb[:, 0:2048], 1.0, h2b, ALU.mult, ALU.mult, [gbB, h2bB], [aB, gbB], accum_out=AA[:, kk:kk + 1])
                      k.act(TG[:, kk:kk + 1], AA[:, kk:kk + 1], AF.Gelu, [aB], [wB])
                      k.act(WW[:, kk:kk + 1], TG[:, kk:kk + 1], AF.Identity, [wB, GTB], [wB], scale=GT[:, kk:kk + 1])
                      dg = DG[kk % 4]; dgB = DGB[kk % 4]
                      k.act(dg, ident_f, AF.Identity, [wB, constB], [dgB], scale=WW[:, kk:kk + 1])
                      for nb in range(4):
                          k.mm(pf[2 + nb][:, :], dg, gb[:, 2048 + nb * 512:2048 + (nb + 1) * 512], kk == 0, kk == 127, [dgB, gbB], [pfB[2 + nb]])
                      if kk < 40:
                          replay(aops, (len(aops) + 39 - kk) // (40 - kk))
                      else:
                          replay(aops, len(aops))
                          replay(tops, (len(tops) + 119 - kk) // max(1, 120 - kk) if kk < 120 else len(tops))
                  replay(aops, len(aops)); replay(tops, len(tops))

              def stageF(tt):
                  x1 = X1[tt % 2]; x1B_ = X1B[tt % 2]
                  for nb in range(4):
                      sl = slice(nb * 512, (nb + 1) * 512)
                      k.tt('dve', ACC[:, sl], pf[2 + nb][:, :], BC[2][:, sl], ALU.mult, [pfB[2 + nb], BCB[2]], [ACCB])
                  k.stt(ACC, x1, ALPHA, ACC, ALU.mult, ALU.add, [x1B_, ACCB], [ACCB])
                  ln_stats(ACC, ACCB, ST2, MV2, ST2B)
                  k.act(ACC, ACC, AF.Identity, [ACCB, ST2B], [ACCB], scale=MV2[:, 3:4], bias=MV2[:, 4:5])
                  k.tt('pool', ACC, ACC, BC[3], ALU.mult, [ACCB, BCB[3]], [ACCB])
                  k.tt('dve', ACC, ACC, BC[4], ALU.add, [ACCB, BCB[4]], [ACCB])
                  k.dma('sp', out[b, tt * 128:(tt + 1) * 128, :], ACC, [ACCB], [outB], ACCB)

              if peer_tiles > 0:
                  stageA(0)
                  for _ in stageT(0):
                      pass
              for tt in range(peer_tiles):
                  aops, tops = [], []
                  if tt + 1 < peer_tiles:
                      aops = capture(stageA, tt + 1)
                      tops = capture(stageT, tt + 1)
                  stageUV(tt, aops, tops)
                  stageF(tt)
              S.barrier()
        except StopBuild as sb:
            S.barrier()
            A.top = A.n - 2048
            dt_ = A.alloc([2048]); dB = Buf('dbgx')
            for i, (ap, n) in enumerate(sb.items):
                k.copy('dve', dt_[:ap.shape[0], 0:n], ap, [], [dB])
                k.dma('sp', dbg[:ap.shape[0], i, 0:n], dt_[:ap.shape[0], 0:n], [dB], [], dB)
        S.barrier()
        S.emit()
    return nc


def _consts():
    c = {}
    c['ident'] = np.eye(128, dtype=np.float32)
    ip = np.arange(128)[:, None]; i = np.arange(128)[None, :]
    mA = np.zeros((128, 31, 128), np.float32)
    for m in range(31):
        d = m - 15
        dt = 16 * (i - ip) + d
        mult = ((dt >= 0) & (dt <= 128)).astype(np.float32)
        if d % 4 == 0:
            mult += ((dt >= 0) & (dt <= 512))
        if d == 0:
            mult += (dt >= 0)
        mA[:, m, :] = mult
    c['maskA'] = mA
    c['maskC'] = (ip <= i).astype(np.float32)
    theta = 500000.0
    invA = theta ** (-np.arange(16, dtype=np.float64) * 2.0 / 32)
    tA = (16 * np.arange(128)[:, None] + np.arange(16)[None, :]).astype(np.float64)
    angA = tA[:, :, None] * invA[None, None, :]
    c['cosA'] = np.cos(angA).astype(np.float32); c['sinA'] = np.sin(angA).astype(np.float32)
    invB = theta ** (-np.arange(32, dtype=np.float64) * 2.0 / 64)
    tB = (128 * np.arange(16)[None, :] + np.arange(128)[:, None]).astype(np.float64)
    angB = tB[:, :, None] * invB[None, None, :]
    c['cosB'] = np.cos(angB).astype(np.float32); c['sinB'] = np.sin(angB).astype(np.float32)
    c['iota'] = np.broadcast_to(np.arange(256, dtype=np.float32), (128, 256)).copy()
    return c


def _prep_shared(inp):
    f = lambda a: np.ascontiguousarray(a, dtype=np.float32)
    sh = dict(_consts())
    w_ada = inp['w_ada'][0]
    sh['w_ada_r'] = f(w_ada.reshape(16, 128, 24, 512).transpose(2, 1, 0, 3))
    sh['b_adaT'] = f(inp['b_ada'][0].reshape(96, 128).T)
    sh['b_ada_row'] = f(inp['b_ada'][0].reshape(1, 12288))
    w_in = inp['w_in'][0]
    sh['w_in_r'] = f(w_in[:, :4096].reshape(16, 128, 8, 512).transpose(2, 1, 0, 3))
    sh['w_in_kr'] = f(w_in[:, 4096:4160].reshape(16, 128, 64).transpose(1, 0, 2))
    sh['g_qT'] = f(inp['g_q_lat'][0].reshape(4, 128).T); sh['g_kvT'] = f(inp['g_kv_lat'][0].reshape(4, 128).T)
    wuq = inp['w_uq'][0].reshape(4, 128, 8, 192).transpose(1, 0, 2, 3)
    sh['w_uqn'] = f(wuq[..., :128]); sh['w_uqr'] = f(wuq[..., 128:])
    sh['w_uk_r'] = f(inp['w_uk'][0].reshape(4, 128, 1024).transpose(1, 0, 2))
    sh['w_uv_r'] = f(inp['w_uv'][0].reshape(4, 128, 1024).transpose(1, 0, 2))
    sh['g_oaT'] = f(inp['g_out_a'][0].reshape(8, 128).T); sh['g_obT'] = f(inp['g_out_b'][0].reshape(8, 128).T)
    sh['w_o_r'] = f(inp['w_o'][0].reshape(16, 128, 2048).transpose(1, 0, 2))
    for n in ('ln1_g', 'ln1_b', 'ln2_g', 'ln2_b'):
        sh[n] = f(inp[n][0].reshape(1, 2048))
    sh['w_pq_r'] = f(inp['w_pq'][0].reshape(16, 128, 16, 128).transpose(2, 1, 0, 3))
    sh['skT'] = f(np.stack([inp['sub_key_1'][0].T, inp['sub_key_2'][0].T], axis=1))
    sh['u_table'] = f(inp['u_table'][0]); sh['v_table'] = f(inp['v_table'][0])
    return sh


def _core_map(sh, inp, b0, nseq):
    m = dict(sh)
    m['x'] = np.ascontiguousarray(inp['x'][b0:b0 + nseq], dtype=np.float32)
    cc = np.asarray(inp['c'][b0:b0 + nseq], dtype=np.float32)
    if nseq == 1:
        cc = np.concatenate([cc, cc], 0)
    m['cT'] = np.ascontiguousarray(cc.reshape(2, 16, 128).transpose(2, 1, 0))
    return m


_NC_CACHE = {}


def kernel(**inputs):
    inp = {k_: np.asarray(v) for k_, v in inputs.items()}
    nseq = 16 // NCORES
    if 'nc' not in _NC_CACHE:
        _NC_CACHE['nc'] = build(nseq=nseq)
    nc = _NC_CACHE['nc']
    sh = _prep_shared(inp)
    maps = [_core_map(sh, inp, c * nseq, nseq) for c in range(NCORES)]
    res = run_bass_kernel_spmd(nc, maps, core_ids=list(range(NCORES)))
    return np.concatenate([r['out'] for r in res.results], axis=0).astype(np.float32)
```

```python
import numpy as np
from contextlib import ExitStack
import concourse.bass as bass
import concourse.mybir as mybir
from concourse.bass_utils import run_bass_kernel_spmd

F32 = mybir.dt.float32; BF16 = mybir.dt.bfloat16; I32 = mybir.dt.int32; U32 = mybir.dt.uint32
ALU = mybir.AluOpType; AF = mybir.ActivationFunctionType; AX = mybir.AxisListType
SAME_SYNC = True
NCORES = 8
S_ = 2048; D_ = 2048
ALPHA = 2.0 ** 0.25
NEG = -1.0e30


class Buf:
    __slots__ = ('name', 'w', 'r', 'sem', 'semcnt', 'excl')

    def __init__(self, name, excl=False):
        self.name = name; self.w = {}; self.r = {}; self.sem = None; self.semcnt = 0; self.excl = excl


class Sched:
    ENG = ('pe', 'act', 'dve', 'pool', 'sp')

    def __init__(self, nc, ctx):
        self.nc = nc; self.ctx = ctx
        self.ops = {e: [] for e in self.ENG}
        self.esem = {e: ctx.enter_context(nc.semaphore('sem_' + e)) for e in self.ENG}
        self.dbufs = []; self.dset = set()
        self.uid = 0

    def op(self, eng, fn, reads=(), writes=(), dma=None, ndma=1):
        ops = self.ops[eng]
        idx = len(ops)
        deps = []
        for b in reads:
            deps.extend(b.w.values())
            if b.excl:
                deps.extend(v for kk, v in b.r.items() if kk != eng)
        for b in writes:
            deps.extend(b.w.values()); deps.extend(b.r.values())
        waits = set()
        for ev in deps:
            if ev[0] == 'E':
                e2 = ev[1]
                if e2 == eng and dma is None and (eng == 'pe' or not SAME_SYNC):
                    continue
                self.ops[e2][ev[2]]['inc'] = True
            waits.add(ev)
        if dma is not None:
            if dma.sem is None:
                dma.sem = self.ctx.enter_context(self.nc.semaphore('ds%d' % len(self.dbufs)))
            if id(dma) not in self.dset:
                self.dset.add(id(dma)); self.dbufs.append(dma)
            dma.semcnt += 16 * ndma
            ev = ('D', dma, dma.semcnt)
            self.uid += 1
            key = ('dma', self.uid)
        else:
            ev = ('E', eng, idx)
            key = eng
        ops.append(dict(fn=fn, waits=waits, inc=False, dma=dma))
        for b in reads:
            b.r[key] = ev
        for b in writes:
            b.w = {key: ev}; b.r = {}
        return ev

    def barrier(self):
        last = {}
        for e in self.ENG:
            for i in range(len(self.ops[e]) - 1, -1, -1):
                if self.ops[e][i]['fn'] is not None and self.ops[e][i]['dma'] is None:
                    last[e] = ('E', e, i); self.ops[e][i]['inc'] = True
                    break
        dmaev = [('D', b, b.semcnt) for b in self.dbufs if b.semcnt > 0]
        for e in self.ENG:
            waits = set(v for k, v in last.items() if (k != e or e != 'pe')) | set(dmaev)
            self.ops[e].append(dict(fn=None, waits=waits, inc=False, dma=None))

    def emit(self):
        nc = self.nc
        seq = {}
        for e in self.ENG:
            c = 0
            for i, o in enumerate(self.ops[e]):
                if o['inc']:
                    c += 1; seq[(e, i)] = c

        def run(eng, e):
            seen = {}
            for i, o in enumerate(self.ops[e]):
                for ev in o['waits']:
                    if ev[0] == 'E':
                        sem = self.esem[ev[1]]; val = seq[(ev[1], ev[2])]; key = ev[1]
                    else:
                        sem = ev[1].sem; val = ev[2]; key = id(ev[1])
                    if seen.get(key, 0) >= val:
                        continue
                    eng.wait_ge(sem, val); seen[key] = val
                if o['fn'] is None:
                    continue
                r = o['fn'](eng)
                if o['dma'] is not None:
                    for ins in (r if isinstance(r, (list, tuple)) else [r]):
                        ins.then_inc(o['dma'].sem, 16)
                elif o['inc']:
                    r.then_inc(self.esem[e], 1)

        with nc.Block() as block:
            block.sync(lambda eng: run(eng, 'sp'))
            block.scalar(lambda eng: run(eng, 'act'))
            block.vector(lambda eng: run(eng, 'dve'))
            block.gpsimd(lambda eng: run(eng, 'pool'))
            block.tensor(lambda eng: run(eng, 'pe'))


class Arena:
    def __init__(self, nc, ctx, nwords):
        self.t = ctx.enter_context(nc.sbuf_tensor('arena', [128, nwords], F32))
        self.n = nwords; self.top = 0

    def alloc(self, shape, dtype=F32):
        n = int(np.prod(shape))
        per = 2 if dtype == BF16 else 1
        words = (n + per - 1) // per
        words = (words + 1) // 2 * 2
        assert self.top + words <= self.n, ('arena OOM', self.top, words, self.n)
        ap = self.t[:, self.top:self.top + words]
        self.top += words
        if dtype != F32:
            ap = ap.bitcast(dtype)
        if dtype == BF16 and n != words * 2:
            ap = ap[:, 0:n]
        if len(shape) > 1:
            names = ' '.join('d%d' % i for i in range(len(shape)))
            ap = ap.rearrange('p (%s) -> p %s' % (names, names), **{'d%d' % i: s for i, s in enumerate(shape)})
        return ap


class K:
    def __init__(self, S):
        self.S = S

    def mm(self, out, lhsT, rhs, start, stop, R, W):
        self.S.op('pe', lambda e: e.matmul(out, lhsT=lhsT, rhs=rhs, start=start, stop=stop), R, W)

    def tr(self, out, in_, ident, R, W):
        self.S.op('pe', lambda e: e.transpose(out=out, in_=in_, identity=ident), R, W)

    def act(self, out, in_, func, R, W, scale=1.0, bias=0.0, accum_out=None):
        if accum_out is None:
            self.S.op('act', lambda e: e.activation(out=out, in_=in_, func=func, bias=bias, scale=scale), R, W)
        else:
            self.S.op('act', lambda e: e.activation(out=out, in_=in_, func=func, bias=bias, scale=scale,
                                                    accum_out=accum_out), R, W)

    def copy(self, eng, out, in_, R, W):
        if eng == 'act':
            self.S.op('act', lambda e: e.activation(out=out, in_=in_, func=AF.Copy), R, W)
        else:
            self.S.op(eng, lambda e: e.tensor_copy(out=out, in_=in_), R, W)

    def tt(self, eng, out, in0, in1, op, R, W):
        self.S.op(eng, lambda e: e.tensor_tensor(out=out, in0=in0, in1=in1, op=op), R, W)

    def ts(self, eng, out, in0, s1, s2, op0, op1, R, W, accum_out=None):
        if op1 is None:
            self.S.op(eng, lambda e: e.tensor_scalar(out=out, in0=in0, scalar1=s1, scalar2=None, op0=op0), R, W)
        elif accum_out is None:
            self.S.op(eng, lambda e: e.tensor_scalar(out=out, in0=in0, scalar1=s1, scalar2=s2, op0=op0, op1=op1), R, W)
        else:
            self.S.op(eng, lambda e: e.tensor_scalar(out=out, in0=in0, scalar1=s1, scalar2=s2, op0=op0, op1=op1,
                                                     accum_out=accum_out), R, W)

    def stt(self, out, in0, scalar, in1, op0, op1, R, W, accum_out=None):
        if accum_out is None:
            self.S.op('dve', lambda e: e.scalar_tensor_tensor(out=out, in0=in0, scalar=scalar, in1=in1, op0=op0, op1=op1), R, W)
        else:
            self.S.op('dve', lambda e: e.scalar_tensor_tensor(out=out, in0=in0, scalar=scalar, in1=in1, op0=op0, op1=op1,
                                                              accum_out=accum_out), R, W)

    def recip(self, out, in_, R, W):
        self.S.op('dve', lambda e: e.reciprocal(out=out, in_=in_), R, W)

    def memset(self, eng, ap, val, R, W):
        self.S.op(eng, lambda e: e.memset(ap, val), R, W)

    def dma(self, eng, out, in_, R, W, sem, slow=False):
        if slow:
            self.S.op(eng, lambda e: e.dma_start(out=out, in_=in_, allow_slow_non_contiguous=True), R, W, dma=sem)
        else:
            self.S.op(eng, lambda e: e.dma_start(out=out, in_=in_), R, W, dma=sem)

    def gather(self, out, table, idx_ap, R, W, sem):
        self.S.op('pool', lambda e: e.indirect_dma_start(
            out=out, out_offset=None, in_=table,
            in_offset=bass.IndirectOffsetOnAxis(ap=idx_ap, axis=0)), R, W, dma=sem)


class StopBuild(Exception):
    def __init__(self, items):
        self.items = items


def bc(ap, shape, axis):
    return ap.unsqueeze(axis).to_broadcast(shape)


def build(nseq=2, stop=None, peer_tiles=16, ntab=16384):
    nc = bass.Bass("TRN2", target_bir_lowering=False)

    def DI(name, shape, dt=F32):
        return nc.dram_tensor(name, shape, dt, kind="ExternalInput").ap()

    x = DI('x', [nseq, S_, D_])
    cT = DI('cT', [128, 16, 2])
    w_ada = DI('w_ada_r', [24, 128, 16, 512])
    b_adaT = DI('b_adaT', [128, 96])
    b_ada_row = DI('b_ada_row', [1, 12288])
    w_in = DI('w_in_r', [8, 128, 16, 512])
    w_in_kr = DI('w_in_kr', [128, 16, 64])
    g_qT = DI('g_qT', [128, 4]); g_kvT = DI('g_kvT', [128, 4])
    w_uqn = DI('w_uqn', [128, 4, 8, 128]); w_uqr = DI('w_uqr', [128, 4, 8, 64])
    w_uk = DI('w_uk_r', [128, 4, 1024]); w_uv = DI('w_uv_r', [128, 4, 1024])
    g_oaT = DI('g_oaT', [128, 8]); g_obT = DI('g_obT', [128, 8])
    w_o = DI('w_o_r', [128, 16, 2048])
    ln1_g = DI('ln1_g', [1, D_]); ln1_b = DI('ln1_b', [1, D_]); ln2_g = DI('ln2_g', [1, D_]); ln2_b = DI('ln2_b', [1, D_])
    w_pq = DI('w_pq_r', [16, 128, 16, 128])
    skT = DI('skT', [128, 2, 128])
    u_table = DI('u_table', [ntab, D_]); v_table = DI('v_table', [ntab, D_])
    ident_d = DI('ident', [128, 128])
    maskA_d = DI('maskA', [128, 31, 128]); maskC_d = DI('maskC', [128, 128])
    cosA_d = DI('cosA', [128, 16, 16]); sinA_d = DI('sinA', [128, 16, 16])
    cosB_d = DI('cosB', [128, 16, 32]); sinB_d = DI('sinB', [128, 16, 32])
    iota_d = DI('iota', [128, 256])
    out = nc.dram_tensor('out', [nseq, S_, D_], F32, kind="ExternalOutput").ap()
    dbgk = "ExternalOutput" if stop else "Internal"
    modrow = nc.dram_tensor('modrow', [nseq, 4, D_], F32, kind="Internal").ap()
    ssqscr = nc.dram_tensor('ssqscr', [nseq, S_], F32, kind="Internal").ap()
    x1scr = nc.dram_tensor('x1scr', [nseq, S_, D_], F32, kind=dbgk).ap()
    uvb = nc.dram_tensor('uvb', [ntab, 2 * D_], BF16, kind="Internal").ap()
    dbg = nc.dram_tensor('dbg', [128, 16, 2048], F32, kind=dbgk).ap() if stop else None

    ctx = ExitStack()
    with ctx:
        S = Sched(nc, ctx)
        k = K(S)
        A = Arena(nc, ctx, 53000)
        A.n = 53000 if not stop else 53000 - 0
        pf = [ctx.enter_context(nc.psum_tensor('pf%d' % i, [128, 512], F32)) for i in range(6)]
        pbf = [ctx.enter_context(nc.psum_tensor('pb%d' % i, [128, 512], F32)) for i in range(2)]
        pb = [t[:, :].bitcast(BF16) for t in pbf]
        pfB = [Buf('pf%d' % i, True) for i in range(6)]
        pbB = [Buf('pb%d' % i, True) for i in range(2)]
        nb_ = [0]

        bcnt = {}; bprev = {}

        def B(name='b'):
            nb_[0] += 1
            bcnt[name] = bcnt.get(name, 0) + 1
            key = (name, bcnt[name])
            nb = Buf('%s%d' % (name, nb_[0]))
            if key in bprev:
                nb.sem = bprev[key].sem; nb.semcnt = bprev[key].semcnt
            bprev[key] = nb
            return nb

        ident_f = A.alloc([128]); ident_b = A.alloc([128], BF16)
        maskA = A.alloc([31, 128], BF16); maskC = A.alloc([128], BF16)
        cosA = A.alloc([16, 16]); sinA = A.alloc([16, 16]); cosB = A.alloc([16, 32]); sinB = A.alloc([16, 32])
        sh1T = A.alloc([16, 2]); sc1T = A.alloc([16, 2])
        gq = A.alloc([4]); gkv = A.alloc([4]); goa = A.alloc([8]); gob = A.alloc([8])
        skT_s = A.alloc([2, 128])
        iota = A.alloc([256])
        constB = B('const')
        def cdma(eng, dst, src):
            cb = B('c'); k.dma(eng, dst, src, [], [cb], cb)
        cdma('sp', ident_f, ident_d)
        cdma('pool', ident_b, ident_d)
        cdma('pool', maskA, maskA_d)
        cdma('pool', maskC, maskC_d)
        for dst, src in ((cosA, cosA_d), (sinA, sinA_d), (cosB, cosB_d), (sinB, sinB_d), (gq, g_qT), (gkv, g_kvT),
                         (goa, g_oaT), (gob, g_obT), (skT_s, skT), (iota, iota_d)):
            cdma('sp', dst, src)
        P_END = A.top

        cact = A.alloc([16, 2]); badaT = A.alloc([96]); brow = A.alloc([12288]); rowt = [A.alloc([512]) for _ in range(2)]
        wblk0 = [A.alloc([16, 512]) for _ in range(2)]
        s0B = B('s0'); wB0 = [B('wada') for _ in range(2)]; rowB = [B('rowt') for _ in range(2)]; modB = B('modT'); mrB = B('modrow')
        s0b2 = B('s0b'); s0b3 = B('s0c')
        k.dma('sp', cact, cT, [], [s0B], s0B)
        k.dma('sp', badaT, b_adaT, [], [s0b2], s0b2)
        k.dma('sp', brow[0:1, :], b_ada_row, [], [s0b3], s0b3)
        k.act(cact, cact, AF.Silu, [s0B], [s0B])
        CB = [A.alloc([4096], BF16) for _ in range(3)]; CBB = [B('cb') for _ in range(3)]; CSB = [B('cs') for _ in range(3)]
        tabB = B('tab')
        ci_ = 0
        for src_t, off_t in ((u_table, 0), (v_table, D_)):
            for ci in range(ntab // 256):
                cb = CB[ci_ % 3]; cbB = CBB[ci_ % 3]; csB = CSB[ci_ % 3]; ci_ += 1
                k.dma('pool', cb, src_t[ci * 256:(ci + 1) * 256, :].rearrange('(p a) d -> p (a d)', a=2), [], [cbB], cbB)
                k.dma('act', uvb[ci * 256:(ci + 1) * 256, off_t:off_t + D_].rearrange('(p a) d -> p a d', a=2),
                      cb.rearrange('p (a d) -> p a d', a=2), [cbB], [tabB], csB)
        ri = 0
        for blk in range(24):
            wb = wblk0[blk % 2]; wbB = wB0[blk % 2]
            k.dma('sp', wb, w_ada[blk], [], [wbB], wbB)
            if blk < 8:
                for j in range(4):
                    ch = (blk % 4) * 4 + j
                    ps = pf[j % 2][:, 0:2]
                    for kc in range(16):
                        k.mm(ps, wb[:, kc, j * 128:(j + 1) * 128], cact[:, kc, :], kc == 0, kc == 15, [wbB, s0B], [pfB[j % 2]])
                    if blk < 4:
                        k.ts('dve', sh1T[:, ch, :], ps, badaT[:, blk * 4 + j:blk * 4 + j + 1], None, ALU.add, None, [pfB[j % 2], s0b2], [modB])
                    else:
                        k.ts('dve', sc1T[:, ch, :], ps, badaT[:, blk * 4 + j:blk * 4 + j + 1], 1.0, ALU.add, ALU.add, [pfB[j % 2], s0b2], [modB])
            else:
                slot = (blk - 8) // 4; q4 = (blk - 8) % 4
                for b in range(nseq):
                    pi = 2 + (ri % 2); ps = pf[pi][0:1, :]
                    for kc in range(16):
                        k.mm(ps, cact[:, kc, b:b + 1], wb[:, kc, :], kc == 0, kc == 15, [wbB, s0B], [pfB[pi]])
                    rt = rowt[ri % 2]; rB = rowB[ri % 2]
                    k.tt('dve', rt[0:1, :], ps, brow[0:1, blk * 512:(blk + 1) * 512], ALU.add, [pfB[pi], s0b3], [rB])
                    if slot == 2:
                        k.ts('dve', rt[0:1, :], rt[0:1, :], 1.0, None, ALU.add, None, [rB], [rB])
                    k.dma('sp', modrow[b, slot:slot + 1, q4 * 512:(q4 + 1) * 512], rt[0:1, :], [rB], [mrB], rB)
                    ri += 1
        S.barrier()
        A.top = P_END

        oTa = A.alloc([8, 2048], BF16)
        ssqA = A.alloc([16, 8]); ssqAB = B('ssqA')
        ssqB_ = A.alloc([16, 8]); ssqBB = B('ssqB')
        rab = A.alloc([2, 16]); rabB = B('rab')
        R1 = A.top
        hT = A.alloc([16, 2048], BF16)
        Z0 = A.top
        ZEND = A.n
        hTB = B('hT'); oTaB = B('oTa'); oTbB = B('oTb')

        try:
          bsnap = dict(bcnt)
          for b in range(nseq):
              bcnt.clear(); bcnt.update(bsnap)
              A.top = Z0
              XT = [A.alloc([2048]) for _ in range(2)]; XN = [A.alloc([2048], BF16) for _ in range(2)]
              STt = [A.alloc([4, 6]) for _ in range(2)]; MV = [A.alloc([8]) for _ in range(2)]
              XTB = [B('xt') for _ in range(2)]; XNB = [B('xn') for _ in range(2)]; STB = [B('st') for _ in range(2)]
              for tt in range(16):
                  xt = XT[tt % 2]; xn = XN[tt % 2]; st = STt[tt % 2]; mv = MV[tt % 2]
                  xtB = XTB[tt % 2]; xnB = XNB[tt % 2]; stB = STB[tt % 2]
                  k.dma('sp', xt, x[b, tt * 128:(tt + 1) * 128, :], [], [xtB], xtB)
                  for c4 in range(4):
                      S.op('dve', (lambda o, i: (lambda e: e.bn_stats(out=o, in_=i)))(st[:, c4, :], xt[:, c4 * 512:(c4 + 1) * 512]), [xtB], [stB])
                  S.op('dve', (lambda o, i: (lambda e: e.bn_aggr(out=o, in_=i)))(mv[:, 0:2], st.rearrange('p a b -> p (a b)')), [stB], [stB])
                  k.act(mv[:, 2:3], mv[:, 1:2], AF.Sqrt, [stB], [stB], bias=1e-5)
                  k.recip(mv[:, 3:4], mv[:, 2:3], [stB], [stB])
                  k.ts('dve', mv[:, 4:5], mv[:, 0:1], mv[:, 3:4], -1.0, ALU.mult, ALU.mult, [stB], [stB])
                  k.act(xn, xt, AF.Identity, [xtB, stB], [xnB], scale=mv[:, 3:4], bias=mv[:, 4:5])
                  for g4 in range(4):
                      pbi = g4 % 2
                      for j in range(4):
                          kc = g4 * 4 + j
                          k.tr(pb[pbi][:, j * 128:(j + 1) * 128], xn[:, kc * 128:(kc + 1) * 128], ident_b, [xnB, constB], [pbB[pbi]])
                      for j in range(4):
                          kc = g4 * 4 + j
                          o = hT[:, kc, tt * 128:(tt + 1) * 128]; i = pb[pbi][:, j * 128:(j + 1) * 128]
                          if pbi == 0:
                              k.act(o, i, AF.Identity, [pbB[pbi], modB], [hTB], scale=sc1T[:, kc, b:b + 1], bias=sh1T[:, kc, b:b + 1])
                          else:
                              k.ts('dve', o, i, sc1T[:, kc, b:b + 1], sh1T[:, kc, b:b + 1], ALU.mult, ALU.add, [pbB[pbi], modB], [hTB])
              if stop == 'S1':
                  S.barrier()
                  dt_ = A.alloc([2048]); dB = B('dbg')
                  for kc in range(16):
                      k.copy('dve', dt_, hT[:, kc, :], [hTB], [dB])
                      k.dma('sp', dbg[:, kc, :], dt_, [dB], [], dB)
                  break
              S.barrier()

              A.top = Z0
              QTM = [A.alloc([4, 128], BF16) for _ in range(2)]; QTMB = [B('qtm') for _ in range(2)]
              RT = [A.alloc([4, 4, 16]) for _ in range(2)]; RTB = [B('rt') for _ in range(2)]
              PE_ = [A.alloc([512], BF16) for _ in range(3)]; PEB = [B('pexp') for _ in range(3)]
              PM = [A.alloc([512], BF16) for _ in range(3)]; PMB = [B('pm') for _ in range(3)]
              OBF = [A.alloc([128], BF16) for _ in range(2)]; OBFB = [B('obf') for _ in range(2)]
              RL = [A.alloc([2]) for _ in range(2)]; RLB = [B('rl') for _ in range(2)]
              JK = A.alloc([512], BF16); JKB = B('junk')
              SMALL_END = A.top
              WB = [A.alloc([16, 512], BF16) for _ in range(2)]; WBB = [B('wblk') for _ in range(2)]
              QT = A.alloc([4, 2048], BF16); KT = A.alloc([4, 2048], BF16); V = A.alloc([16, 4, 130], BF16)
              QTB = B('QT'); KTB = B('KT'); VB = B('V')
              wi = 0
              it_ = 0
              for g in range(2):
                  k.memset('pool', V[:, :, :, 128:130], 1.0, [], [VB])
                  for typ in range(3):
                      blk = typ * 2 + g
                      wb = WB[wi % 2]; wbB = WBB[wi % 2]; wi += 1
                      k.dma('pool', wb, w_in[blk], [], [wbB], wbB)
                      if stop == 'B1':
                          raise StopBuild([(wb[:, 0, :], 512), (V[:, 0, :, :].rearrange('p a b -> p (a b)'), 520)])
                      for r in range(16):
                          pi = r % 2; ps = pf[pi]
                          for kc in range(16):
                              k.mm(ps[:, :], hT[:, kc, r:2048:16], wb[:, kc, :], kc == 0, kc == 15, [hTB, wbB], [pfB[pi]])
                          ps3 = ps[:, :].rearrange('p (h e) -> p h e', h=4)
                          if typ == 2:
                              k.copy('act' if r % 2 == 0 else 'dve', V[:, r, :, 0:128], ps3, [pfB[pi]], [VB])
                              continue
                          qi = it_ % 2; it_ += 1
                          qtm = QTM[qi]; qB = QTMB[qi]; rt = RT[qi]; rB = RTB[qi]
                          k.copy('act', qtm[:, :, 32:128], ps3[:, :, 32:128], [pfB[pi]], [qB])
                          if stop == 'B2a':
                              raise StopBuild([(qtm[:, 0, :], 128)])
                          cs = bc(cosA[:, r, :], [128, 4, 16], 1); sn = bc(sinA[:, r, :], [128, 4, 16], 1)
                          x1_ = ps3[:, :, 0:16]; x2_ = ps3[:, :, 16:32]
                          k.tt('dve', rt[:, 0], x1_, cs, ALU.mult, [pfB[pi], constB], [rB])
                          k.tt('dve', rt[:, 1], x2_, sn, ALU.mult, [pfB[pi], constB], [rB])
                          k.tt('dve', rt[:, 2], x2_, cs, ALU.mult, [pfB[pi], constB], [rB])
                          k.tt('dve', rt[:, 3], x1_, sn, ALU.mult, [pfB[pi], constB], [rB])
                          k.tt('dve', qtm[:, :, 0:16], rt[:, 0], rt[:, 1], ALU.subtract, [rB], [qB])
                          k.tt('dve', qtm[:, :, 16:32], rt[:, 2], rt[:, 3], ALU.add, [rB], [qB])
                          if stop == 'B2b':
                              raise StopBuild([(qtm[:, 0, :], 128), (rt[:, 0].rearrange('p a b -> p (a b)'), 64)])
                          pbi = qi
                          for hl in range(4):
                              k.tr(pb[pbi][:, hl * 128:(hl + 1) * 128], qtm[:, hl, :], ident_b, [qB, constB], [pbB[pbi]])
                          dst = (QT if typ == 0 else KT)[:, :, r * 128:(r + 1) * 128]
                          k.copy('act' if r % 2 == 1 else 'dve', dst, pb[pbi][:, 0:512].rearrange('p (h e) -> p h e', h=4),
                                 [pbB[pbi]], [QTB if typ == 0 else KTB])
                          if stop == 'B2':
                              raise StopBuild([(QT[:, 0, 0:128], 128), (qtm[:, 0, :], 128), (rt[:, 0].rearrange('p a b -> p (a b)'), 64)])
                  if stop == 'B3':
                      raise StopBuild([(QT[:, 0, :], 2048), (KT[:, 0, :], 2048), (V[:, 0:3, :, :].rearrange('p a b c -> p (a b c)'), 1560)])
                  batches = [(hl, c, rp) for hl in range(4) for c in range(4) for rp in range(16)]
                  LA = 2
                  SB = [(pf[0], pfB[0]), (pf[1], pfB[1]), (pbf[1], pbB[1])]

                  def qkA(n):
                      hl, c, rp = batches[n]
                      sps, spB = SB[n % 3]
                      k.mm(sps[:, :], KT[:, hl, rp * 128:(rp + 1) * 128], QT[:, hl, c * 512:(c + 1) * 512], True, True, [KTB, QTB], [spB])
                      pe_ = PE_[n % 3]; peB = PEB[n % 3]; pm = PM[n % 3]; pmB = PMB[n % 3]
                      k.act(pe_, sps[:, :], AF.Exp, [spB], [peB], scale=128.0 ** -0.5)
                      n0 = 15 - rp + 4 * c
                      k.tt('dve', pm, pe_, maskA[:, n0:n0 + 4, :].rearrange('p a b -> p (a b)'), ALU.mult, [peB, constB], [pmB])

                  def pvA(n):
                      hl, c, rp = batches[n]
                      h = g * 4 + hl
                      pm = PM[n % 3]; pmB = PMB[n % 3]
                      for j in range(4):
                          k.mm(pf[2 + j][:, 0:130], pm[:, j * 128:(j + 1) * 128], V[:, rp, hl, :], rp == 0, rp == 15, [pmB, VB], [pfB[2 + j]])
                      if rp == 15:
                          for j in range(4):
                              r = 4 * c + j; fi = j % 2; ops_ = pf[2 + j]; opB = pfB[2 + j]
                              k.recip(RL[fi][:, 0:1], ops_[:, 128:129], [opB], [RLB[fi]])
                              k.ts('dve', OBF[fi], ops_[:, 0:128], RL[fi][:, 0:1], None, ALU.mult, None, [opB, RLB[fi]], [OBFB[fi]])
                              k.act(JK[:, 0:128], ops_[:, 0:128], AF.Square, [opB, RLB[fi]], [JKB, ssqAB], scale=RL[fi][:, 0:1], accum_out=ssqA[:, r, h:h + 1])
                              k.tr(pb[0][:, fi * 128:(fi + 1) * 128], OBF[fi], ident_b, [OBFB[fi], constB], [pbB[0]])
                              k.act(oTa[:, h, r:2048:16], pb[0][:, fi * 128:(fi + 1) * 128], AF.Identity, [pbB[0], constB], [oTaB], scale=goa[:, h:h + 1])

                  for n in range(len(batches) + LA):
                      if n < len(batches):
                          qkA(n)
                      if n - LA >= 0:
                          pvA(n - LA)
              if stop == 'S3':
                  S.barrier()
                  A.top = SMALL_END
                  dt_ = A.alloc([2048]); dB = B('dbg')
                  for kc in range(8):
                      k.copy('dve', dt_, oTa[:, kc, :], [oTaB], [dB])
                      k.dma('sp', dbg[:, kc, :], dt_, [dB], [], dB)
                  k.copy('dve', dt_[:, 0:128], ssqA.rearrange('p a b -> p (a b)'), [ssqAB], [dB])
                  k.dma('sp', dbg[:, 8, 0:128], dt_[:, 0:128], [dB], [], dB)
                  break
              S.barrier()

              A.top = SMALL_END
              CQ0 = ZEND - 9216
              WB = [A.alloc([16, 512], BF16) for _ in range(2)]; WBB = [B('wblk') for _ in range(2)]
              WKR = A.alloc([16, 64], BF16); WKRB = B('wkr')
              CTM = [A.alloc([512], BF16) for _ in range(2)]; CTMB = [B('ctm') for _ in range(2)]
              SQ = [A.alloc([4]) for _ in range(2)]; SQB = [B('sq') for _ in range(2)]
              KRT = [A.alloc([4, 32]) for _ in range(2)]; KRTB = [B('krt') for _ in range(2)]
              KRM = [A.alloc([64], BF16) for _ in range(2)]; KRMB = [B('krm') for _ in range(2)]
              assert A.top <= CQ0
              save = A.top
              A.top = CQ0
              cqnT = A.alloc([4, 2048], BF16); ckvnT = A.alloc([4, 2048], BF16); krT = A.alloc([2048], BF16)
              cqB = B('cqnT'); ckvB = B('ckvnT'); krB = B('krT')
              A.top = save
              k.memset('pool', krT[64:128, :], 0.0, [], [krB])
              k.dma('pool', WB[0], w_in[6], [], [WBB[0]], WBB[0])
              k.dma('pool', WB[1], w_in[7], [], [WBB[1]], WBB[1])
              k.dma('pool', WKR, w_in_kr, [], [WKRB], WKRB)
              it_ = 0
              for typ in range(2):
                  wb = WB[typ]; wbB = WBB[typ]
                  dstT = cqnT if typ == 0 else ckvnT; dstB = cqB if typ == 0 else ckvB; gsc = gq if typ == 0 else gkv
                  for tt in range(16):
                      pi = tt % 2; ps = pf[pi]
                      for kc in range(16):
                          k.mm(ps[:, :], hT[:, kc, tt * 128:(tt + 1) * 128], wb[:, kc, :], kc == 0, kc == 15, [hTB, wbB], [pfB[pi]])
                      qi = it_ % 2; it_ += 1
                      sq = SQ[qi]; sqB = SQB[qi]; ctm = CTM[qi]; ctB = CTMB[qi]
                      k.act(JK, ps[:, :], AF.Square, [pfB[pi]], [JKB, sqB], accum_out=sq[:, 0:1])
                      k.act(sq[:, 1:2], sq[:, 0:1], AF.Sqrt, [sqB], [sqB], scale=1.0 / 512, bias=1e-6)
                      k.recip(sq[:, 2:3], sq[:, 1:2], [sqB], [sqB])
                      k.ts('dve', ctm, ps[:, :], sq[:, 2:3], None, ALU.mult, None, [pfB[pi], sqB], [ctB])
                      for j in range(4):
                          k.tr(pb[qi][:, j * 128:(j + 1) * 128], ctm[:, j * 128:(j + 1) * 128], ident_b, [ctB, constB], [pbB[qi]])
                      k.tt('dve', dstT[:, :, tt * 128:(tt + 1) * 128], pb[qi][:, 0:512].rearrange('p (a b) -> p a b', a=4),
                           bc(gsc, [128, 4, 128], 2), ALU.mult, [pbB[qi], constB], [dstB])
              for tt in range(16):
                  pi = 2 + tt % 2; ps = pf[pi]
                  for kc in range(16):
                      k.mm(ps[:, 0:64], hT[:, kc, tt * 128:(tt + 1) * 128], WKR[:, kc, :], kc == 0, kc == 15, [hTB, WKRB], [pfB[pi]])
                  qi = tt % 2; rt = KRT[qi]; rB = KRTB[qi]; km = KRM[qi]; kmB = KRMB[qi]
                  cs = cosB[:, tt, :]; sn = sinB[:, tt, :]
                  k.tt('dve', rt[:, 0], ps[:, 0:32], cs, ALU.mult, [pfB[pi], constB], [rB])
                  k.tt('dve', rt[:, 1], ps[:, 32:64], sn, ALU.mult, [pfB[pi], constB], [rB])
                  k.tt('dve', rt[:, 2], ps[:, 32:64], cs, ALU.mult, [pfB[pi], constB], [rB])
                  k.tt('dve', rt[:, 3], ps[:, 0:32], sn, ALU.mult, [pfB[pi], constB], [rB])
                  k.tt('dve', km[:, 0:32], rt[:, 0], rt[:, 1], ALU.subtract, [rB], [kmB])
                  k.tt('dve', km[:, 32:64], rt[:, 2], rt[:, 3], ALU.add, [rB], [kmB])
                  k.tr(pb[qi][0:64, 0:128], km, ident_b, [kmB, constB], [pbB[qi]])
                  k.copy('act', krT[0:64, tt * 128:(tt + 1) * 128], pb[qi][0:64, 0:128], [pbB[qi]], [krB])
              S.barrier()

              A.top = R1
              oTb = A.alloc([8, 2048], BF16)
              wuqn_s = A.alloc([4, 8, 128], BF16); wuqr_s = A.alloc([4, 8, 64], BF16)
              wuk_s = A.alloc([4, 1024], BF16); wuv_s = A.alloc([4, 1024], BF16)
              mwQN = B('mwqn'); mwQR = B('mwqr'); mwK = B('mwk'); mwV = B('mwv')
              assert A.top <= Z0
              k.dma('pool', wuqn_s, w_uqn, [], [mwQN], mwQN)
              k.dma('pool', wuqr_s, w_uqr, [], [mwQR], mwQR)
              k.dma('pool', wuk_s, w_uk, [], [mwK], mwK)
              k.dma('pool', wuv_s, w_uv, [], [mwV], mwV)
              A.top = SMALL_END
              qnT = A.alloc([2, 2048], BF16); knT = A.alloc([2, 2048], BF16); qrT = A.alloc([2, 2048], BF16)
              Vb = A.alloc([16, 2, 130], BF16)
              qnB = B('qnT'); knB = B('knT'); qrB = B('qrT'); VbB = B('Vb')
              QRM = [A.alloc([2, 64], BF16) for _ in range(2)]; QRMB = [B('qrm') for _ in range(2)]
              QRT = [A.alloc([4, 2, 32]) for _ in range(2)]; QRTB = [B('qrt') for _ in range(2)]
              assert A.top <= CQ0
              k.memset('pool', qrT[64:128, :, :], 0.0, [], [qrB])
              for g in range(4):
                  h0 = g * 2
                  k.memset('pool', Vb[:, :, :, 128:130], 1.0, [], [VbB])
                  ei = 0
                  for hl in range(2):
                      h = h0 + hl
                      for typ in range(2):
                          for ch in range(4):
                              pi = ei % 2; ps = pf[pi]
                              for kc in range(4):
                                  if typ == 0:
                                      k.mm(ps[:, :], wuqn_s[:, kc, h, :], cqnT[:, kc, ch * 512:(ch + 1) * 512], kc == 0, kc == 3, [mwQN, cqB], [pfB[pi]])
                                  else:
                                      k.mm(ps[:, :], wuk_s[:, kc, h * 128:(h + 1) * 128], ckvnT[:, kc, ch * 512:(ch + 1) * 512], kc == 0, kc == 3, [mwK, ckvB], [pfB[pi]])
                              dst = (qnT if typ == 0 else knT)[:, hl, ch * 512:(ch + 1) * 512]
                              k.copy('act' if ei % 2 == 0 else 'dve', dst, ps[:, :], [pfB[pi]], [qnB if typ == 0 else knB])
                              ei += 1
                  for tt in range(16):
                      pi = 2 + tt % 2; ps = pf[pi]
                      for kc in range(4):
                          k.mm(ps[:, 0:128], cqnT[:, kc, tt * 128:(tt + 1) * 128], wuqr_s[:, kc, h0:h0 + 2, :].rearrange('p a b -> p (a b)'),
                               kc == 0, kc == 3, [mwQR, cqB], [pfB[pi]])
                      qi = tt % 2; rt = QRT[qi]; rB = QRTB[qi]; qm = QRM[qi]; qmB = QRMB[qi]
                      ps3 = ps[:, 0:128].rearrange('p (h e) -> p h e', h=2)
                      cs = bc(cosB[:, tt, :], [128, 2, 32], 1); sn = bc(sinB[:, tt, :], [128, 2, 32], 1)
                      k.tt('dve', rt[:, 0], ps3[:, :, 0:32], cs, ALU.mult, [pfB[pi], constB], [rB])
                      k.tt('dve', rt[:, 1], ps3[:, :, 32:64], sn, ALU.mult, [pfB[pi], constB], [rB])
                      k.tt('dve', rt[:, 2], ps3[:, :, 32:64], cs, ALU.mult, [pfB[pi], constB], [rB])
                      k.tt('dve', rt[:, 3], ps3[:, :, 0:32], sn, ALU.mult, [pfB[pi], constB], [rB])
                      k.tt('dve', qm[:, :, 0:32], rt[:, 0], rt[:, 1], ALU.subtract, [rB], [qmB])
                      k.tt('dve', qm[:, :, 32:64], rt[:, 2], rt[:, 3], ALU.add, [rB], [qmB])
                      for hl in range(2):
                          k.tr(pb[qi][0:64, hl * 128:(hl + 1) * 128], qm[:, hl, :], ident_b, [qmB, constB], [pbB[qi]])
                      k.copy('act', qrT[0:64, :, tt * 128:(tt + 1) * 128], pb[qi][0:64, 0:256].rearrange('p (h e) -> p h e', h=2), [pbB[qi]], [qrB])
                      pi = 4 + tt % 2; ps = pf[pi]
                      for kc in range(4):
                          k.mm(ps[:, 0:256], ckvnT[:, kc, tt * 128:(tt + 1) * 128], wuv_s[:, kc, h0 * 128:(h0 + 2) * 128], kc == 0, kc == 3, [mwV, ckvB], [pfB[pi]])
                      k.copy('dve', Vb[:, tt, :, 0:128], ps[:, 0:256].rearrange('p (h e) -> p h e', h=2), [pfB[pi]], [VbB])
                  batches = [(hl, c, kt) for hl in range(2) for c in range(4) for kt in range(4 * c + 4)]
                  LA = 2
                  SB = [(pf[0], pfB[0]), (pf[1], pfB[1]), (pbf[1], pbB[1])]

                  def qkB(n):
                      hl, c, kt = batches[n]
                      sps, spB = SB[n % 3]
                      k.mm(sps[:, :], knT[:, hl, kt * 128:(kt + 1) * 128], qnT[:, hl, c * 512:(c + 1) * 512], True, False, [knB, qnB], [spB])
                      k.mm(sps[:, :], krT[:, kt * 128:(kt + 1) * 128], qrT[:, hl, c * 512:(c + 1) * 512], False, True, [krB, qrB], [spB])
                      pe_ = PE_[n % 3]; peB = PEB[n % 3]; pm = PM[n % 3]; pmB = PMB[n % 3]
                      j0_ = max(0, kt - 4 * c)
                      k.act(pe_[:, j0_ * 128:512], sps[:, j0_ * 128:512], AF.Exp, [spB], [peB], scale=192.0 ** -0.5)
                      if kt >= 4 * c:
                          k.tt('dve', pm[:, 0:128], pe_[:, j0_ * 128:(j0_ + 1) * 128], maskC, ALU.mult, [peB, constB], [pmB])

                  def pvB(n):
                      hl, c, kt = batches[n]
                      h = h0 + hl
                      pe_ = PE_[n % 3]; peB = PEB[n % 3]; pm = PM[n % 3]; pmB = PMB[n % 3]
                      for j in range(4):
                          qt = 4 * c + j
                          if kt > qt:
                              continue
                          ops_ = pf[2 + j]; opB = pfB[2 + j]
                          if kt == qt:
                              k.mm(ops_[:, 0:130], pm[:, 0:128], Vb[:, kt, hl, :], kt == 0, True, [pmB, VbB], [opB])
                              fi = j % 2
                              k.recip(RL[fi][:, 0:1], ops_[:, 128:129], [opB], [RLB[fi]])
                              k.ts('dve', OBF[fi], ops_[:, 0:128], RL[fi][:, 0:1], None, ALU.mult, None, [opB, RLB[fi]], [OBFB[fi]])
                              k.act(JK[:, 0:128], ops_[:, 0:128], AF.Square, [opB, RLB[fi]], [JKB, ssqBB], scale=RL[fi][:, 0:1], accum_out=ssqB_[:, qt, h:h + 1])
                              k.tr(pb[0][:, fi * 128:(fi + 1) * 128], OBF[fi], ident_b, [OBFB[fi], constB], [pbB[0]])
                              k.act(oTb[:, h, qt * 128:(qt + 1) * 128], pb[0][:, fi * 128:(fi + 1) * 128], AF.Identity, [pbB[0], constB], [oTbB], scale=gob[:, h:h + 1])
                          else:
                              k.mm(ops_[:, 0:130], pe_[:, j * 128:(j + 1) * 128], Vb[:, kt, hl, :], kt == 0, False, [peB, VbB], [opB])

                  for n in range(len(batches) + LA):
                      if n < len(batches):
                          qkB(n)
                      if n - LA >= 0:
                          pvB(n - LA)
              sA = A.alloc([16]); sAn = A.alloc([16]); sBn = A.alloc([16]); sB_ = B('ssum')
              k.S.op('dve', (lambda o, i: (lambda e: e.tensor_reduce(out=o, in_=i, axis=AX.X, op=ALU.add)))(sA, ssqA), [ssqAB], [sB_])
              k.S.op('dve', (lambda o, i: (lambda e: e.tensor_reduce(out=o, in_=i, axis=AX.X, op=ALU.add)))(sBn, ssqB_), [ssqBB], [sB_])
              ssB = B('ssqscr')
              k.dma('sp', ssqscr[b].rearrange('(i r) -> i r', r=16), sA, [sB_], [ssB], sB_)
              k.dma('sp', sAn, ssqscr[b].rearrange('(t p) -> p t', p=128), [ssB], [sB_], sB_, slow=True)
              k.act(rab[:, 0, :], sAn, AF.Sqrt, [sB_], [rabB], scale=1.0 / 1024, bias=1e-6)
              k.act(rab[:, 1, :], sBn, AF.Sqrt, [sB_], [rabB], scale=1.0 / 1024, bias=1e-6)
              k.recip(rab.rearrange('p a b -> p (a b)'), rab.rearrange('p a b -> p (a b)'), [rabB], [rabB])
              if stop == 'S6':
                  S.barrier()
                  A.top = SMALL_END
                  dt_ = A.alloc([2048]); dB = B('dbg')
                  for kc in range(16):
                      k.copy('dve', dt_, (oTa if kc < 8 else oTb)[:, kc % 8, :], [oTaB, oTbB], [dB])
                      k.dma('sp', dbg[:, kc, :], dt_, [dB], [], dB)
                  break
              S.barrier()

              A.top = R1 + 8192
              wo_s = A.alloc([16, 2048], BF16); woB = [B('wo') for _ in range(4)]
              for q4 in range(4):
                  k.dma('pool', wo_s[:, q4 * 4:(q4 + 1) * 4, :], w_o[:, q4 * 4:(q4 + 1) * 4, :], [], [woB[q4]], woB[q4])
              G1 = A.alloc([2048]); L1G = A.alloc([2048]); L1B = A.alloc([2048]); G1B = B('g1'); L1GB = B('l1g'); L1BB = B('l1b')
              k.dma('sp', G1, modrow[b, 0:1, :].to_broadcast([128, 2048]), [mrB], [G1B], G1B)
              k.dma('sp', L1G, ln1_g.to_broadcast([128, 2048]), [], [L1GB], L1GB)
              k.dma('sp', L1B, ln1_b.to_broadcast([128, 2048]), [], [L1BB], L1BB)
              XT = [A.alloc([2048]) for _ in range(2)]; XTB = [B('xt') for _ in range(2)]
              YT = [A.alloc([2048]) for _ in range(2)]; YTB = [B('yt') for _ in range(2)]
              TM = [A.alloc([512]) for _ in range(2)]; TMB = [B('tm') for _ in range(2)]
              STt = [A.alloc([4, 6]) for _ in range(2)]; MV = [A.alloc([8]) for _ in range(2)]; STB = [B('st') for _ in range(2)]
              x1B = B('x1scr')
              ti = 0
              for tt in range(16):
                  xt = XT[tt % 2]; xtB = XTB[tt % 2]; yt = YT[tt % 2]; ytB = YTB[tt % 2]
                  st = STt[tt % 2]; mv = MV[tt % 2]; stB = STB[tt % 2]
                  k.dma('sp', xt, x[b, tt * 128:(tt + 1) * 128, :], [], [xtB], xtB)
                  for nb in range(4):
                      pa = pf[(nb % 2) * 2]; paB = pfB[(nb % 2) * 2]; pb_ = pf[(nb % 2) * 2 + 1]; pbB_ = pfB[(nb % 2) * 2 + 1]
                      for fc in range(8):
                          k.mm(pa[:, :], oTa[:, fc, tt * 128:(tt + 1) * 128], wo_s[:, fc, nb * 512:(nb + 1) * 512], fc == 0, fc == 7, [oTaB, woB[fc // 4]], [paB])
                      for fc in range(8):
                          k.mm(pb_[:, :], oTb[:, fc, tt * 128:(tt + 1) * 128], wo_s[:, 8 + fc, nb * 512:(nb + 1) * 512], fc == 0, fc == 7, [oTbB, woB[2 + fc // 4]], [pbB_])
                      tm = TM[ti % 2]; tmB = TMB[ti % 2]; ti += 1
                      sl = slice(nb * 512, (nb + 1) * 512)
                      k.act(tm, pa[:, :], AF.Identity, [paB, rabB], [tmB], scale=rab[:, 0, tt:tt + 1])
                      k.stt(tm, pb_[:, :], rab[:, 1, tt:tt + 1], tm, ALU.mult, ALU.add, [pbB_, rabB, tmB], [tmB])
                      k.tt('dve', tm, tm, G1[:, sl], ALU.mult, [tmB, G1B], [tmB])
                      k.stt(yt[:, sl], xt[:, sl], ALPHA, tm, ALU.mult, ALU.add, [xtB, tmB], [ytB])
                      S.op('dve', (lambda o, i: (lambda e: e.bn_stats(out=o, in_=i)))(st[:, nb, :], yt[:, sl]), [ytB], [stB])
                  S.op('dve', (lambda o, i: (lambda e: e.bn_aggr(out=o, in_=i)))(mv[:, 0:2], st.rearrange('p a b -> p (a b)')), [stB], [stB])
                  k.act(mv[:, 2:3], mv[:, 1:2], AF.Sqrt, [stB], [stB], bias=1e-5)
                  k.recip(mv[:, 3:4], mv[:, 2:3], [stB], [stB])
                  k.ts('dve', mv[:, 4:5], mv[:, 0:1], mv[:, 3:4], -1.0, ALU.mult, ALU.mult, [stB], [stB])
                  k.act(yt, yt, AF.Identity, [ytB, stB], [ytB], scale=mv[:, 3:4], bias=mv[:, 4:5])
                  k.tt('dve', yt, yt, L1G, ALU.mult, [ytB, L1GB], [ytB])
                  k.tt('dve', yt, yt, L1B, ALU.add, [ytB, L1BB], [ytB])
                  k.dma('sp', x1scr[b, tt * 128:(tt + 1) * 128, :], yt, [ytB], [x1B], ytB)
              if stop == 'S7':
                  break
              S.barrier()

              A.top = P_END
              BC = [A.alloc([2048]) for _ in range(5)]; BCB = [B('bc2') for _ in range(5)]
              k.dma('sp', BC[0], modrow[b, 2:3, :].to_broadcast([128, 2048]), [mrB], [BCB[0]], BCB[0])
              k.dma('sp', BC[1], modrow[b, 1:2, :].to_broadcast([128, 2048]), [mrB], [BCB[1]], BCB[1])
              k.dma('sp', BC[2], modrow[b, 3:4, :].to_broadcast([128, 2048]), [mrB], [BCB[2]], BCB[2])
              k.dma('sp', BC[3], ln2_g.to_broadcast([128, 2048]), [], [BCB[3]], BCB[3])
              k.dma('sp', BC[4], ln2_b.to_broadcast([128, 2048]), [], [BCB[4]], BCB[4])
              X1 = [A.alloc([2048]) for _ in range(2)]; X1B = [B('x1') for _ in range(2)]
              ACC = A.alloc([2048]); ACCB = B('acc')
              H2 = ACC; H2B = ACCB
              H2b = [A.alloc([2048], BF16) for _ in range(2)]; H2bB = [B('h2b') for _ in range(2)]
              H2T = A.alloc([16, 128]); H2TB = B('h2T')
              WPQ = [A.alloc([16, 128]) for _ in range(2)]; WPQB = [B('wpq') for _ in range(2)]
              QTC = [A.alloc([128]) for _ in range(2)]; QTCB = [B('qtc') for _ in range(2)]
              SC = A.alloc([16, 128]); SCB = B('sc')
              WK = [A.alloc([128]) for _ in range(2)]; WKB = [B('wk') for _ in range(2)]
              TV = A.alloc([16, 16]); TVB = B('tv')
              TI = A.alloc([16, 16], U32); TIF = A.alloc([16, 16]); TIB = B('ti')
              CAND = A.alloc([8, 256]); CANDB = B('cand')
              WK2 = A.alloc([256]); WK2B = B('wk2')
              VALS = A.alloc([8, 16]); VALSB = B('vals')
              POS = A.alloc([8, 16], U32); POSB = B('pos')
              ABU = A.alloc([2, 128], U32); ABF = A.alloc([2, 8, 16]); ABB = B('ab')
              OH = CAND.rearrange('p h (a b) -> p h a b', a=16); OHB = CANDB
              SEL = A.alloc([2, 8, 16]); SELB = B('sel')
              EIDX = A.alloc([128]); EB0 = B('eidxf')
              EIDXU = [A.alloc([128], U32) for _ in range(2)]; EB = [B('eidx') for _ in range(2)]
              GT2 = [A.alloc([128]) for _ in range(2)]; GT2B = [B('gates') for _ in range(2)]
              GS = A.alloc([24]); GSB = B('gs')
              AA = A.alloc([128]); AAB = B('aa')
              WW = A.alloc([128]); WWB = B('ww')
              NGB = 7
              GB_ = [A.alloc([4096], BF16) for _ in range(NGB)]; GBB = [B('gb') for _ in range(NGB)]
              TG = A.alloc([128])
              DG = [A.alloc([128], BF16) for _ in range(4)]; DGB = [B('dg') for _ in range(4)]
              if b == 0:
                  print('S8 arena top', A.top, 'of', A.n)
              STt = [A.alloc([4, 6]) for _ in range(2)]; MV = [A.alloc([8]) for _ in range(2)]; STB = [B('st') for _ in range(2)]
              ST2 = A.alloc([4, 6]); MV2 = A.alloc([8]); ST2B = B('st2')
              outB = B('out')
              cnt = {'wq': 0, 'gi': 0}

              def ln_stats(src, srcB, st, mv, stB):
                  for c4 in range(4):
                      S.op('dve', (lambda o, i: (lambda e: e.bn_stats(out=o, in_=i)))(st[:, c4, :], src[:, c4 * 512:(c4 + 1) * 512]), [srcB], [stB])
                  S.op('dve', (lambda o, i: (lambda e: e.bn_aggr(out=o, in_=i)))(mv[:, 0:2], st.rearrange('p a b -> p (a b)')), [stB], [stB])
                  k.act(mv[:, 2:3], mv[:, 1:2], AF.Sqrt, [stB], [stB], bias=1e-5)
                  k.recip(mv[:, 3:4], mv[:, 2:3], [stB], [stB])
                  k.ts('dve', mv[:, 4:5], mv[:, 0:1], mv[:, 3:4], -1.0, ALU.mult, ALU.mult, [stB], [stB])

              def stageA(tt):
                  x1 = X1[tt % 2]; x1B_ = X1B[tt % 2]; st = STt[tt % 2]; mv = MV[tt % 2]; stB = STB[tt % 2]
                  h2b = H2b[tt % 2]; h2bB = H2bB[tt % 2]
                  k.dma('sp', x1, x1scr[b, tt * 128:(tt + 1) * 128, :], [x1B], [x1B_], x1B_)
                  ln_stats(x1, x1B_, st, mv, stB)
                  k.act(H2, x1, AF.Identity, [x1B_, stB], [H2B], scale=mv[:, 3:4], bias=mv[:, 4:5])
                  k.tt('dve', H2, H2, BC[0], ALU.mult, [H2B, BCB[0]], [H2B])
                  k.tt('dve', H2, H2, BC[1], ALU.add, [H2B, BCB[1]], [H2B])
                  k.copy('act', h2b, H2, [H2B], [h2bB])
                  for g4 in range(4):
                      pi = g4 % 2
                      for j in range(4):
                          kc = g4 * 4 + j
                          k.tr(pf[pi][:, j * 128:(j + 1) * 128], H2[:, kc * 128:(kc + 1) * 128], ident_f, [H2B, constB], [pfB[pi]])
                      k.copy('act', H2T[:, g4 * 4:(g4 + 1) * 4, :], pf[pi][:, :].rearrange('p (a b) -> p a b', a=4), [pfB[pi]], [H2TB])
                  for c in range(16):
                      wp = WPQ[cnt['wq'] % 2]; wpB = WPQB[cnt['wq'] % 2]; cnt['wq'] += 1
                      k.dma('sp', wp, w_pq[c], [], [wpB], wpB)
                      pi = c % 2
                      for kc in range(16):
                          k.mm(pf[pi][:, 0:128], wp[:, kc, :], H2T[:, kc, :], kc == 0, kc == 15, [wpB, H2TB], [pfB[pi]])
                      qc = QTC[c % 2]; qcB = QTCB[c % 2]
                      k.copy('act', qc, pf[pi][:, 0:128], [pfB[pi]], [qcB])
                      si = (c // 4) % 2
                      k.mm(pbf[si][:, (c % 4) * 128:(c % 4 + 1) * 128], qc, skT_s[:, c % 2, :], True, True, [qcB, constB], [pbB[si]])
                      if c % 4 == 3:
                          k.copy('act', SC[:, c - 3:c + 1, :], pbf[si][:, :].rearrange('p (a b) -> p a b', a=4), [pbB[si]], [SCB])

              def stageT(tt):
                  eu = EIDXU[tt % 2]; eB = EB[tt % 2]; GT = GT2[tt % 2]; GTB = GT2B[tt % 2]
                  for c in range(16):
                      wk = WK[c % 2]; wkB = WKB[c % 2]
                      S.op('dve', (lambda o, i: (lambda e: e.max(out=o, in_=i)))(TV[:, c, 0:8], SC[:, c, :]), [SCB], [TVB])
                      yield
                      S.op('dve', (lambda o, m, i: (lambda e: e.max_index(out=o, in_max=m, in_values=i)))(TI[:, c, 0:8], TV[:, c, 0:8], SC[:, c, :]), [SCB, TVB], [TIB])
                      yield
                      S.op('dve', (lambda o, m, i: (lambda e: e.match_replace(out=o, in_to_replace=m, in_values=i, imm_value=NEG)))(wk, TV[:, c, 0:8], SC[:, c, :]), [SCB, TVB], [wkB])
                      yield
                      S.op('dve', (lambda o, i: (lambda e: e.max(out=o, in_=i)))(TV[:, c, 8:16], wk), [wkB], [TVB])
                      yield
                      S.op('dve', (lambda o, m, i: (lambda e: e.max_index(out=o, in_max=m, in_values=i)))(TI[:, c, 8:16], TV[:, c, 8:16], wk), [wkB, TVB], [TIB])
                      yield
                  k.copy('dve', TIF, TI, [TIB], [TIB])
                  yield
                  tv4 = TV.rearrange('p (h t) k -> p h t k', t=2); ti4 = TIF.rearrange('p (h t) k -> p h t k', t=2)
                  k.tt('dve', CAND.rearrange('p h (a b) -> p h a b', a=16), bc(tv4[:, :, 0, :], [128, 8, 16, 16], 3), bc(tv4[:, :, 1, :], [128, 8, 16, 16], 2),
                       ALU.add, [TVB], [CANDB])
                  yield
                  for hh in range(8):
                      S.op('dve', (lambda o, i: (lambda e: e.max(out=o, in_=i)))(VALS[:, hh, 0:8], CAND[:, hh, :]), [CANDB], [VALSB])
                      yield
                      S.op('dve', (lambda o, m, i: (lambda e: e.max_index(out=o, in_max=m, in_values=i)))(POS[:, hh, 0:8], VALS[:, hh, 0:8], CAND[:, hh, :]), [CANDB, VALSB], [POSB])
                      yield
                      S.op('dve', (lambda o, m, i: (lambda e: e.match_replace(out=o, in_to_replace=m, in_values=i, imm_value=NEG)))(WK2, VALS[:, hh, 0:8], CAND[:, hh, :]), [CANDB, VALSB], [WK2B])
                      yield
                      S.op('dve', (lambda o, i: (lambda e: e.max(out=o, in_=i)))(VALS[:, hh, 8:16], WK2), [WK2B], [VALSB])
                      yield
                      S.op('dve', (lambda o, m, i: (lambda e: e.max_index(out=o, in_max=m, in_values=i)))(POS[:, hh, 8:16], VALS[:, hh, 8:16], WK2), [WK2B, VALSB], [POSB])
                      yield
                  posf = POS.rearrange('p h k -> p (h k)')
                  S.op('dve', (lambda o, i: (lambda e: e.tensor_single_scalar(out=o, in_=i, scalar=4, op=ALU.logical_shift_right)))(ABU[:, 0, :], posf), [POSB], [ABB])
                  yield
                  S.op('dve', (lambda o, i: (lambda e: e.tensor_single_scalar(out=o, in_=i, scalar=15, op=ALU.bitwise_and)))(ABU[:, 1, :], posf), [POSB], [ABB])
                  yield
                  k.copy('dve', ABF.rearrange('p t h k -> p (t h k)'), ABU.rearrange('p t n -> p (t n)'), [ABB], [ABB])
                  yield
                  io16 = iota[:, 0:16].unsqueeze(1).unsqueeze(1).to_broadcast([128, 8, 16, 16])
                  for t2 in range(2):
                      k.tt('dve', OH, io16, bc(ABF[:, t2], [128, 8, 16, 16], 3), ALU.is_equal, [ABB, constB], [OHB])
                      yield
                      k.tt('dve', OH, OH, bc(ti4[:, :, t2, :], [128, 8, 16, 16], 2), ALU.mult, [OHB, TIB], [OHB])
                      yield
                      S.op('dve', (lambda o, i: (lambda e: e.tensor_reduce(out=o, in_=i, axis=AX.X, op=ALU.add)))(SEL[:, t2], OH), [OHB], [SELB])
                      yield
                  k.stt(EIDX.rearrange('p (h k) -> p h k', h=8), SEL[:, 0], 128.0, SEL[:, 1], ALU.mult, ALU.add, [SELB], [EB0])
                  yield
                  k.copy('dve', eu, EIDX, [EB0], [eB])
                  yield
                  k.S.op('dve', (lambda o, i: (lambda e: e.tensor_reduce(out=o, in_=i, axis=AX.X, op=ALU.max)))(GS[:, 0:8], VALS), [VALSB], [GSB])
                  yield
                  k.ts('dve', GS[:, 8:16], GS[:, 0:8], -1.0, None, ALU.mult, None, [GSB], [GSB])
                  yield
                  for hh in range(8):
                      k.act(GT[:, hh * 16:(hh + 1) * 16], VALS[:, hh, :], AF.Exp, [VALSB, GSB], [GTB, GSB], bias=GS[:, 8 + hh:9 + hh], accum_out=GS[:, 16 + hh:17 + hh])
                      yield
                  k.recip(GS[:, 0:8], GS[:, 16:24], [GSB], [GSB])
                  yield
                  k.tt('dve', GT.rearrange('p (h k) -> p h k', h=8), GT.rearrange('p (h k) -> p h k', h=8), bc(GS[:, 0:8], [128, 8, 16], 2), ALU.mult, [GTB, GSB], [GTB])
                  yield

              def capture(stage, *args):
                  lst = []
                  real = S.op
                  S.op = lambda eng, fn, reads=(), writes=(), dma=None, ndma=1: lst.append((eng, fn, list(reads), list(writes), dma, ndma))
                  try:
                      r = stage(*args)
                      if r is not None:
                          for _ in r:
                              pass
                  finally:
                      S.op = real
                  return lst

              def replay(lst, n):
                  for _ in range(min(n, len(lst))):
                      eng, fn, R, W, dma, ndma = lst.pop(0)
                      S.op(eng, fn, R, W, dma=dma, ndma=ndma)

              def stageUV(tt, aops, tops):
                  eu = EIDXU[tt % 2]; eB = EB[tt % 2]; h2b = H2b[tt % 2]; h2bB = H2bB[tt % 2]; GT = GT2[tt % 2]; GTB = GT2B[tt % 2]
                  for kk in range(128):
                      gb = GB_[cnt['gi'] % NGB]; gbB = GBB[cnt['gi'] % NGB]; cnt['gi'] += 1
                      aB = Buf('aa'); wB = Buf('ww')
                      k.gather(gb, uvb, eu[:, kk:kk + 1], [eB, tabB], [gbB], gbB)
                      k.stt(gb[:, 0:2048], gb[:, 0:2048], 1.0, h2b, ALU.mult, ALU.mult, [gbB, h2bB], [aB, gbB], accum_out=AA[:, kk:kk + 1])
                      k.act(TG[:, kk:kk + 1], AA[:, kk:kk + 1], AF.Gelu, [aB], [wB])
                      k.act(WW[:, kk:kk + 1], TG[:, kk:kk + 1], AF.Identity, [wB, GTB], [wB], scale=GT[:, kk:kk + 1])
                      dg = DG[kk % 4]; dgB = DGB[kk % 4]
                      k.act(dg, ident_f, AF.Identity, [wB, constB], [dgB], scale=WW[:, kk:kk + 1])
                      for nb in range(4):
                          k.mm(pf[2 + nb][:, :], dg, gb[:, 2048 + nb * 512:2048 + (nb + 1) * 512], kk == 0, kk == 127, [dgB, gbB], [pfB[2 + nb]])
                      if kk < 40:
                          replay(aops, (len(aops) + 39 - kk) // (40 - kk))
                      else:
                          replay(aops, len(aops))
                          replay(tops, (len(tops) + 119 - kk) // max(1, 120 - kk) if kk < 120 else len(tops))
                  replay(aops, len(aops)); replay(tops, len(tops))

              def stageF(tt):
                  x1 = X1[tt % 2]; x1B_ = X1B[tt % 2]
                  for nb in range(4):
                      sl = slice(nb * 512, (nb + 1) * 512)
                      k.tt('dve', ACC[:, sl], pf[2 + nb][:, :], BC[2][:, sl], ALU.mult, [pfB[2 + nb], BCB[2]], [ACCB])
                  k.stt(ACC, x1, ALPHA, ACC, ALU.mult, ALU.add, [x1B_, ACCB], [ACCB])
                  ln_stats(ACC, ACCB, ST2, MV2, ST2B)
                  k.act(ACC, ACC, AF.Identity, [ACCB, ST2B], [ACCB], scale=MV2[:, 3:4], bias=MV2[:, 4:5])
                  k.tt('dve', ACC, ACC, BC[3], ALU.mult, [ACCB, BCB[3]], [ACCB])
                  k.tt('dve', ACC, ACC, BC[4], ALU.add, [ACCB, BCB[4]], [ACCB])
                  k.dma('sp', out[b, tt * 128:(tt + 1) * 128, :], ACC, [ACCB], [outB], ACCB)

              if peer_tiles > 0:
                  stageA(0)
                  for _ in stageT(0):
                      pass
              for tt in range(peer_tiles):
                  aops, tops = [], []
                  if tt + 1 < peer_tiles:
                      aops = capture(stageA, tt + 1)
                      tops = capture(stageT, tt + 1)
                  stageUV(tt, aops, tops)
                  stageF(tt)
              S.barrier()
        except StopBuild as sb:
            S.barrier()
            A.top = A.n - 2048
            dt_ = A.alloc([2048]); dB = Buf('dbgx')
            for i, (ap, n) in enumerate(sb.items):
                k.copy('dve', dt_[:ap.shape[0], 0:n], ap, [], [dB])
                k.dma('sp', dbg[:ap.shape[0], i, 0:n], dt_[:ap.shape[0], 0:n], [dB], [], dB)
        S.barrier()
        S.emit()
    return nc


def _consts():
    c = {}
    c['ident'] = np.eye(128, dtype=np.float32)
    ip = np.arange(128)[:, None]; i = np.arange(128)[None, :]
    mA = np.zeros((128, 31, 128), np.float32)
    for m in range(31):
        d = m - 15
        dt = 16 * (i - ip) + d
        mult = ((dt >= 0) & (dt <= 128)).astype(np.float32)
        if d % 4 == 0:
            mult += ((dt >= 0) & (dt <= 512))
        if d == 0:
            mult += (dt >= 0)
        mA[:, m, :] = mult
    c['maskA'] = mA
    c['maskC'] = (ip <= i).astype(np.float32)
    theta = 500000.0
    invA = theta ** (-np.arange(16, dtype=np.float64) * 2.0 / 32)
    tA = (16 * np.arange(128)[:, None] + np.arange(16)[None, :]).astype(np.float64)
    angA = tA[:, :, None] * invA[None, None, :]
    c['cosA'] = np.cos(angA).astype(np.float32); c['sinA'] = np.sin(angA).astype(np.float32)
    invB = theta ** (-np.arange(32, dtype=np.float64) * 2.0 / 64)
    tB = (128 * np.arange(16)[None, :] + np.arange(128)[:, None]).astype(np.float64)
    angB = tB[:, :, None] * invB[None, None, :]
    c['cosB'] = np.cos(angB).astype(np.float32); c['sinB'] = np.sin(angB).astype(np.float32)
    c['iota'] = np.broadcast_to(np.arange(256, dtype=np.float32), (128, 256)).copy()
    return c


def _prep_shared(inp):
    f = lambda a: np.ascontiguousarray(a, dtype=np.float32)
    sh = dict(_consts())
    w_ada = inp['w_ada'][0]
    sh['w_ada_r'] = f(w_ada.reshape(16, 128, 24, 512).transpose(2, 1, 0, 3))
    sh['b_adaT'] = f(inp['b_ada'][0].reshape(96, 128).T)
    sh['b_ada_row'] = f(inp['b_ada'][0].reshape(1, 12288))
    w_in = inp['w_in'][0]
    sh['w_in_r'] = f(w_in[:, :4096].reshape(16, 128, 8, 512).transpose(2, 1, 0, 3))
    sh['w_in_kr'] = f(w_in[:, 4096:4160].reshape(16, 128, 64).transpose(1, 0, 2))
    sh['g_qT'] = f(inp['g_q_lat'][0].reshape(4, 128).T); sh['g_kvT'] = f(inp['g_kv_lat'][0].reshape(4, 128).T)
    wuq = inp['w_uq'][0].reshape(4, 128, 8, 192).transpose(1, 0, 2, 3)
    sh['w_uqn'] = f(wuq[..., :128]); sh['w_uqr'] = f(wuq[..., 128:])
    sh['w_uk_r'] = f(inp['w_uk'][0].reshape(4, 128, 1024).transpose(1, 0, 2))
    sh['w_uv_r'] = f(inp['w_uv'][0].reshape(4, 128, 1024).transpose(1, 0, 2))
    sh['g_oaT'] = f(inp['g_out_a'][0].reshape(8, 128).T); sh['g_obT'] = f(inp['g_out_b'][0].reshape(8, 128).T)
    sh['w_o_r'] = f(inp['w_o'][0].reshape(16, 128, 2048).transpose(1, 0, 2))
    for n in ('ln1_g', 'ln1_b', 'ln2_g', 'ln2_b'):
        sh[n] = f(inp[n][0].reshape(1, 2048))
    sh['w_pq_r'] = f(inp['w_pq'][0].reshape(16, 128, 16, 128).transpose(2, 1, 0, 3))
    sh['skT'] = f(np.stack([inp['sub_key_1'][0].T, inp['sub_key_2'][0].T], axis=1))
    sh['u_table'] = f(inp['u_table'][0]); sh['v_table'] = f(inp['v_table'][0])
    return sh


def _core_map(sh, inp, b0, nseq):
    m = dict(sh)
    m['x'] = np.ascontiguousarray(inp['x'][b0:b0 + nseq], dtype=np.float32)
    cc = np.asarray(inp['c'][b0:b0 + nseq], dtype=np.float32)
    if nseq == 1:
        cc = np.concatenate([cc, cc], 0)
    m['cT'] = np.ascontiguousarray(cc.reshape(2, 16, 128).transpose(2, 1, 0))
    return m


_NC_CACHE = {}


def kernel(**inputs):
    inp = {k_: np.asarray(v) for k_, v in inputs.items()}
    nseq = 16 // NCORES
    if 'nc' not in _NC_CACHE:
        _NC_CACHE['nc'] = build(nseq=nseq)
    nc = _NC_CACHE['nc']
    sh = _prep_shared(inp)
    maps = [_core_map(sh, inp, c * nseq, nseq) for c in range(NCORES)]
    res = run_bass_kernel_spmd(nc, maps, core_ids=list(range(NCORES)))
    return np.concatenate([r['out'] for r in res.results], axis=0).astype(np.float32)
```

```python
import numpy as np
from contextlib import ExitStack
import concourse.bass as bass
import concourse.mybir as mybir
from concourse.bass_utils import run_bass_kernel_spmd

F32 = mybir.dt.float32; BF16 = mybir.dt.bfloat16; I32 = mybir.dt.int32; U32 = mybir.dt.uint32
ALU = mybir.AluOpType; AF = mybir.ActivationFunctionType; AX = mybir.AxisListType
SAME_SYNC = True
NCORES = 8
S_ = 2048; D_ = 2048
ALPHA = 2.0 ** 0.25
NEG = -1.0e30


class Buf:
    __slots__ = ('name', 'w', 'r', 'sem', 'semcnt', 'excl')

    def __init__(self, name, excl=False):
        self.name = name; self.w = {}; self.r = {}; self.sem = None; self.semcnt = 0; self.excl = excl


class Sched:
    ENG = ('pe', 'act', 'dve', 'pool', 'sp')

    def __init__(self, nc, ctx):
        self.nc = nc; self.ctx = ctx
        self.ops = {e: [] for e in self.ENG}
        self.esem = {e: ctx.enter_context(nc.semaphore('sem_' + e)) for e in self.ENG}
        self.dbufs = []; self.dset = set()
        self.uid = 0

    def op(self, eng, fn, reads=(), writes=(), dma=None, ndma=1):
        ops = self.ops[eng]
        idx = len(ops)
        deps = []
        for b in reads:
            deps.extend(b.w.values())
            if b.excl:
                deps.extend(v for kk, v in b.r.items() if kk != eng)
        for b in writes:
            deps.extend(b.w.values()); deps.extend(b.r.values())
        waits = set()
        for ev in deps:
            if ev[0] == 'E':
                e2 = ev[1]
                if e2 == eng and dma is None and (eng == 'pe' or not SAME_SYNC):
                    continue
                self.ops[e2][ev[2]]['inc'] = True
            waits.add(ev)
        if dma is not None:
            if dma.sem is None:
                dma.sem = self.ctx.enter_context(self.nc.semaphore('ds%d' % len(self.dbufs)))
            if id(dma) not in self.dset:
                self.dset.add(id(dma)); self.dbufs.append(dma)
            dma.semcnt += 16 * ndma
            ev = ('D', dma, dma.semcnt)
            self.uid += 1
            key = ('dma', self.uid)
        else:
            ev = ('E', eng, idx)
            key = eng
        ops.append(dict(fn=fn, waits=waits, inc=False, dma=dma))
        for b in reads:
            b.r[key] = ev
        for b in writes:
            b.w = {key: ev}; b.r = {}
        return ev

    def barrier(self):
        last = {}
        for e in self.ENG:
            for i in range(len(self.ops[e]) - 1, -1, -1):
                if self.ops[e][i]['fn'] is not None and self.ops[e][i]['dma'] is None:
                    last[e] = ('E', e, i); self.ops[e][i]['inc'] = True
                    break
        dmaev = [('D', b, b.semcnt) for b in self.dbufs if b.semcnt > 0]
        for e in self.ENG:
            waits = set(v for k, v in last.items() if (k != e or e != 'pe')) | set(dmaev)
            self.ops[e].append(dict(fn=None, waits=waits, inc=False, dma=None))

    def emit(self):
        nc = self.nc
        seq = {}
        for e in self.ENG:
            c = 0
            for i, o in enumerate(self.ops[e]):
                if o['inc']:
                    c += 1; seq[(e, i)] = c

        def run(eng, e):
            seen = {}
            for i, o in enumerate(self.ops[e]):
                for ev in o['waits']:
                    if ev[0] == 'E':
                        sem = self.esem[ev[1]]; val = seq[(ev[1], ev[2])]; key = ev[1]
                    else:
                        sem = ev[1].sem; val = ev[2]; key = id(ev[1])
                    if seen.get(key, 0) >= val:
                        continue
                    eng.wait_ge(sem, val); seen[key] = val
                if o['fn'] is None:
                    continue
                r = o['fn'](eng)
                if o['dma'] is not None:
                    for ins in (r if isinstance(r, (list, tuple)) else [r]):
                        ins.then_inc(o['dma'].sem, 16)
                elif o['inc']:
                    r.then_inc(self.esem[e], 1)

        with nc.Block() as block:
            block.sync(lambda eng: run(eng, 'sp'))
            block.scalar(lambda eng: run(eng, 'act'))
            block.vector(lambda eng: run(eng, 'dve'))
            block.gpsimd(lambda eng: run(eng, 'pool'))
            block.tensor(lambda eng: run(eng, 'pe'))


class Arena:
    def __init__(self, nc, ctx, nwords):
        self.t = ctx.enter_context(nc.sbuf_tensor('arena', [128, nwords], F32))
        self.n = nwords; self.top = 0

    def alloc(self, shape, dtype=F32):
        n = int(np.prod(shape))
        per = 2 if dtype == BF16 else 1
        words = (n + per - 1) // per
        words = (words + 1) // 2 * 2
        assert self.top + words <= self.n, ('arena OOM', self.top, words, self.n)
        ap = self.t[:, self.top:self.top + words]
        self.top += words
        if dtype != F32:
            ap = ap.bitcast(dtype)
        if dtype == BF16 and n != words * 2:
            ap = ap[:, 0:n]
        if len(shape) > 1:
            names = ' '.join('d%d' % i for i in range(len(shape)))
            ap = ap.rearrange('p (%s) -> p %s' % (names, names), **{'d%d' % i: s for i, s in enumerate(shape)})
        return ap


class K:
    def __init__(self, S):
        self.S = S

    def mm(self, out, lhsT, rhs, start, stop, R, W):
        self.S.op('pe', lambda e: e.matmul(out, lhsT=lhsT, rhs=rhs, start=start, stop=stop), R, W)

    def tr(self, out, in_, ident, R, W):
        self.S.op('pe', lambda e: e.transpose(out=out, in_=in_, identity=ident), R, W)

    def act(self, out, in_, func, R, W, scale=1.0, bias=0.0, accum_out=None):
        if accum_out is None:
            self.S.op('act', lambda e: e.activation(out=out, in_=in_, func=func, bias=bias, scale=scale), R, W)
        else:
            self.S.op('act', lambda e: e.activation(out=out, in_=in_, func=func, bias=bias, scale=scale,
                                                    accum_out=accum_out), R, W)

    def copy(self, eng, out, in_, R, W):
        if eng == 'act':
            self.S.op('act', lambda e: e.activation(out=out, in_=in_, func=AF.Copy), R, W)
        else:
            self.S.op(eng, lambda e: e.tensor_copy(out=out, in_=in_), R, W)

    def tt(self, eng, out, in0, in1, op, R, W):
        self.S.op(eng, lambda e: e.tensor_tensor(out=out, in0=in0, in1=in1, op=op), R, W)

    def ts(self, eng, out, in0, s1, s2, op0, op1, R, W, accum_out=None):
        if op1 is None:
            self.S.op(eng, lambda e: e.tensor_scalar(out=out, in0=in0, scalar1=s1, scalar2=None, op0=op0), R, W)
        elif accum_out is None:
            self.S.op(eng, lambda e: e.tensor_scalar(out=out, in0=in0, scalar1=s1, scalar2=s2, op0=op0, op1=op1), R, W)
        else:
            self.S.op(eng, lambda e: e.tensor_scalar(out=out, in0=in0, scalar1=s1, scalar2=s2, op0=op0, op1=op1,
                                                     accum_out=accum_out), R, W)

    def stt(self, out, in0, scalar, in1, op0, op1, R, W, accum_out=None):
        if accum_out is None:
            self.S.op('dve', lambda e: e.scalar_tensor_tensor(out=out, in0=in0, scalar=scalar, in1=in1, op0=op0, op1=op1), R, W)
        else:
            self.S.op('dve', lambda e: e.scalar_tensor_tensor(out=out, in0=in0, scalar=scalar, in1=in1, op0=op0, op1=op1,
                                                              accum_out=accum_out), R, W)

    def recip(self, out, in_, R, W):
        self.S.op('dve', lambda e: e.reciprocal(out=out, in_=in_), R, W)

    def memset(self, eng, ap, val, R, W):
        self.S.op(eng, lambda e: e.memset(ap, val), R, W)

    def dma(self, eng, out, in_, R, W, sem, slow=False):
        if slow:
            self.S.op(eng, lambda e: e.dma_start(out=out, in_=in_, allow_slow_non_contiguous=True), R, W, dma=sem)
        else:
            self.S.op(eng, lambda e: e.dma_start(out=out, in_=in_), R, W, dma=sem)

    def gather(self, out, table, idx_ap, R, W, sem):
        self.S.op('pool', lambda e: e.indirect_dma_start(
            out=out, out_offset=None, in_=table,
            in_offset=bass.IndirectOffsetOnAxis(ap=idx_ap, axis=0)), R, W, dma=sem)


class StopBuild(Exception):
    def __init__(self, items):
        self.items = items


def bc(ap, shape, axis):
    return ap.unsqueeze(axis).to_broadcast(shape)


def build(nseq=2, stop=None, peer_tiles=16, ntab=16384):
    nc = bass.Bass("TRN2", target_bir_lowering=False)

    def DI(name, shape, dt=F32):
        return nc.dram_tensor(name, shape, dt, kind="ExternalInput").ap()

    x = DI('x', [nseq, S_, D_])
    cT = DI('cT', [128, 16, 2])
    w_ada = DI('w_ada_r', [24, 128, 16, 512])
    b_adaT = DI('b_adaT', [128, 96])
    b_ada_row = DI('b_ada_row', [1, 12288])
    w_in = DI('w_in_r', [8, 128, 16, 512])
    w_in_kr = DI('w_in_kr', [128, 16, 64])
    g_qT = DI('g_qT', [128, 4]); g_kvT = DI('g_kvT', [128, 4])
    w_uqn = DI('w_uqn', [128, 4, 8, 128]); w_uqr = DI('w_uqr', [128, 4, 8, 64])
    w_uk = DI('w_uk_r', [128, 4, 1024]); w_uv = DI('w_uv_r', [128, 4, 1024])
    g_oaT = DI('g_oaT', [128, 8]); g_obT = DI('g_obT', [128, 8])
    w_o = DI('w_o_r', [128, 16, 2048])
    ln1_g = DI('ln1_g', [1, D_]); ln1_b = DI('ln1_b', [1, D_]); ln2_g = DI('ln2_g', [1, D_]); ln2_b = DI('ln2_b', [1, D_])
    w_pq = DI('w_pq_r', [16, 128, 16, 128])
    skT = DI('skT', [128, 2, 128])
    u_table = DI('u_table', [ntab, D_]); v_table = DI('v_table', [ntab, D_])
    ident_d = DI('ident', [128, 128])
    maskA_d = DI('maskA', [128, 31, 128]); maskC_d = DI('maskC', [128, 128])
    cosA_d = DI('cosA', [128, 16, 16]); sinA_d = DI('sinA', [128, 16, 16])
    cosB_d = DI('cosB', [128, 16, 32]); sinB_d = DI('sinB', [128, 16, 32])
    iota_d = DI('iota', [128, 256])
    out = nc.dram_tensor('out', [nseq, S_, D_], F32, kind="ExternalOutput").ap()
    dbgk = "ExternalOutput" if stop else "Internal"
    modrow = nc.dram_tensor('modrow', [nseq, 4, D_], F32, kind="Internal").ap()
    ssqscr = nc.dram_tensor('ssqscr', [nseq, S_], F32, kind="Internal").ap()
    x1scr = nc.dram_tensor('x1scr', [nseq, S_, D_], F32, kind=dbgk).ap()
    uvb = nc.dram_tensor('uvb', [ntab, 2 * D_], BF16, kind="Internal").ap()
    dbg = nc.dram_tensor('dbg', [128, 16, 2048], F32, kind=dbgk).ap() if stop else None

    ctx = ExitStack()
    with ctx:
        S = Sched(nc, ctx)
        k = K(S)
        A = Arena(nc, ctx, 53000)
        A.n = 53000 if not stop else 53000 - 0
        pf = [ctx.enter_context(nc.psum_tensor('pf%d' % i, [128, 512], F32)) for i in range(6)]
        pbf = [ctx.enter_context(nc.psum_tensor('pb%d' % i, [128, 512], F32)) for i in range(2)]
        pb = [t[:, :].bitcast(BF16) for t in pbf]
        pfB = [Buf('pf%d' % i, True) for i in range(6)]
        pbB = [Buf('pb%d' % i, True) for i in range(2)]
        nb_ = [0]

        bcnt = {}; bprev = {}

        def B(name='b'):
            nb_[0] += 1
            bcnt[name] = bcnt.get(name, 0) + 1
            key = (name, bcnt[name])
            nb = Buf('%s%d' % (name, nb_[0]))
            if key in bprev:
                nb.sem = bprev[key].sem; nb.semcnt = bprev[key].semcnt
            bprev[key] = nb
            return nb

        ident_f = A.alloc([128]); ident_b = A.alloc([128], BF16)
        maskA = A.alloc([31, 128], BF16); maskC = A.alloc([128], BF16)
        cosA = A.alloc([16, 16]); sinA = A.alloc([16, 16]); cosB = A.alloc([16, 32]); sinB = A.alloc([16, 32])
        sh1T = A.alloc([16, 2]); sc1T = A.alloc([16, 2])
        gq = A.alloc([4]); gkv = A.alloc([4]); goa = A.alloc([8]); gob = A.alloc([8])
        skT_s = A.alloc([2, 128])
        iota = A.alloc([256])
        constB = B('const')
        def cdma(eng, dst, src):
            cb = B('c'); k.dma(eng, dst, src, [], [cb], cb)
        cdma('sp', ident_f, ident_d)
        cdma('pool', ident_b, ident_d)
        cdma('pool', maskA, maskA_d)
        cdma('pool', maskC, maskC_d)
        for dst, src in ((cosA, cosA_d), (sinA, sinA_d), (cosB, cosB_d), (sinB, sinB_d), (gq, g_qT), (gkv, g_kvT),
                         (goa, g_oaT), (gob, g_obT), (skT_s, skT), (iota, iota_d)):
            cdma('sp', dst, src)
        P_END = A.top

        cact = A.alloc([16, 2]); badaT = A.alloc([96]); brow = A.alloc([12288]); rowt = [A.alloc([512]) for _ in range(2)]
        wblk0 = [A.alloc([16, 512]) for _ in range(2)]
        s0B = B('s0'); wB0 = [B('wada') for _ in range(2)]; rowB = [B('rowt') for _ in range(2)]; modB = B('modT'); mrB = B('modrow')
        s0b2 = B('s0b'); s0b3 = B('s0c')
        k.dma('sp', cact, cT, [], [s0B], s0B)
        k.dma('sp', badaT, b_adaT, [], [s0b2], s0b2)
        k.dma('sp', brow[0:1, :], b_ada_row, [], [s0b3], s0b3)
        k.act(cact, cact, AF.Silu, [s0B], [s0B])
        CB = [A.alloc([4096], BF16) for _ in range(3)]; CBB = [B('cb') for _ in range(3)]; CSB = [B('cs') for _ in range(3)]
        tabB = B('tab')
        ci_ = 0
        for src_t, off_t in ((u_table, 0), (v_table, D_)):
            for ci in range(ntab // 256):
                cb = CB[ci_ % 3]; cbB = CBB[ci_ % 3]; csB = CSB[ci_ % 3]; ci_ += 1
                k.dma('pool', cb, src_t[ci * 256:(ci + 1) * 256, :].rearrange('(p a) d -> p (a d)', a=2), [], [cbB], cbB)
                k.dma('act', uvb[ci * 256:(ci + 1) * 256, off_t:off_t + D_].rearrange('(p a) d -> p a d', a=2),
                      cb.rearrange('p (a d) -> p a d', a=2), [cbB], [tabB], csB)
        ri = 0
        for blk in range(24):
            wb = wblk0[blk % 2]; wbB = wB0[blk % 2]
            k.dma('sp', wb, w_ada[blk], [], [wbB], wbB)
            if blk < 8:
                for j in range(4):
                    ch = (blk % 4) * 4 + j
                    ps = pf[j % 2][:, 0:2]
                    for kc in range(16):
                        k.mm(ps, wb[:, kc, j * 128:(j + 1) * 128], cact[:, kc, :], kc == 0, kc == 15, [wbB, s0B], [pfB[j % 2]])
                    if blk < 4:
                        k.ts('dve', sh1T[:, ch, :], ps, badaT[:, blk * 4 + j:blk * 4 + j + 1], None, ALU.add, None, [pfB[j % 2], s0b2], [modB])
                    else:
                        k.ts('dve', sc1T[:, ch, :], ps, badaT[:, blk * 4 + j:blk * 4 + j + 1], 1.0, ALU.add, ALU.add, [pfB[j % 2], s0b2], [modB])
            else:
                slot = (blk - 8) // 4; q4 = (blk - 8) % 4
                for b in range(nseq):
                    pi = 2 + (ri % 2); ps = pf[pi][0:1, :]
                    for kc in range(16):
                        k.mm(ps, cact[:, kc, b:b + 1], wb[:, kc, :], kc == 0, kc == 15, [wbB, s0B], [pfB[pi]])
                    rt = rowt[ri % 2]; rB = rowB[ri % 2]
                    k.tt('dve', rt[0:1, :], ps, brow[0:1, blk * 512:(blk + 1) * 512], ALU.add, [pfB[pi], s0b3], [rB])
                    if slot == 2:
                        k.ts('dve', rt[0:1, :], rt[0:1, :], 1.0, None, ALU.add, None, [rB], [rB])
                    k.dma('sp', modrow[b, slot:slot + 1, q4 * 512:(q4 + 1) * 512], rt[0:1, :], [rB], [mrB], rB)
                    ri += 1
        S.barrier()
        A.top = P_END

        oTa = A.alloc([8, 2048], BF16)
        ssqA = A.alloc([16, 8]); ssqAB = B('ssqA')
        ssqB_ = A.alloc([16, 8]); ssqBB = B('ssqB')
        rab = A.alloc([2, 16]); rabB = B('rab')
        R1 = A.top
        hT = A.alloc([16, 2048], BF16)
        Z0 = A.top
        ZEND = A.n
        hTB = B('hT'); oTaB = B('oTa'); oTbB = B('oTb')

        try:
          bsnap = dict(bcnt)
          for b in range(nseq):
              bcnt.clear(); bcnt.update(bsnap)
              A.top = Z0
              XT = [A.alloc([2048]) for _ in range(2)]; XN = [A.alloc([2048], BF16) for _ in range(2)]
              STt = [A.alloc([4, 6]) for _ in range(2)]; MV = [A.alloc([8]) for _ in range(2)]
              XTB = [B('xt') for _ in range(2)]; XNB = [B('xn') for _ in range(2)]; STB = [B('st') for _ in range(2)]
              for tt in range(16):
                  xt = XT[tt % 2]; xn = XN[tt % 2]; st = STt[tt % 2]; mv = MV[tt % 2]
                  xtB = XTB[tt % 2]; xnB = XNB[tt % 2]; stB = STB[tt % 2]
                  k.dma('sp', xt, x[b, tt * 128:(tt + 1) * 128, :], [], [xtB], xtB)
                  for c4 in range(4):
                      S.op('dve', (lambda o, i: (lambda e: e.bn_stats(out=o, in_=i)))(st[:, c4, :], xt[:, c4 * 512:(c4 + 1) * 512]), [xtB], [stB])
                  S.op('dve', (lambda o, i: (lambda e: e.bn_aggr(out=o, in_=i)))(mv[:, 0:2], st.rearrange('p a b -> p (a b)')), [stB], [stB])
                  k.act(mv[:, 2:3], mv[:, 1:2], AF.Sqrt, [stB], [stB], bias=1e-5)
                  k.recip(mv[:, 3:4], mv[:, 2:3], [stB], [stB])
                  k.ts('dve', mv[:, 4:5], mv[:, 0:1], mv[:, 3:4], -1.0, ALU.mult, ALU.mult, [stB], [stB])
                  k.act(xn, xt, AF.Identity, [xtB, stB], [xnB], scale=mv[:, 3:4], bias=mv[:, 4:5])
                  for g4 in range(4):
                      pbi = g4 % 2
                      for j in range(4):
                          kc = g4 * 4 + j
                          k.tr(pb[pbi][:, j * 128:(j + 1) * 128], xn[:, kc * 128:(kc + 1) * 128], ident_b, [xnB, constB], [pbB[pbi]])
                      for j in range(4):
                          kc = g4 * 4 + j
                          o = hT[:, kc, tt * 128:(tt + 1) * 128]; i = pb[pbi][:, j * 128:(j + 1) * 128]
                          if pbi == 0:
                              k.act(o, i, AF.Identity, [pbB[pbi], modB], [hTB], scale=sc1T[:, kc, b:b + 1], bias=sh1T[:, kc, b:b + 1])
                          else:
                              k.ts('dve', o, i, sc1T[:, kc, b:b + 1], sh1T[:, kc, b:b + 1], ALU.mult, ALU.add, [pbB[pbi], modB], [hTB])
              if stop == 'S1':
                  S.barrier()
                  dt_ = A.alloc([2048]); dB = B('dbg')
                  for kc in range(16):
                      k.copy('dve', dt_, hT[:, kc, :], [hTB], [dB])
                      k.dma('sp', dbg[:, kc, :], dt_, [dB], [], dB)
                  break
              S.barrier()

              A.top = Z0
              QTM = [A.alloc([4, 128], BF16) for _ in range(2)]; QTMB = [B('qtm') for _ in range(2)]
              RT = [A.alloc([4, 4, 16]) for _ in range(2)]; RTB = [B('rt') for _ in range(2)]
              PE_ = [A.alloc([512], BF16) for _ in range(3)]; PEB = [B('pexp') for _ in range(3)]
              PM = [A.alloc([512], BF16) for _ in range(3)]; PMB = [B('pm') for _ in range(3)]
              OBF = [A.alloc([128], BF16) for _ in range(2)]; OBFB = [B('obf') for _ in range(2)]
              RL = [A.alloc([2]) for _ in range(2)]; RLB = [B('rl') for _ in range(2)]
              JK = A.alloc([512], BF16); JKB = B('junk')
              SMALL_END = A.top
              WB = [A.alloc([16, 512], BF16) for _ in range(2)]; WBB = [B('wblk') for _ in range(2)]
              QT = A.alloc([4, 2048], BF16); KT = A.alloc([4, 2048], BF16); V = A.alloc([16, 4, 130], BF16)
              QTB = B('QT'); KTB = B('KT'); VB = B('V')
              wi = 0
              it_ = 0
              for g in range(2):
                  k.memset('pool', V[:, :, :, 128:130], 1.0, [], [VB])
                  for typ in range(3):
                      blk = typ * 2 + g
                      wb = WB[wi % 2]; wbB = WBB[wi % 2]; wi += 1
                      k.dma('pool', wb, w_in[blk], [], [wbB], wbB)
                      if stop == 'B1':
                          raise StopBuild([(wb[:, 0, :], 512), (V[:, 0, :, :].rearrange('p a b -> p (a b)'), 520)])
                      for r in range(16):
                          pi = r % 2; ps = pf[pi]
                          for kc in range(16):
                              k.mm(ps[:, :], hT[:, kc, r:2048:16], wb[:, kc, :], kc == 0, kc == 15, [hTB, wbB], [pfB[pi]])
                          ps3 = ps[:, :].rearrange('p (h e) -> p h e', h=4)
                          if typ == 2:
                              k.copy('act' if r % 2 == 0 else 'dve', V[:, r, :, 0:128], ps3, [pfB[pi]], [VB])
                              continue
                          qi = it_ % 2; it_ += 1
                          qtm = QTM[qi]; qB = QTMB[qi]; rt = RT[qi]; rB = RTB[qi]
                          k.copy('act', qtm[:, :, 32:128], ps3[:, :, 32:128], [pfB[pi]], [qB])
                          if stop == 'B2a':
                              raise StopBuild([(qtm[:, 0, :], 128)])
                          cs = bc(cosA[:, r, :], [128, 4, 16], 1); sn = bc(sinA[:, r, :], [128, 4, 16], 1)
                          x1_ = ps3[:, :, 0:16]; x2_ = ps3[:, :, 16:32]
                          k.tt('dve', rt[:, 0], x1_, cs, ALU.mult, [pfB[pi], constB], [rB])
                          k.tt('dve', rt[:, 1], x2_, sn, ALU.mult, [pfB[pi], constB], [rB])
                          k.tt('dve', rt[:, 2], x2_, cs, ALU.mult, [pfB[pi], constB], [rB])
                          k.tt('dve', rt[:, 3], x1_, sn, ALU.mult, [pfB[pi], constB], [rB])
                          k.tt('dve', qtm[:, :, 0:16], rt[:, 0], rt[:, 1], ALU.subtract, [rB], [qB])
                          k.tt('dve', qtm[:, :, 16:32], rt[:, 2], rt[:, 3], ALU.add, [rB], [qB])
                          if stop == 'B2b':
                              raise StopBuild([(qtm[:, 0, :], 128), (rt[:, 0].rearrange('p a b -> p (a b)'), 64)])
                          pbi = qi
                          for hl in range(4):
                              k.tr(pb[pbi][:, hl * 128:(hl + 1) * 128], qtm[:, hl, :], ident_b, [qB, constB], [pbB[pbi]])
                          dst = (QT if typ == 0 else KT)[:, :, r * 128:(r + 1) * 128]
                          k.copy('act' if r % 2 == 1 else 'dve', dst, pb[pbi][:, 0:512].rearrange('p (h e) -> p h e', h=4),
                                 [pbB[pbi]], [QTB if typ == 0 else KTB])
                          if stop == 'B2':
                              raise StopBuild([(QT[:, 0, 0:128], 128), (qtm[:, 0, :], 128), (rt[:, 0].rearrange('p a b -> p (a b)'), 64)])
                  if stop == 'B3':
                      raise StopBuild([(QT[:, 0, :], 2048), (KT[:, 0, :], 2048), (V[:, 0:3, :, :].rearrange('p a b c -> p (a b c)'), 1560)])
                  batches = [(hl, c, rp) for hl in range(4) for c in range(4) for rp in range(16)]
                  LA = 2
                  SB = [(pf[0], pfB[0]), (pf[1], pfB[1]), (pbf[1], pbB[1])]

                  def qkA(n):
                      hl, c, rp = batches[n]
                      sps, spB = SB[n % 3]
                      k.mm(sps[:, :], KT[:, hl, rp * 128:(rp + 1) * 128], QT[:, hl, c * 512:(c + 1) * 512], True, True, [KTB, QTB], [spB])
                      pe_ = PE_[n % 3]; peB = PEB[n % 3]; pm = PM[n % 3]; pmB = PMB[n % 3]
                      k.act(pe_, sps[:, :], AF.Exp, [spB], [peB], scale=128.0 ** -0.5)
                      n0 = 15 - rp + 4 * c
                      k.tt('dve', pm, pe_, maskA[:, n0:n0 + 4, :].rearrange('p a b -> p (a b)'), ALU.mult, [peB, constB], [pmB])

                  def pvA(n):
                      hl, c, rp = batches[n]
                      h = g * 4 + hl
                      pm = PM[n % 3]; pmB = PMB[n % 3]
                      for j in range(4):
                          k.mm(pf[2 + j][:, 0:130], pm[:, j * 128:(j + 1) * 128], V[:, rp, hl, :], rp == 0, rp == 15, [pmB, VB], [pfB[2 + j]])
                      if rp == 15:
                          for j in range(4):
                              r = 4 * c + j; fi = j % 2; ops_ = pf[2 + j]; opB = pfB[2 + j]
                              k.recip(RL[fi][:, 0:1], ops_[:, 128:129], [opB], [RLB[fi]])
                              k.ts('dve', OBF[fi], ops_[:, 0:128], RL[fi][:, 0:1], None, ALU.mult, None, [opB, RLB[fi]], [OBFB[fi]])
                              k.act(JK[:, 0:128], ops_[:, 0:128], AF.Square, [opB, RLB[fi]], [JKB, ssqAB], scale=RL[fi][:, 0:1], accum_out=ssqA[:, r, h:h + 1])
                              k.tr(pb[0][:, fi * 128:(fi + 1) * 128], OBF[fi], ident_b, [OBFB[fi], constB], [pbB[0]])
                              k.act(oTa[:, h, r:2048:16], pb[0][:, fi * 128:(fi + 1) * 128], AF.Identity, [pbB[0], constB], [oTaB], scale=goa[:, h:h + 1])

                  for n in range(len(batches) + LA):
                      if n < len(batches):
                          qkA(n)
                      if n - LA >= 0:
                          pvA(n - LA)
              if stop == 'S3':
                  S.barrier()
                  A.top = SMALL_END
                  dt_ = A.alloc([2048]); dB = B('dbg')
                  for kc in range(8):
                      k.copy('dve', dt_, oTa[:, kc, :], [oTaB], [dB])
                      k.dma('sp', dbg[:, kc, :], dt_, [dB], [], dB)
                  k.copy('dve', dt_[:, 0:128], ssqA.rearrange('p a b -> p (a b)'), [ssqAB], [dB])
                  k.dma('sp', dbg[:, 8, 0:128], dt_[:, 0:128], [dB], [], dB)
                  break
              S.barrier()

              A.top = SMALL_END
              CQ0 = ZEND - 9216
              WB = [A.alloc([16, 512], BF16) for _ in range(2)]; WBB = [B('wblk') for _ in range(2)]
              WKR = A.alloc([16, 64], BF16); WKRB = B('wkr')
              CTM = [A.alloc([512], BF16) for _ in range(2)]; CTMB = [B('ctm') for _ in range(2)]
              SQ = [A.alloc([4]) for _ in range(2)]; SQB = [B('sq') for _ in range(2)]
              KRT = [A.alloc([4, 32]) for _ in range(2)]; KRTB = [B('krt') for _ in range(2)]
              KRM = [A.alloc([64], BF16) for _ in range(2)]; KRMB = [B('krm') for _ in range(2)]
              assert A.top <= CQ0
              save = A.top
              A.top = CQ0
              cqnT = A.alloc([4, 2048], BF16); ckvnT = A.alloc([4, 2048], BF16); krT = A.alloc([2048], BF16)
              cqB = B('cqnT'); ckvB = B('ckvnT'); krB = B('krT')
              A.top = save
              k.memset('pool', krT[64:128, :], 0.0, [], [krB])
              k.dma('pool', WB[0], w_in[6], [], [WBB[0]], WBB[0])
              k.dma('pool', WB[1], w_in[7], [], [WBB[1]], WBB[1])
              k.dma('pool', WKR, w_in_kr, [], [WKRB], WKRB)
              it_ = 0
              for typ in range(2):
                  wb = WB[typ]; wbB = WBB[typ]
                  dstT = cqnT if typ == 0 else ckvnT; dstB = cqB if typ == 0 else ckvB; gsc = gq if typ == 0 else gkv
                  for tt in range(16):
                      pi = tt % 2; ps = pf[pi]
                      for kc in range(16):
                          k.mm(ps[:, :], hT[:, kc, tt * 128:(tt + 1) * 128], wb[:, kc, :], kc == 0, kc == 15, [hTB, wbB], [pfB[pi]])
                      qi = it_ % 2; it_ += 1
                      sq = SQ[qi]; sqB = SQB[qi]; ctm = CTM[qi]; ctB = CTMB[qi]
                      k.act(JK, ps[:, :], AF.Square, [pfB[pi]], [JKB, sqB], accum_out=sq[:, 0:1])
                      k.act(sq[:, 1:2], sq[:, 0:1], AF.Sqrt, [sqB], [sqB], scale=1.0 / 512, bias=1e-6)
                      k.recip(sq[:, 2:3], sq[:, 1:2], [sqB], [sqB])
                      k.ts('dve', ctm, ps[:, :], sq[:, 2:3], None, ALU.mult, None, [pfB[pi], sqB], [ctB])
                      for j in range(4):
                          k.tr(pb[qi][:, j * 128:(j + 1) * 128], ctm[:, j * 128:(j + 1) * 128], ident_b, [ctB, constB], [pbB[qi]])
                      k.tt('dve', dstT[:, :, tt * 128:(tt + 1) * 128], pb[qi][:, 0:512].rearrange('p (a b) -> p a b', a=4),
                           bc(gsc, [128, 4, 128], 2), ALU.mult, [pbB[qi], constB], [dstB])
              for tt in range(16):
                  pi = 2 + tt % 2; ps = pf[pi]
                  for kc in range(16):
                      k.mm(ps[:, 0:64], hT[:, kc, tt * 128:(tt + 1) * 128], WKR[:, kc, :], kc == 0, kc == 15, [hTB, WKRB], [pfB[pi]])
                  qi = tt % 2; rt = KRT[qi]; rB = KRTB[qi]; km = KRM[qi]; kmB = KRMB[qi]
                  cs = cosB[:, tt, :]; sn = sinB[:, tt, :]
                  k.tt('dve', rt[:, 0], ps[:, 0:32], cs, ALU.mult, [pfB[pi], constB], [rB])
                  k.tt('dve', rt[:, 1], ps[:, 32:64], sn, ALU.mult, [pfB[pi], constB], [rB])
                  k.tt('dve', rt[:, 2], ps[:, 32:64], cs, ALU.mult, [pfB[pi], constB], [rB])
                  k.tt('dve', rt[:, 3], ps[:, 0:32], sn, ALU.mult, [pfB[pi], constB], [rB])
                  k.tt('dve', km[:, 0:32], rt[:, 0], rt[:, 1], ALU.subtract, [rB], [kmB])
                  k.tt('dve', km[:, 32:64], rt[:, 2], rt[:, 3], ALU.add, [rB], [kmB])
                  k.tr(pb[qi][0:64, 0:128], km, ident_b, [kmB, constB], [pbB[qi]])
                  k.copy('act', krT[0:64, tt * 128:(tt + 1) * 128], pb[qi][0:64, 0:128], [pbB[qi]], [krB])
              S.barrier()

              A.top = R1
              oTb = A.alloc([8, 2048], BF16)
              wuqn_s = A.alloc([4, 8, 128], BF16); wuqr_s = A.alloc([4, 8, 64], BF16)
              wuk_s = A.alloc([4, 1024], BF16); wuv_s = A.alloc([4, 1024], BF16)
              mwQN = B('mwqn'); mwQR = B('mwqr'); mwK = B('mwk'); mwV = B('mwv')
              assert A.top <= Z0
              k.dma('pool', wuqn_s, w_uqn, [], [mwQN], mwQN)
              k.dma('pool', wuqr_s, w_uqr, [], [mwQR], mwQR)
              k.dma('pool', wuk_s, w_uk, [], [mwK], mwK)
              k.dma('pool', wuv_s, w_uv, [], [mwV], mwV)
              A.top = SMALL_END
              qnT = A.alloc([2, 2048], BF16); knT = A.alloc([2, 2048], BF16); qrT = A.alloc([2, 2048], BF16)
              Vb = A.alloc([16, 2, 130], BF16)
              qnB = B('qnT'); knB = B('knT'); qrB = B('qrT'); VbB = B('Vb')
              QRM = [A.alloc([2, 64], BF16) for _ in range(2)]; QRMB = [B('qrm') for _ in range(2)]
              QRT = [A.alloc([4, 2, 32]) for _ in range(2)]; QRTB = [B('qrt') for _ in range(2)]
              assert A.top <= CQ0
              k.memset('pool', qrT[64:128, :, :], 0.0, [], [qrB])
              for g in range(4):
                  h0 = g * 2
                  k.memset('pool', Vb[:, :, :, 128:130], 1.0, [], [VbB])
                  ei = 0
                  for hl in range(2):
                      h = h0 + hl
                      for typ in range(2):
                          for ch in range(4):
                              pi = ei % 2; ps = pf[pi]
                              for kc in range(4):
                                  if typ == 0:
                                      k.mm(ps[:, :], wuqn_s[:, kc, h, :], cqnT[:, kc, ch * 512:(ch + 1) * 512], kc == 0, kc == 3, [mwQN, cqB], [pfB[pi]])
                                  else:
                                      k.mm(ps[:, :], wuk_s[:, kc, h * 128:(h + 1) * 128], ckvnT[:, kc, ch * 512:(ch + 1) * 512], kc == 0, kc == 3, [mwK, ckvB], [pfB[pi]])
                              dst = (qnT if typ == 0 else knT)[:, hl, ch * 512:(ch + 1) * 512]
                              k.copy('act' if ei % 2 == 0 else 'dve', dst, ps[:, :], [pfB[pi]], [qnB if typ == 0 else knB])
                              ei += 1
                  for tt in range(16):
                      pi = 2 + tt % 2; ps = pf[pi]
                      for kc in range(4):
                          k.mm(ps[:, 0:128], cqnT[:, kc, tt * 128:(tt + 1) * 128], wuqr_s[:, kc, h0:h0 + 2, :].rearrange('p a b -> p (a b)'),
                               kc == 0, kc == 3, [mwQR, cqB], [pfB[pi]])
                      qi = tt % 2; rt = QRT[qi]; rB = QRTB[qi]; qm = QRM[qi]; qmB = QRMB[qi]
                      ps3 = ps[:, 0:128].rearrange('p (h e) -> p h e', h=2)
                      cs = bc(cosB[:, tt, :], [128, 2, 32], 1); sn = bc(sinB[:, tt, :], [128, 2, 32], 1)
                      k.tt('dve', rt[:, 0], ps3[:, :, 0:32], cs, ALU.mult, [pfB[pi], constB], [rB])
                      k.tt('dve', rt[:, 1], ps3[:, :, 32:64], sn, ALU.mult, [pfB[pi], constB], [rB])
                      k.tt('dve', rt[:, 2], ps3[:, :, 32:64], cs, ALU.mult, [pfB[pi], constB], [rB])
                      k.tt('dve', rt[:, 3], ps3[:, :, 0:32], sn, ALU.mult, [pfB[pi], constB], [rB])
                      k.tt('dve', qm[:, :, 0:32], rt[:, 0], rt[:, 1], ALU.subtract, [rB], [qmB])
                      k.tt('dve', qm[:, :, 32:64], rt[:, 2], rt[:, 3], ALU.add, [rB], [qmB])
                      for hl in range(2):
                          k.tr(pb[qi][0:64, hl * 128:(hl + 1) * 128], qm[:, hl, :], ident_b, [qmB, constB], [pbB[qi]])
                      k.copy('act', qrT[0:64, :, tt * 128:(tt + 1) * 128], pb[qi][0:64, 0:256].rearrange('p (h e) -> p h e', h=2), [pbB[qi]], [qrB])
                      pi = 4 + tt % 2; ps = pf[pi]
                      for kc in range(4):
                          k.mm(ps[:, 0:256], ckvnT[:, kc, tt * 128:(tt + 1) * 128], wuv_s[:, kc, h0 * 128:(h0 + 2) * 128], kc == 0, kc == 3, [mwV, ckvB], [pfB[pi]])
                      k.copy('dve', Vb[:, tt, :, 0:128], ps[:, 0:256].rearrange('p (h e) -> p h e', h=2), [pfB[pi]], [VbB])
                  if g == 3:
                      save_top = A.top
                      A.top = R1 + 8192
                      woA_ = A.alloc([8, 2048], BF16)
                      assert A.top <= Z0
                      A.top = CQ0
                      woB_ = A.alloc([8, 2048], BF16)
                      A.top = save_top
                      woB = [B('wo') for _ in range(4)]
                      for q4 in range(2):
                          k.dma('pool', woA_[:, q4 * 4:(q4 + 1) * 4, :], w_o[:, q4 * 4:(q4 + 1) * 4, :], [], [woB[q4], mwQN, mwQR, mwK, mwV], woB[q4])
                      for q4 in range(2):
                          k.dma('pool', woB_[:, q4 * 4:(q4 + 1) * 4, :], w_o[:, 8 + q4 * 4:8 + (q4 + 1) * 4, :], [], [woB[2 + q4], cqB, ckvB], woB[2 + q4])
                  batches = [(hl, c, kt) for hl in range(2) for c in range(4) for kt in range(4 * c + 4)]
                  LA = 2
                  SB = [(pf[0], pfB[0]), (pf[1], pfB[1]), (pbf[1], pbB[1])]

                  def qkB(n):
                      hl, c, kt = batches[n]
                      sps, spB = SB[n % 3]
                      k.mm(sps[:, :], knT[:, hl, kt * 128:(kt + 1) * 128], qnT[:, hl, c * 512:(c + 1) * 512], True, False, [knB, qnB], [spB])
                      k.mm(sps[:, :], krT[:, kt * 128:(kt + 1) * 128], qrT[:, hl, c * 512:(c + 1) * 512], False, True, [krB, qrB], [spB])
                      pe_ = PE_[n % 3]; peB = PEB[n % 3]; pm = PM[n % 3]; pmB = PMB[n % 3]
                      j0_ = max(0, kt - 4 * c)
                      k.act(pe_[:, j0_ * 128:512], sps[:, j0_ * 128:512], AF.Exp, [spB], [peB], scale=192.0 ** -0.5)
                      if kt >= 4 * c:
                          k.tt('dve', pm[:, 0:128], pe_[:, j0_ * 128:(j0_ + 1) * 128], maskC, ALU.mult, [peB, constB], [pmB])

                  def pvB(n):
                      hl, c, kt = batches[n]
                      h = h0 + hl
                      pe_ = PE_[n % 3]; peB = PEB[n % 3]; pm = PM[n % 3]; pmB = PMB[n % 3]
                      for j in range(4):
                          qt = 4 * c + j
                          if kt > qt:
                              continue
                          ops_ = pf[2 + j]; opB = pfB[2 + j]
                          if kt == qt:
                              k.mm(ops_[:, 0:130], pm[:, 0:128], Vb[:, kt, hl, :], kt == 0, True, [pmB, VbB], [opB])
                              fi = j % 2
                              k.recip(RL[fi][:, 0:1], ops_[:, 128:129], [opB], [RLB[fi]])
                              k.ts('dve', OBF[fi], ops_[:, 0:128], RL[fi][:, 0:1], None, ALU.mult, None, [opB, RLB[fi]], [OBFB[fi]])
                              k.act(JK[:, 0:128], ops_[:, 0:128], AF.Square, [opB, RLB[fi]], [JKB, ssqBB], scale=RL[fi][:, 0:1], accum_out=ssqB_[:, qt, h:h + 1])
                              k.tr(pb[0][:, fi * 128:(fi + 1) * 128], OBF[fi], ident_b, [OBFB[fi], constB], [pbB[0]])
                              k.act(oTb[:, h, qt * 128:(qt + 1) * 128], pb[0][:, fi * 128:(fi + 1) * 128], AF.Identity, [pbB[0], constB], [oTbB], scale=gob[:, h:h + 1])
                          else:
                              k.mm(ops_[:, 0:130], pe_[:, j * 128:(j + 1) * 128], Vb[:, kt, hl, :], kt == 0, False, [peB, VbB], [opB])

                  for n in range(len(batches) + LA):
                      if n < len(batches):
                          qkB(n)
                      if n - LA >= 0:
                          pvB(n - LA)
              sA = A.alloc([16]); sAn = A.alloc([16]); sBn = A.alloc([16]); sB_ = B('ssum')
              k.S.op('dve', (lambda o, i: (lambda e: e.tensor_reduce(out=o, in_=i, axis=AX.X, op=ALU.add)))(sA, ssqA), [ssqAB], [sB_])
              k.S.op('dve', (lambda o, i: (lambda e: e.tensor_reduce(out=o, in_=i, axis=AX.X, op=ALU.add)))(sBn, ssqB_), [ssqBB], [sB_])
              ssB = B('ssqscr')
              k.dma('sp', ssqscr[b].rearrange('(i r) -> i r', r=16), sA, [sB_], [ssB], sB_)
              k.dma('sp', sAn, ssqscr[b].rearrange('(t p) -> p t', p=128), [ssB], [sB_], sB_, slow=True)
              k.act(rab[:, 0, :], sAn, AF.Sqrt, [sB_], [rabB], scale=1.0 / 1024, bias=1e-6)
              k.act(rab[:, 1, :], sBn, AF.Sqrt, [sB_], [rabB], scale=1.0 / 1024, bias=1e-6)
              k.recip(rab.rearrange('p a b -> p (a b)'), rab.rearrange('p a b -> p (a b)'), [rabB], [rabB])
              if stop == 'S6':
                  S.barrier()
                  A.top = SMALL_END
                  dt_ = A.alloc([2048]); dB = B('dbg')
                  for kc in range(16):
                      k.copy('dve', dt_, (oTa if kc < 8 else oTb)[:, kc % 8, :], [oTaB, oTbB], [dB])
                      k.dma('sp', dbg[:, kc, :], dt_, [dB], [], dB)
                  break
              S.barrier()

              A.top = Z0
              G1 = A.alloc([2048]); L1G = A.alloc([2048]); L1B = A.alloc([2048]); G1B = B('g1'); L1GB = B('l1g'); L1BB = B('l1b')
              k.dma('sp', G1, modrow[b, 0:1, :].to_broadcast([128, 2048]), [mrB], [G1B], G1B)
              k.dma('sp', L1G, ln1_g.to_broadcast([128, 2048]), [], [L1GB], L1GB)
              k.dma('sp', L1B, ln1_b.to_broadcast([128, 2048]), [], [L1BB], L1BB)
              XT = [A.alloc([2048]) for _ in range(2)]; XTB = [B('xt') for _ in range(2)]
              YT = [A.alloc([2048]) for _ in range(2)]; YTB = [B('yt') for _ in range(2)]
              STt = [A.alloc([4, 6]) for _ in range(2)]; MV = [A.alloc([8]) for _ in range(2)]; STB = [B('st') for _ in range(2)]
              assert A.top <= CQ0, (A.top, CQ0)
              A.top = CQ0 + 8192
              TM = [A.alloc([512]) for _ in range(2)]; TMB = [B('tm') for _ in range(2)]
              x1B = B('x1scr')
              ti = 0
              for tt in range(16):
                  xt = XT[tt % 2]; xtB = XTB[tt % 2]; yt = YT[tt % 2]; ytB = YTB[tt % 2]
                  st = STt[tt % 2]; mv = MV[tt % 2]; stB = STB[tt % 2]
                  k.dma('sp', xt, x[b, tt * 128:(tt + 1) * 128, :], [], [xtB], xtB)
                  for nb in range(4):
                      pa = pf[(nb % 2) * 2]; paB = pfB[(nb % 2) * 2]; pb_ = pf[(nb % 2) * 2 + 1]; pbB_ = pfB[(nb % 2) * 2 + 1]
                      for fc in range(8):
                          k.mm(pa[:, :], oTa[:, fc, tt * 128:(tt + 1) * 128], woA_[:, fc, nb * 512:(nb + 1) * 512], fc == 0, fc == 7, [oTaB, woB[fc // 4]], [paB])
                      for fc in range(8):
                          k.mm(pb_[:, :], oTb[:, fc, tt * 128:(tt + 1) * 128], woB_[:, fc, nb * 512:(nb + 1) * 512], fc == 0, fc == 7, [oTbB, woB[2 + fc // 4]], [pbB_])
                      tm = TM[ti % 2]; tmB = TMB[ti % 2]; ti += 1
                      sl = slice(nb * 512, (nb + 1) * 512)
                      k.act(tm, pa[:, :], AF.Identity, [paB, rabB], [tmB], scale=rab[:, 0, tt:tt + 1])
                      k.stt(tm, pb_[:, :], rab[:, 1, tt:tt + 1], tm, ALU.mult, ALU.add, [pbB_, rabB, tmB], [tmB])
                      k.tt('dve', tm, tm, G1[:, sl], ALU.mult, [tmB, G1B], [tmB])
                      k.stt(yt[:, sl], xt[:, sl], ALPHA, tm, ALU.mult, ALU.add, [xtB, tmB], [ytB])
                      S.op('dve', (lambda o, i: (lambda e: e.bn_stats(out=o, in_=i)))(st[:, nb, :], yt[:, sl]), [ytB], [stB])
                  S.op('dve', (lambda o, i: (lambda e: e.bn_aggr(out=o, in_=i)))(mv[:, 0:2], st.rearrange('p a b -> p (a b)')), [stB], [stB])
                  k.act(mv[:, 2:3], mv[:, 1:2], AF.Sqrt, [stB], [stB], bias=1e-5)
                  k.recip(mv[:, 3:4], mv[:, 2:3], [stB], [stB])
                  k.ts('dve', mv[:, 4:5], mv[:, 0:1], mv[:, 3:4], -1.0, ALU.mult, ALU.mult, [stB], [stB])
                  k.act(yt, yt, AF.Identity, [ytB, stB], [ytB], scale=mv[:, 3:4], bias=mv[:, 4:5])
                  k.tt('dve', yt, yt, L1G, ALU.mult, [ytB, L1GB], [ytB])
                  k.tt('dve', yt, yt, L1B, ALU.add, [ytB, L1BB], [ytB])
                  k.dma('sp', x1scr[b, tt * 128:(tt + 1) * 128, :], yt, [ytB], [x1B], ytB)
              if stop == 'S7':
                  break
              S.barrier()

              A.top = P_END
              BC = [A.alloc([2048]) for _ in range(5)]; BCB = [B('bc2') for _ in range(5)]
              k.dma('sp', BC[0], modrow[b, 2:3, :].to_broadcast([128, 2048]), [mrB], [BCB[0]], BCB[0])
              k.dma('sp', BC[1], modrow[b, 1:2, :].to_broadcast([128, 2048]), [mrB], [BCB[1]], BCB[1])
              k.dma('sp', BC[2], modrow[b, 3:4, :].to_broadcast([128, 2048]), [mrB], [BCB[2]], BCB[2])
              k.dma('sp', BC[3], ln2_g.to_broadcast([128, 2048]), [], [BCB[3]], BCB[3])
              k.dma('sp', BC[4], ln2_b.to_broadcast([128, 2048]), [], [BCB[4]], BCB[4])
              X1 = [A.alloc([2048]) for _ in range(2)]; X1B = [B('x1') for _ in range(2)]
              ACC = A.alloc([2048]); ACCB = B('acc')
              H2 = ACC; H2B = ACCB
              H2b = [A.alloc([2048], BF16) for _ in range(2)]; H2bB = [B('h2b') for _ in range(2)]
              H2T = A.alloc([16, 128]); H2TB = B('h2T')
              WPQ = [A.alloc([16, 128]) for _ in range(2)]; WPQB = [B('wpq') for _ in range(2)]
              QTC = [A.alloc([128]) for _ in range(2)]; QTCB = [B('qtc') for _ in range(2)]
              SC = A.alloc([16, 128]); SCB = B('sc')
              WK = [A.alloc([128]) for _ in range(2)]; WKB = [B('wk') for _ in range(2)]
              TV = A.alloc([16, 16]); TVB = B('tv')
              TI = A.alloc([16, 16], U32); TIF = A.alloc([16, 16]); TIB = B('ti')
              CAND = A.alloc([8, 256]); CANDB = B('cand')
              WK2 = A.alloc([256]); WK2B = B('wk2')
              VALS = A.alloc([8, 16]); VALSB = B('vals')
              POS = A.alloc([8, 16], U32); POSB = B('pos')
              ABU = A.alloc([2, 128], U32); ABF = A.alloc([2, 8, 16]); ABB = B('ab')
              OH = CAND.rearrange('p h (a b) -> p h a b', a=16); OHB = CANDB
              SEL = A.alloc([2, 8, 16]); SELB = B('sel')
              EIDX = A.alloc([128]); EB0 = B('eidxf')
              EIDXU = [A.alloc([128], U32) for _ in range(2)]; EB = [B('eidx') for _ in range(2)]
              GT2 = [A.alloc([128]) for _ in range(2)]; GT2B = [B('gates') for _ in range(2)]
              GS = A.alloc([24]); GSB = B('gs')
              AA = A.alloc([128]); AAB = B('aa')
              WW = A.alloc([128]); WWB = B('ww')
              NGB = 7
              GB_ = [A.alloc([4096], BF16) for _ in range(NGB)]; GBB = [B('gb') for _ in range(NGB)]
              TG = A.alloc([128])
              DG = [A.alloc([128], BF16) for _ in range(4)]; DGB = [B('dg') for _ in range(4)]
              if b == 0:
                  print('S8 arena top', A.top, 'of', A.n)
              STt = [A.alloc([4, 6]) for _ in range(2)]; MV = [A.alloc([8]) for _ in range(2)]; STB = [B('st') for _ in range(2)]
              ST2 = A.alloc([4, 6]); MV2 = A.alloc([8]); ST2B = B('st2')
              outB = B('out')
              cnt = {'wq': 0, 'gi': 0}

              def ln_stats(src, srcB, st, mv, stB):
                  for c4 in range(4):
                      S.op('dve', (lambda o, i: (lambda e: e.bn_stats(out=o, in_=i)))(st[:, c4, :], src[:, c4 * 512:(c4 + 1) * 512]), [srcB], [stB])
                  S.op('dve', (lambda o, i: (lambda e: e.bn_aggr(out=o, in_=i)))(mv[:, 0:2], st.rearrange('p a b -> p (a b)')), [stB], [stB])
                  k.act(mv[:, 2:3], mv[:, 1:2], AF.Sqrt, [stB], [stB], bias=1e-5)
                  k.recip(mv[:, 3:4], mv[:, 2:3], [stB], [stB])
                  k.ts('dve', mv[:, 4:5], mv[:, 0:1], mv[:, 3:4], -1.0, ALU.mult, ALU.mult, [stB], [stB])

              def stageA(tt):
                  x1 = X1[tt % 2]; x1B_ = X1B[tt % 2]; st = STt[tt % 2]; mv = MV[tt % 2]; stB = STB[tt % 2]
                  h2b = H2b[tt % 2]; h2bB = H2bB[tt % 2]
                  k.dma('sp', x1, x1scr[b, tt * 128:(tt + 1) * 128, :], [x1B], [x1B_], x1B_)
                  ln_stats(x1, x1B_, st, mv, stB)
                  k.act(H2, x1, AF.Identity, [x1B_, stB], [H2B], scale=mv[:, 3:4], bias=mv[:, 4:5])
                  k.tt('dve', H2, H2, BC[0], ALU.mult, [H2B, BCB[0]], [H2B])
                  k.tt('dve', H2, H2, BC[1], ALU.add, [H2B, BCB[1]], [H2B])
                  k.copy('act', h2b, H2, [H2B], [h2bB])
                  for g4 in range(4):
                      pi = g4 % 2
                      for j in range(4):
                          kc = g4 * 4 + j
                          k.tr(pf[pi][:, j * 128:(j + 1) * 128], H2[:, kc * 128:(kc + 1) * 128], ident_f, [H2B, constB], [pfB[pi]])
                      k.copy('act', H2T[:, g4 * 4:(g4 + 1) * 4, :], pf[pi][:, :].rearrange('p (a b) -> p a b', a=4), [pfB[pi]], [H2TB])
                  for c in range(16):
                      wp = WPQ[cnt['wq'] % 2]; wpB = WPQB[cnt['wq'] % 2]; cnt['wq'] += 1
                      k.dma('sp', wp, w_pq[c], [], [wpB], wpB)
                      pi = c % 2
                      for kc in range(16):
                          k.mm(pf[pi][:, 0:128], wp[:, kc, :], H2T[:, kc, :], kc == 0, kc == 15, [wpB, H2TB], [pfB[pi]])
                      qc = QTC[c % 2]; qcB = QTCB[c % 2]
                      k.copy('act', qc, pf[pi][:, 0:128], [pfB[pi]], [qcB])
                      si = (c // 4) % 2
                      k.mm(pbf[si][:, (c % 4) * 128:(c % 4 + 1) * 128], qc, skT_s[:, c % 2, :], True, True, [qcB, constB], [pbB[si]])
                      if c % 4 == 3:
                          k.copy('act', SC[:, c - 3:c + 1, :], pbf[si][:, :].rearrange('p (a b) -> p a b', a=4), [pbB[si]], [SCB])

              def stageT(tt):
                  eu = EIDXU[tt % 2]; eB = EB[tt % 2]; GT = GT2[tt % 2]; GTB = GT2B[tt % 2]
                  for c in range(16):
                      wk = WK[c % 2]; wkB = WKB[c % 2]
                      S.op('dve', (lambda o, i: (lambda e: e.max(out=o, in_=i)))(TV[:, c, 0:8], SC[:, c, :]), [SCB], [TVB])
                      yield
                      S.op('dve', (lambda o, m, i: (lambda e: e.max_index(out=o, in_max=m, in_values=i)))(TI[:, c, 0:8], TV[:, c, 0:8], SC[:, c, :]), [SCB, TVB], [TIB])
                      yield
                      S.op('dve', (lambda o, m, i: (lambda e: e.match_replace(out=o, in_to_replace=m, in_values=i, imm_value=NEG)))(wk, TV[:, c, 0:8], SC[:, c, :]), [SCB, TVB], [wkB])
                      yield
                      S.op('dve', (lambda o, i: (lambda e: e.max(out=o, in_=i)))(TV[:, c, 8:16], wk), [wkB], [TVB])
                      yield
                      S.op('dve', (lambda o, m, i: (lambda e: e.max_index(out=o, in_max=m, in_values=i)))(TI[:, c, 8:16], TV[:, c, 8:16], wk), [wkB, TVB], [TIB])
                      yield
                  k.copy('dve', TIF, TI, [TIB], [TIB])
                  yield
                  tv4 = TV.rearrange('p (h t) k -> p h t k', t=2); ti4 = TIF.rearrange('p (h t) k -> p h t k', t=2)
                  k.tt('dve', CAND.rearrange('p h (a b) -> p h a b', a=16), bc(tv4[:, :, 0, :], [128, 8, 16, 16], 3), bc(tv4[:, :, 1, :], [128, 8, 16, 16], 2),
                       ALU.add, [TVB], [CANDB])
                  yield
                  for hh in range(8):
                      S.op('dve', (lambda o, i: (lambda e: e.max(out=o, in_=i)))(VALS[:, hh, 0:8], CAND[:, hh, :]), [CANDB], [VALSB])
                      yield
                      S.op('dve', (lambda o, m, i: (lambda e: e.max_index(out=o, in_max=m, in_values=i)))(POS[:, hh, 0:8], VALS[:, hh, 0:8], CAND[:, hh, :]), [CANDB, VALSB], [POSB])
                      yield
                      S.op('dve', (lambda o, m, i: (lambda e: e.match_replace(out=o, in_to_replace=m, in_values=i, imm_value=NEG)))(WK2, VALS[:, hh, 0:8], CAND[:, hh, :]), [CANDB, VALSB], [WK2B])
                      yield
                      S.op('dve', (lambda o, i: (lambda e: e.max(out=o, in_=i)))(VALS[:, hh, 8:16], WK2), [WK2B], [VALSB])
                      yield
                      S.op('dve', (lambda o, m, i: (lambda e: e.max_index(out=o, in_max=m, in_values=i)))(POS[:, hh, 8:16], VALS[:, hh, 8:16], WK2), [WK2B, VALSB], [POSB])
                      yield
                  posf = POS.rearrange('p h k -> p (h k)')
                  S.op('dve', (lambda o, i: (lambda e: e.tensor_single_scalar(out=o, in_=i, scalar=4, op=ALU.logical_shift_right)))(ABU[:, 0, :], posf), [POSB], [ABB])
                  yield
                  S.op('dve', (lambda o, i: (lambda e: e.tensor_single_scalar(out=o, in_=i, scalar=15, op=ALU.bitwise_and)))(ABU[:, 1, :], posf), [POSB], [ABB])
                  yield
                  k.copy('dve', ABF.rearrange('p t h k -> p (t h k)'), ABU.rearrange('p t n -> p (t n)'), [ABB], [ABB])
                  yield
                  io16 = iota[:, 0:16].unsqueeze(1).unsqueeze(1).to_broadcast([128, 8, 16, 16])
                  for t2 in range(2):
                      k.tt('dve', OH, io16, bc(ABF[:, t2], [128, 8, 16, 16], 3), ALU.is_equal, [ABB, constB], [OHB])
                      yield
                      k.tt('dve', OH, OH, bc(ti4[:, :, t2, :], [128, 8, 16, 16], 2), ALU.mult, [OHB, TIB], [OHB])
                      yield
                      S.op('dve', (lambda o, i: (lambda e: e.tensor_reduce(out=o, in_=i, axis=AX.X, op=ALU.add)))(SEL[:, t2], OH), [OHB], [SELB])
                      yield
                  k.stt(EIDX.rearrange('p (h k) -> p h k', h=8), SEL[:, 0], 128.0, SEL[:, 1], ALU.mult, ALU.add, [SELB], [EB0])
                  yield
                  k.copy('dve', eu, EIDX, [EB0], [eB])
                  yield
                  k.S.op('dve', (lambda o, i: (lambda e: e.tensor_reduce(out=o, in_=i, axis=AX.X, op=ALU.max)))(GS[:, 0:8], VALS), [VALSB], [GSB])
                  yield
                  k.ts('dve', GS[:, 8:16], GS[:, 0:8], -1.0, None, ALU.mult, None, [GSB], [GSB])
                  yield
                  for hh in range(8):
                      k.act(GT[:, hh * 16:(hh + 1) * 16], VALS[:, hh, :], AF.Exp, [VALSB, GSB], [GTB, GSB], bias=GS[:, 8 + hh:9 + hh], accum_out=GS[:, 16 + hh:17 + hh])
                      yield
                  k.recip(GS[:, 0:8], GS[:, 16:24], [GSB], [GSB])
                  yield
                  k.tt('dve', GT.rearrange('p (h k) -> p h k', h=8), GT.rearrange('p (h k) -> p h k', h=8), bc(GS[:, 0:8], [128, 8, 16], 2), ALU.mult, [GTB, GSB], [GTB])
                  yield

              def capture(stage, *args):
                  lst = []
                  real = S.op
                  S.op = lambda eng, fn, reads=(), writes=(), dma=None, ndma=1: lst.append((eng, fn, list(reads), list(writes), dma, ndma))
                  try:
                      r = stage(*args)
                      if r is not None:
                          for _ in r:
                              pass
                  finally:
                      S.op = real
                  return lst

              def replay(lst, n):
                  for _ in range(min(n, len(lst))):
                      eng, fn, R, W, dma, ndma = lst.pop(0)
                      S.op(eng, fn, R, W, dma=dma, ndma=ndma)

              def stageUV(tt, aops, tops):
                  eu = EIDXU[tt % 2]; eB = EB[tt % 2]; h2b = H2b[tt % 2]; h2bB = H2bB[tt % 2]; GT = GT2[tt % 2]; GTB = GT2B[tt % 2]
                  for kk in range(128):
                      gb = GB_[cnt['gi'] % NGB]; gbB = GBB[cnt['gi'] % NGB]; cnt['gi'] += 1
                      aB = Buf('aa'); wB = Buf('ww')
                      k.gather(gb, uvb, eu[:, kk:kk + 1], [eB, tabB], [gbB], gbB)
                      k.stt(gb[:, 0:2048], gb[:, 0:2048], 1.0, h2b, ALU.mult, ALU.mult, [gbB, h2bB], [aB, gbB], accum_out=AA[:, kk:kk + 1])
                      k.act(TG[:, kk:kk + 1], AA[:, kk:kk + 1], AF.Gelu, [aB], [wB])
                      k.act(WW[:, kk:kk + 1], TG[:, kk:kk + 1], AF.Identity, [wB, GTB], [wB], scale=GT[:, kk:kk + 1])
                      dg = DG[kk % 4]; dgB = DGB[kk % 4]
                      k.act(dg, ident_f, AF.Identity, [wB, constB], [dgB], scale=WW[:, kk:kk + 1])
                      for nb in range(4):
                          k.mm(pf[2 + nb][:, :], dg, gb[:, 2048 + nb * 512:2048 + (nb + 1) * 512], kk == 0, kk == 127, [dgB, gbB], [pfB[2 + nb]])
                      if kk < 40:
                          replay(aops, (len(aops) + 39 - kk) // (40 - kk))
                      else:
                          replay(aops, len(aops))
                          replay(tops, (len(tops) + 119 - kk) // max(1, 120 - kk) if kk < 120 else len(tops))
                  replay(aops, len(aops)); replay(tops, len(tops))

              def stageF(tt):
                  x1 = X1[tt % 2]; x1B_ = X1B[tt % 2]
                  for nb in range(4):
                      sl = slice(nb * 512, (nb + 1) * 512)
                      k.tt('dve', ACC[:, sl], pf[2 + nb][:, :], BC[2][:, sl], ALU.mult, [pfB[2 + nb], BCB[2]], [ACCB])
                  k.stt(ACC, x1, ALPHA, ACC, ALU.mult, ALU.add, [x1B_, ACCB], [ACCB])
                  ln_stats(ACC, ACCB, ST2, MV2, ST2B)
                  k.act(ACC, ACC, AF.Identity, [ACCB, ST2B], [ACCB], scale=MV2[:, 3:4], bias=MV2[:, 4:5])
                  k.tt('dve', ACC, ACC, BC[3], ALU.mult, [ACCB, BCB[3]], [ACCB])
                  k.tt('dve', ACC, ACC, BC[4], ALU.add, [ACCB, BCB[4]], [ACCB])
                  k.dma('sp', out[b, tt * 128:(tt + 1) * 128, :], ACC, [ACCB], [outB], ACCB)

              if peer_tiles > 0:
                  stageA(0)
                  for _ in stageT(0):
                      pass
              for tt in range(peer_tiles):
                  aops, tops = [], []
                  if tt + 1 < peer_tiles:
                      aops = capture(stageA, tt + 1)
                      tops = capture(stageT, tt + 1)
                  stageUV(tt, aops, tops)
                  stageF(tt)
              S.barrier()
        except StopBuild as sb:
            S.barrier()
            A.top = A.n - 2048
            dt_ = A.alloc([2048]); dB = Buf('dbgx')
            for i, (ap, n) in enumerate(sb.items):
                k.copy('dve', dt_[:ap.shape[0], 0:n], ap, [], [dB])
                k.dma('sp', dbg[:ap.shape[0], i, 0:n], dt_[:ap.shape[0], 0:n], [dB], [], dB)
        S.barrier()
        S.emit()
    return nc


def _consts():
    c = {}
    c['ident'] = np.eye(128, dtype=np.float32)
    ip = np.arange(128)[:, None]; i = np.arange(128)[None, :]
    mA = np.zeros((128, 31, 128), np.float32)
    for m in range(31):
        d = m - 15
        dt = 16 * (i - ip) + d
        mult = ((dt >= 0) & (dt <= 128)).astype(np.float32)
        if d % 4 == 0:
            mult += ((dt >= 0) & (dt <= 512))
        if d == 0:
            mult += (dt >= 0)
        mA[:, m, :] = mult
    c['maskA'] = mA
    c['maskC'] = (ip <= i).astype(np.float32)
    theta = 500000.0
    invA = theta ** (-np.arange(16, dtype=np.float64) * 2.0 / 32)
    tA = (16 * np.arange(128)[:, None] + np.arange(16)[None, :]).astype(np.float64)
    angA = tA[:, :, None] * invA[None, None, :]
    c['cosA'] = np.cos(angA).astype(np.float32); c['sinA'] = np.sin(angA).astype(np.float32)
    invB = theta ** (-np.arange(32, dtype=np.float64) * 2.0 / 64)
    tB = (128 * np.arange(16)[None, :] + np.arange(128)[:, None]).astype(np.float64)
    angB = tB[:, :, None] * invB[None, None, :]
    c['cosB'] = np.cos(angB).astype(np.float32); c['sinB'] = np.sin(angB).astype(np.float32)
    c['iota'] = np.broadcast_to(np.arange(256, dtype=np.float32), (128, 256)).copy()
    return c


def _prep_shared(inp):
    f = lambda a: np.ascontiguousarray(a, dtype=np.float32)
    sh = dict(_consts())
    w_ada = inp['w_ada'][0]
    sh['w_ada_r'] = f(w_ada.reshape(16, 128, 24, 512).transpose(2, 1, 0, 3))
    sh['b_adaT'] = f(inp['b_ada'][0].reshape(96, 128).T)
    sh['b_ada_row'] = f(inp['b_ada'][0].reshape(1, 12288))
    w_in = inp['w_in'][0]
    sh['w_in_r'] = f(w_in[:, :4096].reshape(16, 128, 8, 512).transpose(2, 1, 0, 3))
    sh['w_in_kr'] = f(w_in[:, 4096:4160].reshape(16, 128, 64).transpose(1, 0, 2))
    sh['g_qT'] = f(inp['g_q_lat'][0].reshape(4, 128).T); sh['g_kvT'] = f(inp['g_kv_lat'][0].reshape(4, 128).T)
    wuq = inp['w_uq'][0].reshape(4, 128, 8, 192).transpose(1, 0, 2, 3)
    sh['w_uqn'] = f(wuq[..., :128]); sh['w_uqr'] = f(wuq[..., 128:])
    sh['w_uk_r'] = f(inp['w_uk'][0].reshape(4, 128, 1024).transpose(1, 0, 2))
    sh['w_uv_r'] = f(inp['w_uv'][0].reshape(4, 128, 1024).transpose(1, 0, 2))
    sh['g_oaT'] = f(inp['g_out_a'][0].reshape(8, 128).T); sh['g_obT'] = f(inp['g_out_b'][0].reshape(8, 128).T)
    sh['w_o_r'] = f(inp['w_o'][0].reshape(16, 128, 2048).transpose(1, 0, 2))
    for n in ('ln1_g', 'ln1_b', 'ln2_g', 'ln2_b'):
        sh[n] = f(inp[n][0].reshape(1, 2048))
    sh['w_pq_r'] = f(inp['w_pq'][0].reshape(16, 128, 16, 128).transpose(2, 1, 0, 3))
    sh['skT'] = f(np.stack([inp['sub_key_1'][0].T, inp['sub_key_2'][0].T], axis=1))
    sh['u_table'] = f(inp['u_table'][0]); sh['v_table'] = f(inp['v_table'][0])
    return sh


def _core_map(sh, inp, b0, nseq):
    m = dict(sh)
    m['x'] = np.ascontiguousarray(inp['x'][b0:b0 + nseq], dtype=np.float32)
    cc = np.asarray(inp['c'][b0:b0 + nseq], dtype=np.float32)
    if nseq == 1:
        cc = np.concatenate([cc, cc], 0)
    m['cT'] = np.ascontiguousarray(cc.reshape(2, 16, 128).transpose(2, 1, 0))
    return m


_NC_CACHE = {}


def kernel(**inputs):
    inp = {k_: np.asarray(v) for k_, v in inputs.items()}
    nseq = 16 // NCORES
    if 'nc' not in _NC_CACHE:
        _NC_CACHE['nc'] = build(nseq=nseq)
    nc = _NC_CACHE['nc']
    sh = _prep_shared(inp)
    maps = [_core_map(sh, inp, c * nseq, nseq) for c in range(NCORES)]
    res = run_bass_kernel_spmd(nc, maps, core_ids=list(range(NCORES)))
    return np.concatenate([r['out'] for r in res.results], axis=0).astype(np.float32)
```
